# Optimizing a Trainium2 kernel written in Bass

```python
import jax, jax.numpy as jnp
from jax import lax
import numpy as np

D_MODEL = 1024
BATCH = 8
SEQ = 4096
DEPTH = 1
DEC_BATCH = 32
DEC_SEQ = 8
PAST_LEN = 16384
PAGE_SIZE = 128

F32 = jnp.float32
EPS = 1e-6
N_META = 16
FOX_HEADS = 8
FOX_HEAD_DIM = D_MODEL // 16
FOX_WIDTH = FOX_HEADS * FOX_HEAD_DIM
FOX_FORGET_BIAS_MIN = 4.0
FOX_FORGET_BIAS_MAX = 10.0
Q_BLOCK = 128
GLA_HEADS = 4
GLA_KEY_DIM = D_MODEL // 16
GLA_VAL_DIM = D_MODEL // 8
GLA_K = GLA_HEADS * GLA_KEY_DIM
GLA_V = GLA_HEADS * GLA_VAL_DIM
GLA_RANK = 16
GLA_TAU = 16.0
GLA_CHUNK = 128
N_GROUPS = 4
EXPERTS_PER_GROUP = 8
N_EXPERTS = N_GROUPS * EXPERTS_PER_GROUP
TOP_K_IN_GROUP = 2
D_EXPERT = D_MODEL // 4
PROJ_SIZES = (FOX_WIDTH, FOX_WIDTH, FOX_WIDTH, FOX_HEADS,
              GLA_K, GLA_K, GLA_V, GLA_RANK, GLA_V,
              D_MODEL, D_MODEL)
PROJ_WIDTH = sum(PROJ_SIZES)
SPLIT_POINTS = tuple(sum(PROJ_SIZES[:i + 1]) for i in range(len(PROJ_SIZES) - 1))

kernel_name = "fox_gla_gated_hier_moe_step"


def rmsnorm(x, g):
    xf = x.astype(F32)
    y = xf * lax.rsqrt(jnp.mean(xf * xf, axis=-1, keepdims=True) + EPS)
    return (y * g.astype(F32)).astype(x.dtype)


def project(h, w_in, fox_f_bias, gla_w_a2, gla_b_a):
    n, l, _ = h.shape
    z = jnp.einsum('nld,dp->nlp', h, w_in)
    qf, kf, vf, ff, qg, kg, vg, lrg, rg, gate_f, gate_g = jnp.split(z, SPLIT_POINTS, axis=-1)
    log_f = jax.nn.log_sigmoid(ff.astype(F32) + fox_f_bias.astype(F32))
    log_a = jax.nn.log_sigmoid((lrg @ gla_w_a2 + gla_b_a).astype(F32)) / GLA_TAU
    fh = lambda t: t.reshape(n, l, FOX_HEADS, FOX_HEAD_DIM)
    gh = lambda t: t.reshape(n, l, GLA_HEADS, -1)
    return (fh(qf), fh(kf), fh(vf), log_f, gh(qg), gh(kg), gh(vg), gh(log_a), rg, gate_f, gate_g)


def fox_attend_prompt(q, k, v, log_f):
    n, l = q.shape[:2]
    scale = FOX_HEAD_DIM ** -0.5
    c = jnp.cumsum(log_f, axis=1)
    c_keys = c.transpose(0, 2, 1)[:, :, None, :]
    key_pos = jnp.arange(l)

    def attend(qb, cb, qpos):
        s = jnp.einsum('nthd,nshd->nhts', qb, k, preferred_element_type=F32) * scale
        s = s + cb.transpose(0, 2, 1)[..., None] - c_keys
        s = jnp.where((key_pos[None, :] <= qpos[:, None])[None, None], s, -jnp.inf)
        p = jax.nn.softmax(s, axis=-1)
        return jnp.einsum('nhts,nshd->nthd', p.astype(v.dtype), v)

    o_meta = attend(q[:, :N_META], c[:, :N_META], jnp.arange(N_META))
    n_blocks = (l - N_META) // Q_BLOCK

    def block(i):
        start = N_META + i * Q_BLOCK
        qb = lax.dynamic_slice_in_dim(q, start, Q_BLOCK, axis=1)
        cb = lax.dynamic_slice_in_dim(c, start, Q_BLOCK, axis=1)
        return attend(qb, cb, start + jnp.arange(Q_BLOCK))

    o_blocks = lax.map(block, jnp.arange(n_blocks))
    o_real = o_blocks.transpose(1, 0, 2, 3, 4).reshape(n, l - N_META, FOX_HEADS, FOX_HEAD_DIM)
    return jnp.concatenate([o_meta, o_real], axis=1)


def fox_attend_sample(q, k, v, log_f, k_past, v_past, log_f_past):
    scale = FOX_HEAD_DIM ** -0.5
    t = q.shape[1]
    p_len = k_past.shape[1]
    lfp = log_f_past.astype(F32)
    r = lax.cumsum(lfp, axis=1, reverse=True) - lfp
    a = jnp.cumsum(log_f, axis=1)
    a_q = a.transpose(0, 2, 1)[..., None]
    s_past = (jnp.einsum('nthd,nshd->nhts', q, k_past, preferred_element_type=F32) * scale
              + a_q + r.transpose(0, 2, 1)[:, :, None, :])
    s_new = (jnp.einsum('nthd,nshd->nhts', q, k, preferred_element_type=F32) * scale
             + a_q - a.transpose(0, 2, 1)[:, :, None, :])
    s_new = jnp.where(jnp.tril(jnp.ones((t, t), bool)), s_new, -jnp.inf)
    p = jax.nn.softmax(jnp.concatenate([s_past, s_new], axis=-1), axis=-1).astype(v.dtype)
    return (jnp.einsum('nhts,nshd->nthd', p[..., :p_len], v_past)
            + jnp.einsum('nhts,nshd->nthd', p[..., p_len:], v))


def gla_chunk(q, k, v, log_a, s0):
    c = q.shape[1]
    qf = q.astype(F32) * GLA_KEY_DIM ** -0.5
    kf = k.astype(F32)
    vf = v.astype(F32)
    b = jnp.cumsum(log_a, axis=1)
    o_inter = jnp.einsum('nchk,nhkv->nchv', qf * jnp.exp(b), s0)
    causal = jnp.tril(jnp.ones((c, c), bool))[None, :, :, None, None]
    decay = jnp.exp(jnp.where(causal, b[:, :, None] - b[:, None, :], -jnp.inf))
    scores = jnp.einsum('ntshk,nshk->ntsh', qf[:, :, None] * decay, kf)
    o_intra = jnp.einsum('ntsh,nshv->nthv', scores, vf)
    b_end = b[:, -1]
    k_dec = kf * jnp.exp(b_end[:, None] - b)
    s_new = jnp.exp(b_end)[..., None] * s0 + jnp.einsum('nchk,nchv->nhkv', k_dec, vf)
    return o_inter + o_intra, s_new


def gla_prompt(q, k, v, log_a):
    n, l = q.shape[:2]
    s0 = jnp.zeros((n, GLA_HEADS, GLA_KEY_DIM, GLA_VAL_DIM), F32)
    o_meta, s_meta = gla_chunk(q[:, :N_META], k[:, :N_META], v[:, :N_META], log_a[:, :N_META], s0)
    n_chunks = (l - N_META) // GLA_CHUNK

    def chunks(t):
        t = t[:, N_META:]
        return t.reshape((n, n_chunks, GLA_CHUNK) + t.shape[2:]).swapaxes(0, 1)

    def step(s, inp):
        o, s_next = gla_chunk(*inp, s)
        return s_next, o

    s_final, o_chunks = lax.scan(step, s_meta, (chunks(q), chunks(k), chunks(v), chunks(log_a)))
    o_real = o_chunks.swapaxes(0, 1).reshape(n, l - N_META, GLA_HEADS, GLA_VAL_DIM)
    return jnp.concatenate([o_meta, o_real], axis=1), s_final


def merge_branches(o_fox, o_gla, r, gate_f, gate_g, gla_norm_g, w_fox_out, w_gla_out, w_o):
    n, l = o_fox.shape[:2]
    of = o_fox.reshape(n, l, FOX_WIDTH)
    ogn = o_gla * lax.rsqrt(jnp.mean(o_gla * o_gla, axis=-1, keepdims=True) + EPS)
    og = (ogn.reshape(n, l, GLA_V) * gla_norm_g.astype(F32) * jax.nn.silu(r.astype(F32))).astype(r.dtype)
    branch_f = of @ w_fox_out
    branch_g = og @ w_gla_out
    merged = jax.nn.sigmoid(gate_f) * branch_f + jax.nn.sigmoid(gate_g) * branch_g
    return merged @ w_o


def hier_moe(h, w_group, b_group, w_er, b_er, w_g, w_u, w_d):
    n, l, d = h.shape
    hf = h.reshape(n * l, d)
    g_logits = (hf @ w_group + b_group).astype(F32)
    g_prob = jax.nn.softmax(g_logits, axis=-1)
    g_idx = jnp.argmax(g_logits, axis=-1)
    g_sel = jnp.max(g_prob, axis=-1)
    e_logits = (hf @ w_er + b_er).astype(F32).reshape(n * l, N_GROUPS, EXPERTS_PER_GROUP)
    e_in_group = jnp.sum(e_logits * jax.nn.one_hot(g_idx, N_GROUPS, dtype=F32)[..., None], axis=1)
    vals, idx = lax.top_k(e_in_group, TOP_K_IN_GROUP)
    w_sel = jax.nn.softmax(vals, axis=-1) * g_sel[:, None]
    e_id = g_idx[:, None] * EXPERTS_PER_GROUP + idx
    gates = jnp.sum(jax.nn.one_hot(e_id, N_EXPERTS, dtype=F32) * w_sel[..., None], axis=1).astype(h.dtype)
    y = jnp.zeros_like(hf)
    for e in range(N_EXPERTS):
        he = jax.nn.silu(hf @ w_g[e]) * (hf @ w_u[e])
        y = y + gates[:, e:e + 1] * (he @ w_d[e])
    return y.reshape(n, l, d)


def setup_inputs(seed: int = 0) -> dict:
    key = jax.random.key(seed)
    ks = jax.random.split(key, 32)
    n_pages = PAST_LEN // PAGE_SIZE
    n_used = DEC_BATCH * n_pages
    n_phys = (5 * n_used + 3) // 4
    nrm = lambda k, shape, scale=1.0: scale * jax.random.normal(k, shape, F32)
    head_bias = jnp.linspace(FOX_FORGET_BIAS_MIN, FOX_FORGET_BIAS_MAX, FOX_HEADS, dtype=F32)
    return {
        "x_prompt": nrm(ks[0], (BATCH, SEQ, D_MODEL)),
        "x_sample": nrm(ks[1], (DEC_BATCH, DEC_SEQ, D_MODEL)),
        "cache_k": nrm(ks[2], (DEPTH, n_phys, PAGE_SIZE, FOX_HEADS, FOX_HEAD_DIM)),
        "cache_v": nrm(ks[3], (DEPTH, n_phys, PAGE_SIZE, FOX_HEADS, FOX_HEAD_DIM)),
        "cache_log_f": jax.nn.log_sigmoid(head_bias + nrm(ks[4], (DEPTH, n_phys, PAGE_SIZE, FOX_HEADS))),
        "state_gla": nrm(ks[5], (DEPTH, DEC_BATCH, GLA_HEADS, GLA_KEY_DIM, GLA_VAL_DIM), 0.5),
        "page_table": jax.random.permutation(ks[6], n_phys)[:n_used].reshape(DEC_BATCH, n_pages).astype(jnp.int32),
        "meta_tokens": nrm(ks[7], (N_META, D_MODEL)),
        "norm_mix_g": 1.0 + nrm(ks[8], (DEPTH, D_MODEL), 0.02),
        "w_in": nrm(ks[9], (DEPTH, D_MODEL, PROJ_WIDTH), D_MODEL ** -0.5),
        "fox_f_bias": head_bias + nrm(ks[10], (DEPTH, FOX_HEADS), 0.1),
        "gla_w_a2": nrm(ks[11], (DEPTH, GLA_RANK, GLA_K), GLA_RANK ** -0.5),
        "gla_b_a": nrm(ks[12], (DEPTH, GLA_K), 0.1),
        "gla_norm_g": 1.0 + nrm(ks[13], (DEPTH, GLA_V), 0.02),
        "w_fox_out": nrm(ks[14], (DEPTH, FOX_WIDTH, D_MODEL), FOX_WIDTH ** -0.5),
        "w_gla_out": nrm(ks[15], (DEPTH, GLA_V, D_MODEL), GLA_V ** -0.5),
        "w_o": nrm(ks[16], (DEPTH, D_MODEL, D_MODEL), D_MODEL ** -0.5),
        "norm_ffn_g": 1.0 + nrm(ks[17], (DEPTH, D_MODEL), 0.02),
        "w_group_router": nrm(ks[18], (DEPTH, D_MODEL, N_GROUPS), D_MODEL ** -0.5),
        "b_group_router": nrm(ks[19], (DEPTH, N_GROUPS), 0.01),
        "w_expert_router": nrm(ks[20], (DEPTH, D_MODEL, N_EXPERTS), D_MODEL ** -0.5),
        "b_expert_router": nrm(ks[21], (DEPTH, N_EXPERTS), 0.01),
        "w_expert_gate": nrm(ks[22], (DEPTH, N_EXPERTS, D_MODEL, D_EXPERT), D_MODEL ** -0.5),
        "w_expert_up": nrm(ks[23], (DEPTH, N_EXPERTS, D_MODEL, D_EXPERT), D_MODEL ** -0.5),
        "w_expert_down": nrm(ks[24], (DEPTH, N_EXPERTS, D_EXPERT, D_MODEL), D_EXPERT ** -0.5),
        "norm_final_g": 1.0 + nrm(ks[25], (D_MODEL,), 0.02),
    }


def reference(x_prompt, x_sample, cache_k, cache_v, cache_log_f, state_gla, page_table,
              meta_tokens, norm_mix_g, w_in, fox_f_bias, gla_w_a2, gla_b_a, gla_norm_g,
              w_fox_out, w_gla_out, w_o, norm_ffn_g, w_group_router, b_group_router,
              w_expert_router, b_expert_router, w_expert_gate, w_expert_up, w_expert_down,
              norm_final_g):
    b = x_prompt.shape[0]
    db = x_sample.shape[0]
    past = page_table.shape[1] * PAGE_SIZE
    meta = jnp.broadcast_to(meta_tokens.astype(x_prompt.dtype)[None], (b, N_META, D_MODEL))
    xp = jnp.concatenate([meta, x_prompt], axis=1)
    xs = x_sample
    kp, vp, lfp, sgp, ksm, vsm, lfs, sgs = [], [], [], [], [], [], [], []
    for layer in range(DEPTH):
        proj_w = (w_in[layer], fox_f_bias[layer], gla_w_a2[layer], gla_b_a[layer])
        merge_w = (gla_norm_g[layer], w_fox_out[layer], w_gla_out[layer], w_o[layer])
        moe_w = (w_group_router[layer], b_group_router[layer], w_expert_router[layer],
                 b_expert_router[layer], w_expert_gate[layer], w_expert_up[layer], w_expert_down[layer])
        qf, kf, vf, lf, qg, kg, vg, la, r, gf, gg = project(rmsnorm(xp, norm_mix_g[layer]), *proj_w)
        o_fox = fox_attend_prompt(qf, kf, vf, lf)
        o_gla, s_gla = gla_prompt(qg, kg, vg, la)
        xp = xp + merge_branches(o_fox, o_gla, r, gf, gg, *merge_w)
        xp = xp + hier_moe(rmsnorm(xp, norm_ffn_g[layer]), *moe_w)
        kp.append(kf); vp.append(vf); lfp.append(lf); sgp.append(s_gla)
        qf, kf, vf, lf, qg, kg, vg, la, r, gf, gg = project(rmsnorm(xs, norm_mix_g[layer]), *proj_w)
        k_past = cache_k[layer][page_table].reshape(db, past, FOX_HEADS, FOX_HEAD_DIM)
        v_past = cache_v[layer][page_table].reshape(db, past, FOX_HEADS, FOX_HEAD_DIM)
        lf_past = cache_log_f[layer][page_table].reshape(db, past, FOX_HEADS)
        o_fox = fox_attend_sample(qf, kf, vf, lf, k_past, v_past, lf_past)
        o_gla, s_gla = gla_chunk(qg, kg, vg, la, state_gla[layer].astype(F32))
        xs = xs + merge_branches(o_fox, o_gla, r, gf, gg, *merge_w)
        xs = xs + hier_moe(rmsnorm(xs, norm_ffn_g[layer]), *moe_w)
        ksm.append(kf); vsm.append(vf); lfs.append(lf); sgs.append(s_gla)
    y_prompt = rmsnorm(xp, norm_final_g)[:, N_META:]
    y_sample = rmsnorm(xs, norm_final_g)
    new_k_prompt = jnp.stack(kp)
    new_v_prompt = jnp.stack(vp)
    new_log_f_prompt = jnp.stack(lfp)
    new_gla_prompt = jnp.stack(sgp)
    new_k_sample = jnp.stack(ksm)
    new_v_sample = jnp.stack(vsm)
    new_log_f_sample = jnp.stack(lfs)
    new_gla_sample = jnp.stack(sgs)
    return (y_prompt, y_sample, new_k_prompt, new_v_prompt, new_log_f_prompt, new_gla_prompt,
            new_k_sample, new_v_sample, new_log_f_sample, new_gla_sample)
```

```python
import numpy as np
import concourse.bass as bass
import concourse.mybir as mybir
from concourse.bass_utils import run_bass_kernel_spmd

F32 = mybir.dt.float32
BF16 = mybir.dt.bfloat16
I32 = mybir.dt.int32
AF = mybir.ActivationFunctionType
ALU = mybir.AluOpType
AX = mybir.AxisListType
ET = mybir.EngineType

D = 1024
PW = 5144
C_QF, C_KF, C_VF, C_FF, C_QG, C_KG, C_VG, C_LR, C_RG, C_GF, C_GG = (
    0, 512, 1024, 1536, 1544, 1800, 2056, 2568, 2584, 3096, 4120)
EPS = 1e-6
NE = 32
NSD = 12
SB_BASE = 16640
DEBUG = False
STOP = ''
DEBUG_TILE = 0


class StopBuild(Exception):
    pass


class Buf:
    __slots__ = ("w", "r", "name", "excl")

    def __init__(self, name="", excl=False):
        self.w = None
        self.r = {}
        self.name = name
        self.excl = excl


class Eng:
    def __init__(self, name, e, sem):
        self.name, self.e, self.sem, self.cnt, self.waited = name, e, sem, 0, {}


class Queue:
    def __init__(self, E, sems):
        self.E, self.sems, self.k = E, sems, 0


class Sched:
    def __init__(self, nc):
        self.nc = nc
        mk = lambda n: nc.alloc_semaphore(n)
        self.PE = Eng("pe", nc.tensor, mk("s_pe"))
        self.ACT = Eng("act", nc.scalar, mk("s_act"))
        self.DVE = Eng("dve", nc.vector, mk("s_dve"))
        self.POOL = Eng("pool", nc.gpsimd, mk("s_pool"))
        self.SP = Eng("sp", nc.sync, mk("s_sp"))
        self.engs = [self.PE, self.ACT, self.DVE, self.POOL, self.SP]
        self.QS = Queue(self.SP, [mk(f"s_qs{i}") for i in range(NSD)])
        self.QP = Queue(self.POOL, [mk(f"s_qp{i}") for i in range(NSD)])

    def _wait(self, E, need):
        for s, v in need.items():
            if E is self.PE and s is self.PE.sem:
                continue
            if E.waited.get(s, 0) < v:
                E.e.wait_ge(s, v)
                E.waited[s] = v

    @staticmethod
    def _need(r, w, own=None):
        need = {}

        def add(tok):
            if tok is None:
                return
            s, v = tok
            if need.get(s, 0) < v:
                need[s] = v
        for b in r:
            add(b.w)
            if b.excl:
                for s, v in b.r.items():
                    if s is not own:
                        add((s, v))
        for b in w:
            add(b.w)
            for s, v in b.r.items():
                add((s, v))
        return need

    @staticmethod
    def _mark(tok, r, w):
        s, v = tok
        for b in r:
            if b.r.get(s, 0) < v:
                b.r[s] = v
        for b in w:
            b.w = tok
            b.r = {}

    def op(self, E, fn, r=(), w=()):
        self._wait(E, self._need(r, w, E.sem))
        ins = fn(E.e)
        E.cnt += 1
        ins.then_inc(E.sem, 1)
        self._mark((E.sem, E.cnt), r, w)
        return ins

    def dma(self, Q, out, in_, r=(), w=(), **kw):
        E = Q.E
        k = Q.k
        Q.k += 1
        s = Q.sems[k % NSD]
        base = 16 * (k // NSD)
        need = self._need(r, w)
        if base > 0 and need.get(s, 0) < base:
            need[s] = base
        self._wait(E, need)
        ins = E.e.dma_start(out=out, in_=in_, **kw)
        ins.then_inc(s, 16)
        self._mark((s, base + 16), r, w)
        return ins

    def idma(self, out, in_, idx_ap, r=(), w=()):
        Q = self.QP
        E = Q.E
        k = Q.k
        Q.k += 1
        s = Q.sems[k % NSD]
        base = 16 * (k // NSD)
        need = self._need(r, w)
        if base > 0 and need.get(s, 0) < base:
            need[s] = base
        self._wait(E, need)
        ins = E.e.indirect_dma_start(out=out, out_offset=None, in_=in_,
                                     in_offset=bass.IndirectOffsetOnAxis(idx_ap, 0))
        ins.then_inc(s, 16)
        self._mark((s, base + 16), r, w)
        return ins

    def barrier(self):
        toks = {}
        for E in self.engs:
            if E.cnt:
                toks[E.sem] = E.cnt
        for Q in (self.QS, self.QP):
            for i, s in enumerate(Q.sems):
                n = (Q.k - i + NSD - 1) // NSD if Q.k > i else 0
                if n:
                    toks[s] = 16 * n
        for E in self.engs:
            need = {s: v for s, v in toks.items() if s is not E.sem}
            for s, v in need.items():
                if E.waited.get(s, 0) < v:
                    E.e.wait_ge(s, v)
                    E.waited[s] = v


class Alloc:
    def __init__(self, nc):
        self.nc, self.cur, self.n = nc, SB_BASE, 0

    def t(self, shape, dt, name=None):
        isz = {F32: 4, BF16: 2, I32: 4}[dt]
        per = int(np.prod(shape[1:])) * isz
        per = (per + 63) // 64 * 64
        self.n += 1
        h = self.nc.alloc_sbuf_tensor_at(f"{name or 't'}_{self.n}", list(shape), dt, offset=self.cur)
        self.cur += per
        assert self.cur <= 229000, f"SBUF overflow {self.cur}"
        return h


def build(NPT, NSS, NPG, NPHYS):
    nc = bass.Bass("TRN2", target_bir_lowering=False)
    st = {}
    try:
        _build(nc, st, NPT, NSS, NPG, NPHYS)
    except StopBuild:
        st['S'].barrier()
    return nc


def _build(nc, st, NPT, NSS, NPG, NPHYS):
    LTOK = 16 + NPT * 128
    TOK = LTOK + NSS * 8
    NJ = NPT + 1

    def din(name, shape, dt=F32):
        return nc.dram_tensor(name, list(shape), dt, kind="ExternalInput").ap()

    def dout(name, shape, dt=F32):
        return nc.dram_tensor(name, list(shape), dt, kind="ExternalOutput").ap()

    def dscr(name, shape, dt=F32):
        return nc.dram_tensor(name, list(shape), dt, kind="Internal").ap()

    xp = din("xp", [NPT * 128, D])
    xs = din("xs", [NSS * 8, D])
    ck = din("ck", [NPHYS, 128, 512])
    cv = din("cv", [NPHYS, 128, 512])
    clf = din("clf", [NPHYS, 1024])
    sgl = din("sgl", [NSS, 4, 64, 128])
    ptab = din("ptab", [1, NSS * NPG], I32)
    ptcol = din("ptcol", [NSS * NPG, 1], I32)
    meta = din("meta", [16, D])
    g_mix = din("g_mix", [D])
    w_in = din("w_in", [D, PW])
    f_bias = din("f_bias", [8])
    w_a2 = din("w_a2", [16, 256])
    b_a = din("b_a", [256])
    g_gla = din("g_gla", [512])
    w_fo = din("w_fo", [512, D])
    w_go = din("w_go", [512, D])
    w_o = din("w_o", [D, D])
    g_ffn = din("g_ffn", [D])
    w_gr = din("w_gr", [D, 4])
    b_gr = din("b_gr", [4])
    w_er = din("w_er", [D, 32])
    b_er = din("b_er", [32])
    w_eg = din("w_eg", [NE, D, 256])
    w_eu = din("w_eu", [NE, D, 256])
    w_ed = din("w_ed", [NE, 256, D])
    g_fin = din("g_fin", [D])

    y_p = dout("y_p", [NPT * 128, D])
    y_s = dout("y_s", [NSS * 8, D])
    nk_p = dout("nk_p", [LTOK, 512])
    nv_p = dout("nv_p", [LTOK, 512])
    nlf_p = dout("nlf_p", [LTOK, 8])
    ngl_p = dout("ngl_p", [4, 64, 128])
    nk_s = dout("nk_s", [NSS * 8, 512])
    nv_s = dout("nv_s", [NSS * 8, 512])
    nlf_s = dout("nlf_s", [NSS * 8, 8])
    ngl_s = dout("ngl_s", [NSS, 4, 64, 128])

    qT_scr = dscr("qT_scr", [128, 4, TOK], BF16)
    kT_scr = dscr("kT_scr", [128, 4, TOK], BF16)
    v1_scr = dscr("v1_scr", [TOK, 520], BF16)
    c_scr = dscr("c_scr", [TOK, 8])
    sgf_scr = dscr("sgf_scr", [TOK, D], BF16)
    mg_scr = dscr("mg_scr", [TOK, D], BF16)
    x1_scr = dscr("x1_scr", [TOK, D])
    h2T_scr = dscr("h2T_scr", [128, 8, TOK], BF16)
    gat_scr = dscr("gat_scr", [TOK, 32])

    _lp = nc.allow_low_precision(reason="bf16 matmul operands by design; fp32 accumulation")
    _lp.__enter__()
    S = Sched(nc)
    st['S'] = S
    PE, ACT, DVE, POOL, SP = S.PE, S.ACT, S.DVE, S.POOL, S.SP
    QS, QP = S.QS, S.QP
    A = Alloc(nc)

    psb = [nc.alloc_psum_tensor(f"ps{i}", [128, 512], F32) for i in range(8)]
    psB = [Buf(f"ps{i}", excl=True) for i in range(8)]
    rot = {"i": 0, "n": 8}

    def ps():
        i = rot["i"] % rot["n"]
        rot["i"] += 1
        return psb[i], psB[i]

    def bfv(p):
        return p[:, :].bitcast(BF16)

    cnt = {"i": 0}

    def T(shape, dt, name="t"):
        return A.t(shape, dt, name), Buf(name)

    def T2(shape, dt, name="t", n=2):
        return [T(shape, dt, f"{name}{i}") for i in range(n)]

    ones_f, B_ones = T([128, 128], F32, "ones")
    tri_f, B_tri = T([128, 128], F32, "tri")
    triR_f, B_triR = T([128, 128], F32, "triR")
    id_f, B_idf = T([128, 128], F32, "idf")
    id_b, B_idb = T([128, 128], BF16, "idb")
    tri_b, B_trib = T([128, 128], BF16, "trib")
    ones_b, B_onesb = T([128, 128], BF16, "onesb")
    zer_b, B_zer = T([128, 512], BF16, "zer")
    junk = A.t([128, 1024], BF16, "junk")

    S.op(POOL, lambda e: e.memset(ones_f[:, :], 1.0), w=[B_ones])
    S.op(POOL, lambda e: e.memset(zer_b[:, :], 0.0), w=[B_zer])
    S.op(POOL, lambda e: e.affine_select(out=tri_f[:, :], in_=ones_f[:, :], pattern=[[1, 128]],
                                         compare_op=ALU.is_ge, fill=0.0, base=0, channel_multiplier=-1),
         r=[B_ones], w=[B_tri])
    S.op(POOL, lambda e: e.affine_select(out=triR_f[:, :], in_=ones_f[:, :], pattern=[[-1, 128]],
                                         compare_op=ALU.is_ge, fill=0.0, base=-1, channel_multiplier=1),
         r=[B_ones], w=[B_triR])
    S.op(POOL, lambda e: e.affine_select(out=id_f[:, :], in_=ones_f[:, :], pattern=[[1, 128]],
                                         compare_op=ALU.is_equal, fill=0.0, base=0, channel_multiplier=-1),
         r=[B_ones], w=[B_idf])
    S.op(POOL, lambda e: e.tensor_copy(out=id_b[:, :], in_=id_f[:, :]), r=[B_idf], w=[B_idb])
    S.op(POOL, lambda e: e.tensor_copy(out=tri_b[:, :], in_=tri_f[:, :]), r=[B_tri], w=[B_trib])
    S.op(POOL, lambda e: e.tensor_copy(out=ones_b[:, :], in_=ones_f[:, :]), r=[B_ones], w=[B_onesb])

    def bc_load(src1d, n, name):
        t, b = T([128, n], F32, name)
        S.dma(QS, t[:, :], src1d.partition_broadcast(128), w=[b])
        return t, b

    gmix_bc, B_gmix = bc_load(g_mix, D, "gmix")
    gffn_bc, B_gffn = bc_load(g_ffn, D, "gffn")
    ggla_bc, B_ggla = bc_load(g_gla, 512, "ggla")
    fb_bc, B_fb = bc_load(f_bias, 8, "fb")
    rb_bc, B_rb = T([128, 36], F32, "rb")
    S.dma(QS, rb_bc[:, 0:4], b_gr.partition_broadcast(128), w=[B_rb])
    S.dma(QS, rb_bc[:, 4:36], b_er.partition_broadcast(128), w=[B_rb])
    nba, B_nba = T([128, 2], F32, "nba")
    with nc.allow_non_contiguous_dma(reason="tiny bias column load"):
        S.dma(QS, nba[:, :], b_a.rearrange("(c p) -> p c", p=128), w=[B_nba])
    S.op(DVE, lambda e: e.tensor_scalar(out=nba[:, :], in0=nba[:, :], scalar1=-1.0, scalar2=None,
                                        op0=ALU.mult), r=[B_nba], w=[B_nba])
    wa2, B_wa2 = T([16, 256], F32, "wa2")
    S.dma(QS, wa2[:, :], w_a2, w=[B_wa2])
    wr, B_wr = T([128, 8, 36], F32, "wr")
    S.dma(QS, wr[:, :, 0:4], w_gr.rearrange("(c p) n -> p c n", p=128), w=[B_wr])
    S.dma(QS, wr[:, :, 4:36], w_er.rearrange("(c p) n -> p c n", p=128), w=[B_wr])
    NPGT = NSS * NPG
    ptb_i, B_ptbi = T([128, NPGT], I32, "ptbi")
    S.dma(QS, ptb_i[:, :], ptab[0].partition_broadcast(128), w=[B_ptbi])
    slot_i, B_sloti = T([128, 1], I32, "sloti")
    S.op(POOL, lambda e: e.iota(out=slot_i[:, :], pattern=[[0, 1]], base=0, channel_multiplier=1), w=[B_sloti])
    slot_f, B_slotf = T([128, 1], F32, "slotf")
    S.op(DVE, lambda e: e.tensor_copy(out=slot_f[:, :], in_=slot_i[:, :]), r=[B_sloti], w=[B_slotf])
    ptb_f, B_ptbf = T([128, NPGT], F32, "ptbf")
    S.op(DVE, lambda e: e.tensor_copy(out=ptb_f[:, :], in_=ptb_i[:, :]), r=[B_ptbi], w=[B_ptbf])
    S.op(DVE, lambda e: e.tensor_scalar(out=ptb_f[:, :], in0=ptb_f[:, :], scalar1=128.0, scalar2=slot_f[:, 0:1],
                                        op0=ALU.mult, op1=ALU.add), r=[B_ptbf, B_slotf], w=[B_ptbf])
    idx_all, B_idx = T([128, NPGT], I32, "idxall")
    S.op(DVE, lambda e: e.tensor_copy(out=idx_all[:, :], in_=ptb_f[:, :]), r=[B_ptbf], w=[B_idx])
    ck_rows = ck.rearrange("n s f -> (n s) f")
    cv_rows = cv.rearrange("n s f -> (n s) f")
    cref, B_cref = T([128, NPT // 4 + 2, 8], F32, "cref")
    carry, B_carry = T([128, 8], F32, "carry")
    S.op(DVE, lambda e: e.memset(carry[:, :], 0.0), w=[B_carry])
    wgo, B_wgo = T([128, 4, D], BF16, "wgo")
    for c in range(4):
        S.dma(QP, wgo[:, c, :], w_go[c * 128:(c + 1) * 128, :], w=[B_wgo])
    eps_c, B_eps = T([128, 1], F32, "eps")
    S.op(DVE, lambda e: e.memset(eps_c[:, :], EPS), w=[B_eps])
    one_c, B_one = T([128, 1], F32, "onec")
    S.op(DVE, lambda e: e.memset(one_c[:, :], 1.0), w=[B_one])
    hm = []
    for par in range(2):
        t_, b_ = T([128, 1], F32, f"hm{par}")
        S.op(DVE, lambda e, t_=t_: e.memset(t_[:, :], 0.0), w=[b_])
        S.op(DVE, lambda e, t_=t_, par=par: e.memset(t_[par * 64:(par + 1) * 64, :], 0.125), w=[b_])
        hm.append((t_, b_))
    persist_mark = A.cur

    if STOP == "0":
        S.barrier()
        return nc
    win, B_win = T([128, 8, PW], BF16, "win")
    w_in3 = w_in.rearrange("(c p) n -> p c n", p=128)
    for c0 in range(0, PW, 2048):
        c1 = min(PW, c0 + 2048)
        S.dma(QP, win[:, :, c0:c1], w_in3[:, :, c0:c1], w=[B_win])

    xt = T2([128, D], F32, "xt")
    ssq = T2([128, 1], F32, "ssq")
    rstd = T2([128, 1], F32, "rstd")
    hb = T2([128, D], BF16, "hb")
    hT = T2([128, 8, 128], BF16, "hT")
    qTt = T2([128, 4, 128], BF16, "qTt")
    kTt = T2([128, 4, 128], BF16, "kTt")
    lrgT = T2([16, 128], F32, "lrgT")
    kout = T2([128, 512], F32, "kout")
    vout = T2([128, 512], F32, "vout")
    v1t = T2([128, 8, 65], BF16, "v1t")
    vgb = T2([128, 512], BF16, "vgb")
    etmp = T2([128, D], F32, "etmp", 3)
    rsil = T2([128, 512], F32, "rsil")
    sgf = T2([128, D], BF16, "sgf")
    sgg = T2([128, D], BF16, "sgg")
    lft = T2([128, 8], F32, "lft")
    ltmp = T2([128, 8], F32, "ltmp")
    ctile = T2([128, 8], F32, "ctile")
    ea = T2([128, 2, 128], F32, "ea")
    csum = T2([128, 2, 128], F32, "csum")
    ebt = T2([128, 2, 128], F32, "ebt")
    enbt = T2([128, 2, 128], F32, "enbt")
    qtl = T2([128, 2, 2, 128], BF16, "qtl")
    ktl = T2([128, 2, 128], BF16, "ktl")
    ktok = T2([128, 256], BF16, "ktok")
    Am = T2([128, 4, 128], BF16, "Am")
    Sst, B_S = T([128, 2, 128], F32, "S")
    Sb, B_Sb = T([128, 2, 128], BF16, "Sb")
    ssg = T2([128, 4], F32, "ssg")
    rsg = T2([128, 4], F32, "rsg")
    o1 = T2([128, 512], F32, "o1")
    ogb = T2([128, 4, 128], BF16, "ogb")
    ogT = T2([128, 4, 128], BF16, "ogT")
    mgt = T2([128, D], BF16, "mgt")
    for i in range(2):
        S.op(POOL, lambda e, i=i: e.memset(v1t[i][0][:, :, 64:65], 1.0), w=[v1t[i][1]])

    def rmsnorm_stats(x_ap, Bx, nt, ss_, rs_, n):
        S.op(ACT, lambda e: e.activation(out=junk[0:nt, 0:n], in_=x_ap, func=AF.Square,
                                         accum_out=ss_[0][0:nt, :]), r=[Bx], w=[ss_[1]])
        S.op(ACT, lambda e: e.activation(out=rs_[0][0:nt, :], in_=ss_[0][0:nt, :], func=AF.Ln,
                                         scale=1.0 / n, bias=eps_c[0:nt, :]), r=[ss_[1], B_eps], w=[rs_[1]])
        S.op(ACT, lambda e: e.activation(out=rs_[0][0:nt, :], in_=rs_[0][0:nt, :], func=AF.Exp,
                                         scale=-0.5), r=[rs_[1]], w=[rs_[1]])

    def sigmoid_from(ps_ap, Bp, nt, n, tmp, out_ap, Bout):
        S.op(ACT, lambda e: e.activation(out=tmp[0][0:nt, 0:n], in_=ps_ap, func=AF.Exp, scale=-1.0),
             r=[Bp], w=[tmp[1]])
        S.op(POOL, lambda e: e.tensor_scalar(out=tmp[0][0:nt, 0:n], in0=tmp[0][0:nt, 0:n], scalar1=1.0,
                                             scalar2=None, op0=ALU.add), r=[tmp[1]], w=[tmp[1]])
        S.op(DVE, lambda e: e.reciprocal(out=out_ap, in_=tmp[0][0:nt, 0:n]), r=[tmp[1]], w=[Bout])

    def proj_tok(hTs, nt, col0, ncols=512):
        p, Bp = ps()
        for c in range(8):
            S.op(PE, lambda e, c=c: e.matmul(out=p[0:nt, 0:ncols], lhsT=hTs[0][:, c, 0:nt],
                                             rhs=win[:, c, col0:col0 + ncols], start=(c == 0), stop=(c == 7)),
                 r=[hTs[1], B_win], w=[Bp])
        return p, Bp

    def proj_fm(hTs, nt, col0, nblk, m=128):
        p, Bp = ps()
        for b in range(nblk):
            for c in range(8):
                S.op(PE, lambda e, c=c, b=b: e.matmul(out=p[0:m, b * 128:b * 128 + nt],
                                                      lhsT=win[:, c, col0 + b * 128:col0 + b * 128 + m],
                                                      rhs=hTs[0][:, c, 0:nt], start=(c == 0), stop=(c == 7)),
                     r=[hTs[1], B_win], w=[Bp])
        return p, Bp

    def chk(tag):
        if STOP == tag:
            raise StopBuild()

    def phase1a(ti, kind, nt, idx, tok0):
        k = ti % 2
        X, SS, RS, HB, HT = xt[k], ssq[k], rstd[k], hb[k], hT[k]
        src = meta if kind == "m" else (xp[idx * 128:(idx + 1) * 128, :] if kind == "p"
                                        else xs[idx * 8:(idx + 1) * 8, :])
        S.dma(QS, X[0][0:nt, :], src, w=[X[1]])
        rmsnorm_stats(X[0][0:nt, :], X[1], nt, SS, RS, D)
        S.op(DVE, lambda e: e.scalar_tensor_tensor(out=HB[0][0:nt, :], in0=X[0][0:nt, :], scalar=RS[0][0:nt, :],
                                                   in1=gmix_bc[0:nt, :], op0=ALU.mult, op1=ALU.mult),
             r=[X[1], RS[1], B_gmix], w=[HB[1]])
        p, Bp = ps()
        pv = bfv(p)
        for c in range(8):
            S.op(PE, lambda e, c=c: e.transpose(out=pv[:, c * 128:c * 128 + nt], in_=HB[0][0:nt, c * 128:(c + 1) * 128],
                                                identity=id_b[0:nt, 0:nt]), r=[HB[1], B_idb], w=[Bp])
        S.op(ACT, lambda e: e.activation(out=HT[0][:, :, 0:nt],
                                         in_=pv.rearrange("p (c t) -> p c t", c=8)[:, :, 0:nt], func=AF.Copy),
             r=[Bp], w=[HT[1]])
        chk("a1")
        for (col0, TT, scr) in ((C_QF, qTt[k], qT_scr), (C_KF, kTt[k], kT_scr)):
            p, Bp = proj_fm(HT, nt, col0, 4)
            S.op(ACT, lambda e, p=p, TT=TT: e.activation(out=TT[0][:, :, 0:nt],
                                                         in_=p[:, :].rearrange("p (c t) -> p c t", c=4)[:, :, 0:nt],
                                                         func=AF.Copy), r=[Bp], w=[TT[1]])
            S.dma(QS, scr[:, :, tok0:tok0 + nt], TT[0][:, :, 0:nt], r=[TT[1]])
        p, Bp = proj_fm(HT, nt, C_LR, 1, m=16)
        LR = lrgT[k]
        S.op(ACT, lambda e: e.activation(out=LR[0][0:16, 0:nt], in_=p[0:16, 0:nt], func=AF.Copy), r=[Bp], w=[LR[1]])
        chk("a2")
        dk, dv, dlf = (nk_p, nv_p, nlf_p) if kind != "s" else (nk_s, nv_s, nlf_s)
        orow = tok0 if kind != "s" else idx * 8
        p, Bp = proj_tok(HT, nt, C_KF)
        KO = kout[k]
        S.op(ACT, lambda e: e.activation(out=KO[0][0:nt, :], in_=p[0:nt, :], func=AF.Copy), r=[Bp], w=[KO[1]])
        chk("b0")
        S.dma(QS, dk[orow:orow + nt, :], KO[0][0:nt, :], r=[KO[1]])
        chk("b1")
        p, Bp = proj_tok(HT, nt, C_VF)
        VO, V1 = vout[k], v1t[k]
        S.op(ACT, lambda e: e.activation(out=VO[0][0:nt, :], in_=p[0:nt, :], func=AF.Copy), r=[Bp], w=[VO[1]])
        S.op(DVE, lambda e: e.tensor_copy(out=V1[0][0:nt, :, 0:64],
                                          in_=p[0:nt, :].rearrange("p (h d) -> p h d", h=8)), r=[Bp], w=[V1[1]])
        chk("b2")
        S.dma(QS, dv[orow:orow + nt, :], VO[0][0:nt, :], r=[VO[1]])
        S.dma(QS, v1_scr[tok0:tok0 + nt, :], V1[0][0:nt, :, :].rearrange("p h d -> p (h d)"), r=[V1[1]])
        chk("b3")
        p, Bp = proj_tok(HT, nt, C_VG)
        VG = vgb[k]
        S.op(DVE, lambda e: e.tensor_copy(out=VG[0][0:nt, :], in_=p[0:nt, :]), r=[Bp], w=[VG[1]])
        chk("a3")
        p, Bp = proj_tok(HT, nt, C_RG)
        E0, RSL = etmp[0], rsil[k]
        sigmoid_from(p[0:nt, :], Bp, nt, 512, E0, E0[0][0:nt, 0:512], E0[1])
        S.op(DVE, lambda e: e.tensor_tensor(out=RSL[0][0:nt, :], in0=p[0:nt, :], in1=E0[0][0:nt, 0:512], op=ALU.mult),
             r=[Bp, E0[1]], w=[RSL[1]])
        S.op(POOL, lambda e: e.tensor_tensor(out=RSL[0][0:nt, :], in0=RSL[0][0:nt, :], in1=ggla_bc[0:nt, :],
                                             op=ALU.mult), r=[RSL[1], B_ggla], w=[RSL[1]])
        chk("a4")
        SGF, SGG = sgf[k], sgg[k]
        for (col0, SG, ei) in ((C_GF, SGF, 1), (C_GG, SGG, 2)):
            for half in range(2):
                p, Bp = proj_tok(HT, nt, col0 + half * 512)
                ET_ = etmp[ei]
                S.op(ACT, lambda e, p=p, ET_=ET_, half=half: e.activation(
                    out=ET_[0][0:nt, half * 512:(half + 1) * 512], in_=p[0:nt, :], func=AF.Exp, scale=-1.0),
                    r=[Bp], w=[ET_[1]])
            S.op(POOL, lambda e, ET_=ET_: e.tensor_scalar(out=ET_[0][0:nt, :], in0=ET_[0][0:nt, :], scalar1=1.0,
                                                          scalar2=None, op0=ALU.add), r=[ET_[1]], w=[ET_[1]])
            S.op(DVE, lambda e, ET_=ET_, SG=SG: e.reciprocal(out=SG[0][0:nt, :], in_=ET_[0][0:nt, :]),
                 r=[ET_[1]], w=[SG[1]])
        S.dma(QS, sgf_scr[tok0:tok0 + nt, :], SGF[0][0:nt, :], r=[SGF[1]])
        chk("a5")
        p, Bp = proj_tok(HT, nt, C_FF, 8)
        LF, LT, CT = lft[k], ltmp[k], ctile[k]
        S.op(DVE, lambda e: e.tensor_tensor(out=LT[0][0:nt, :], in0=p[0:nt, 0:8], in1=fb_bc[0:nt, :], op=ALU.add),
             r=[Bp, B_fb], w=[LT[1]])
        S.op(ACT, lambda e: e.activation(out=LT[0][0:nt, :], in_=LT[0][0:nt, :], func=AF.Exp, scale=-1.0),
             r=[LT[1]], w=[LT[1]])
        S.op(ACT, lambda e: e.activation(out=LT[0][0:nt, :], in_=LT[0][0:nt, :], func=AF.Ln, bias=one_c[0:nt, :]),
             r=[LT[1]], w=[LT[1]])
        S.op(DVE, lambda e: e.tensor_scalar(out=LF[0][0:nt, :], in0=LT[0][0:nt, :], scalar1=-1.0, scalar2=None,
                                            op0=ALU.mult), r=[LT[1]], w=[LF[1]])
        S.dma(QS, dlf[orow:orow + nt, :], LF[0][0:nt, :], r=[LF[1]])
        p, Bp = ps()
        S.op(PE, lambda e: e.matmul(out=p[0:nt, 0:8], lhsT=tri_f[0:nt, 0:nt], rhs=LF[0][0:nt, :], start=True, stop=True),
             r=[B_tri, LF[1]], w=[Bp])
        if kind == "s":
            S.op(DVE, lambda e: e.tensor_copy(out=CT[0][0:nt, :], in_=p[0:nt, 0:8]), r=[Bp], w=[CT[1]])
        else:
            S.op(DVE, lambda e: e.tensor_tensor(out=CT[0][0:nt, :], in0=p[0:nt, 0:8], in1=carry[0:nt, :], op=ALU.add),
                 r=[Bp, B_carry], w=[CT[1]])
            p2, Bp2 = ps()
            S.op(PE, lambda e: e.matmul(out=p2[:, 0:8], lhsT=ones_f[0:nt, :], rhs=LF[0][0:nt, :], start=True, stop=True),
                 r=[B_ones, LF[1]], w=[Bp2])
            S.op(DVE, lambda e: e.tensor_tensor(out=carry[:, :], in0=p2[:, 0:8], in1=carry[:, :], op=ALU.add),
                 r=[Bp2, B_carry], w=[B_carry])
        S.dma(QS, c_scr[tok0:tok0 + nt, :], CT[0][0:nt, :], r=[CT[1]])
        chk("a6")
        return

    def phase1a_gla(ti, kind, nt, idx, tok0):
        k = ti % 2
        HT, LR, VG, RSL, SGG = hT[k], lrgT[k], vgb[k], rsil[k], sgg[k]
        EA, CS, EB, ENB, QTL, KTL, KTK, AM = ea[k], csum[k], ebt[k], enbt[k], qtl[k], ktl[k], ktok[k], Am[k]
        p, Bp = ps()
        for c in range(2):
            S.op(PE, lambda e, c=c: e.matmul(out=p[:, c * 128:c * 128 + nt], lhsT=wa2[0:16, c * 128:(c + 1) * 128],
                                             rhs=LR[0][0:16, 0:nt], start=True, stop=True), r=[B_wa2, LR[1]], w=[Bp])
        for c in range(2):
            S.op(ACT, lambda e, c=c: e.activation(out=EA[0][:, c, 0:nt], in_=p[:, c * 128:c * 128 + nt], func=AF.Exp,
                                                  scale=-1.0, bias=nba[:, c:c + 1]), r=[Bp, B_nba], w=[EA[1]])
        S.op(ACT, lambda e: e.activation(out=EA[0][:, :, 0:nt], in_=EA[0][:, :, 0:nt], func=AF.Ln, bias=one_c[:, :]),
             r=[EA[1]], w=[EA[1]])
        for c in range(2):
            S.op(DVE, lambda e, c=c: e.tensor_tensor_scan(out=CS[0][:, c, 0:nt], data0=ones_f[:, 0:nt],
                                                          data1=EA[0][:, c, 0:nt], initial=0.0,
                                                          op0=ALU.mult, op1=ALU.add), r=[EA[1], B_ones], w=[CS[1]])
        S.op(ACT, lambda e: e.activation(out=EB[0][:, :, 0:nt], in_=CS[0][:, :, 0:nt], func=AF.Exp, scale=-1.0 / 16),
             r=[CS[1]], w=[EB[1]])
        S.op(ACT, lambda e: e.activation(out=ENB[0][:, :, 0:nt], in_=CS[0][:, :, 0:nt], func=AF.Exp, scale=1.0 / 16),
             r=[CS[1]], w=[ENB[1]])
        pqk, Bpqk = ps()
        for b in range(4):
            col0 = C_QG + b * 128
            for c in range(8):
                S.op(PE, lambda e, c=c, b=b, col0=col0: e.matmul(out=pqk[:, b * 128:b * 128 + nt],
                                                                 lhsT=win[:, c, col0:col0 + 128],
                                                                 rhs=HT[0][:, c, 0:nt], start=(c == 0), stop=(c == 7)),
                     r=[HT[1], B_win], w=[Bpqk])
        pq3 = pqk[:, :].rearrange("p (b t) -> p b t", b=4)
        for par in range(2):
            S.op(DVE, lambda e, par=par: e.scalar_tensor_tensor(out=QTL[0][:, par, :, 0:nt], in0=pq3[:, 0:2, 0:nt],
                                                                scalar=hm[par][0][:, 0:1], in1=EB[0][:, :, 0:nt],
                                                                op0=ALU.mult, op1=ALU.mult),
                 r=[Bpqk, EB[1], hm[par][1]], w=[QTL[1]])
        S.op(DVE, lambda e: e.tensor_tensor(out=KTL[0][:, :, 0:nt], in0=pq3[:, 2:4, 0:nt], in1=ENB[0][:, :, 0:nt],
                                            op=ALU.mult), r=[Bpqk, ENB[1]], w=[KTL[1]])
        p, Bp = ps()
        pv = bfv(p)
        for c in range(2):
            S.op(PE, lambda e, c=c: e.transpose(out=pv[0:nt, c * 128:(c + 1) * 128], in_=KTL[0][:, c, 0:nt],
                                                identity=id_b[:, :]), r=[KTL[1], B_idb], w=[Bp])
        S.op(ACT, lambda e: e.activation(out=KTK[0][0:nt, :], in_=pv[0:nt, 0:256], func=AF.Copy), r=[Bp], w=[KTK[1]])
        chk("a7")
        pa, Bpa = ps()
        for h in range(4):
            r0, c = (h % 2) * 64, h // 2
            S.op(PE, lambda e, h=h, c=c: e.matmul(out=pa[0:nt, h * 128:h * 128 + nt],
                                                  lhsT=KTL[0][:, c, 0:nt],
                                                  rhs=QTL[0][:, h % 2, c, 0:nt], start=True, stop=True),
                 r=[KTL[1], QTL[1]], w=[Bpa])
        S.op(DVE, lambda e: e.tensor_tensor(out=AM[0][0:nt, :, 0:nt],
                                            in0=pa[:, :].rearrange("p (h t) -> p h t", h=4)[0:nt, :, 0:nt],
                                            in1=tri_b[0:nt, 0:nt].unsqueeze(1).to_broadcast([nt, 4, nt]),
                                            op=ALU.mult), r=[Bpa, B_trib], w=[AM[1]])
        po_, Bpo = ps()
        for h in range(4):
            r0, c = (h % 2) * 64, h // 2
            S.op(PE, lambda e, h=h: e.matmul(out=po_[0:nt, h * 128:(h + 1) * 128], lhsT=AM[0][0:nt, h, 0:nt],
                                             rhs=VG[0][0:nt, h * 128:(h + 1) * 128], start=True, stop=False),
                 r=[AM[1], VG[1]], w=[Bpo])
            S.op(PE, lambda e, h=h, c=c: e.matmul(out=po_[0:nt, h * 128:(h + 1) * 128],
                                                  lhsT=QTL[0][:, h % 2, c, 0:nt],
                                                  rhs=Sb[:, c, :], start=False, stop=True),
                 r=[QTL[1], B_Sb], w=[Bpo])
        chk("a8")
        pd, Bpd = ps()
        for h in range(4):
            r0, c = (h % 2) * 64, h // 2
            S.op(PE, lambda e, h=h, r0=r0, c=c: e.matmul(out=pd[r0:r0 + 64, c * 128:(c + 1) * 128],
                                                         lhsT=KTK[0][0:nt, h * 64:(h + 1) * 64],
                                                         rhs=VG[0][0:nt, h * 128:(h + 1) * 128], start=True, stop=True),
                 r=[KTK[1], VG[1]], w=[Bpd])
        S.op(DVE, lambda e: e.tensor_tensor(out=Sst[:, :, :], in0=pd[:, 0:256].rearrange("p (c v) -> p c v", c=2),
                                            in1=Sst[:, :, :], op=ALU.add), r=[Bpd, B_S], w=[B_S])
        for c in range(2):
            S.op(DVE, lambda e, c=c: e.tensor_scalar(out=Sst[:, c, :], in0=Sst[:, c, :], scalar1=EB[0][:, c, nt - 1:nt],
                                                     scalar2=None, op0=ALU.mult), r=[B_S, EB[1]], w=[B_S])
        S.op(POOL, lambda e: e.tensor_copy(out=Sb[:, :, :], in_=Sst[:, :, :]), r=[B_S], w=[B_Sb])
        if DEBUG and ti == DEBUG_TILE:
            def dbg(name, t, B, shape, dt=F32):
                d = dout("dbg_" + name, shape, dt)
                S.dma(QS, d, t, r=[B])
            dbg("sp", EA[0][:, :, :], EA[1], [128, 2, 128])
            dbg("cs", CS[0][:, :, :], CS[1], [128, 2, 128])
            dbg("ktok", KTK[0][:, :], KTK[1], [128, 256], BF16)
            dbg("vgb", VG[0][:, :], VG[1], [128, 512], BF16)
            dbg("qtl", QTL[0][:, 0, :, :], QTL[1], [128, 2, 128], BF16)
            dbg("ktl", KTL[0][:, :, :], KTL[1], [128, 2, 128], BF16)
            dbg("am", AM[0][:, :, :], AM[1], [128, 4, 128], BF16)
            dbg("S", Sst[:, :, :], B_S, [128, 2, 128])
            dbg("lrg", LR[0][:, :], LR[1], [16, 128])
        chk("a9")
        SG_, RG_, O1, OGB, OGT, MG = ssg[k], rsg[k], o1[k], ogb[k], ogT[k], mgt[k]
        for h in range(4):
            S.op(ACT, lambda e, h=h: e.activation(out=junk[0:nt, 0:128], in_=po_[0:nt, h * 128:(h + 1) * 128],
                                                  func=AF.Square, accum_out=SG_[0][0:nt, h:h + 1]), r=[Bpo], w=[SG_[1]])
        S.op(ACT, lambda e: e.activation(out=RG_[0][0:nt, :], in_=SG_[0][0:nt, :], func=AF.Ln, scale=1.0 / 128,
                                         bias=eps_c[0:nt, :]), r=[SG_[1]], w=[RG_[1]])
        S.op(ACT, lambda e: e.activation(out=RG_[0][0:nt, :], in_=RG_[0][0:nt, :], func=AF.Exp, scale=-0.5),
             r=[RG_[1]], w=[RG_[1]])
        S.op(DVE, lambda e: e.tensor_tensor(out=O1[0][0:nt, :], in0=po_[0:nt, :], in1=RSL[0][0:nt, :], op=ALU.mult),
             r=[Bpo, RSL[1]], w=[O1[1]])
        S.op(POOL, lambda e: e.tensor_tensor(out=OGB[0][0:nt, :, :],
                                             in0=O1[0][0:nt, :].rearrange("p (h v) -> p h v", h=4),
                                             in1=RG_[0][0:nt, :].unsqueeze(2).to_broadcast([nt, 4, 128]),
                                             op=ALU.mult), r=[O1[1], RG_[1]], w=[OGB[1]])
        p, Bp = ps()
        pv = bfv(p)
        for c in range(4):
            S.op(PE, lambda e, c=c: e.transpose(out=pv[:, c * 128:c * 128 + nt], in_=OGB[0][0:nt, c, :],
                                                identity=id_b[0:nt, 0:nt]), r=[OGB[1], B_idb], w=[Bp])
        S.op(ACT, lambda e: e.activation(out=OGT[0][:, :, 0:nt],
                                         in_=pv[:, 0:512].rearrange("p (c t) -> p c t", c=4)[:, :, 0:nt], func=AF.Copy),
             r=[Bp], w=[OGT[1]])
        for half in range(2):
            p, Bp = ps()
            for c in range(4):
                S.op(PE, lambda e, c=c, p=p, half=half: e.matmul(out=p[0:nt, :], lhsT=OGT[0][:, c, 0:nt],
                                                                 rhs=wgo[:, c, half * 512:(half + 1) * 512],
                                                                 start=(c == 0), stop=(c == 3)),
                     r=[OGT[1], B_wgo], w=[Bp])
            S.op(DVE, lambda e, p=p, half=half: e.tensor_tensor(out=MG[0][0:nt, half * 512:(half + 1) * 512],
                                                                in0=p[0:nt, :],
                                                                in1=SGG[0][0:nt, half * 512:(half + 1) * 512],
                                                                op=ALU.mult), r=[Bp, SGG[1]], w=[MG[1]])
        S.dma(QS, mg_scr[tok0:tok0 + nt, :], MG[0][0:nt, :], r=[MG[1]])

    S.op(DVE, lambda e: e.memset(Sst[:, :, :], 0.0), w=[B_S])
    S.op(POOL, lambda e: e.memset(Sb[:, :, :], 0.0), w=[B_Sb])
    if STOP == "0w":
        S.barrier()
        return nc
    tiles1 = [("m", 16, 0, 0)] + [("p", 128, i, 16 + i * 128) for i in range(NPT)] + \
             [("s", 8, j, LTOK + j * 8) for j in range(NSS)]
    ngroups = NPT // 4

    def run_gla(n):
        kind, nt, idx, tok0 = tiles1[n]
        if kind == "s":
            S.dma(QS, Sst[:, :, :], sgl[idx].rearrange("(c t) k v -> (t k) c v", t=2), w=[B_S])
            S.op(POOL, lambda e: e.tensor_copy(out=Sb[:, :, :], in_=Sst[:, :, :]), r=[B_S], w=[B_Sb])
        phase1a_gla(n, kind, nt, idx, tok0)
        if kind == "p" and idx == NPT - 1:
            S.dma(QS, ngl_p.rearrange("(c t) k v -> (t k) c v", t=2), Sst[:, :, :], r=[B_S])
        if kind == "s":
            S.dma(QS, ngl_s[idx].rearrange("(c t) k v -> (t k) c v", t=2), Sst[:, :, :], r=[B_S])

    for n, (kind, nt, idx, tok0) in enumerate(tiles1):
        phase1a(n, kind, nt, idx, tok0)
        if kind == "m":
            S.op(DVE, lambda e: e.tensor_copy(out=cref[:, 0, :], in_=carry[:, :]), r=[B_carry], w=[B_cref])
        if kind == "p" and idx % 4 == 3 and idx // 4 + 1 < ngroups:
            g = idx // 4 + 1
            S.op(DVE, lambda e, g=g: e.tensor_copy(out=cref[:, g, :], in_=carry[:, :]), r=[B_carry], w=[B_cref])
        if n >= 1:
            run_gla(n - 1)
        if n == 0 and STOP == "1am":
            run_gla(0)
            S.barrier()
            return nc
    run_gla(len(tiles1) - 1)

    S.barrier()
    if STOP == "1a":
        return nc
    A.cur = persist_mark
    rot["n"] = 6
    wfo, B_wfo = T([128, 4, D], BF16, "wfo")
    wo, B_wo = T([128, 8, D], BF16, "wo")
    for c in range(4):
        S.dma(QP, wfo[:, c, :], w_fo[c * 128:(c + 1) * 128, :], w=[B_wfo])
    for c in range(8):
        S.dma(QP, wo[:, c, :], w_o[c * 128:(c + 1) * 128, :], w=[B_wo])
    sgfl = T2([128, D], BF16, "sgfl")
    mgl = T2([128, D], BF16, "mgl")
    xl = T2([128, D], F32, "xl")
    m1 = T2([128, D], F32, "m1")
    mrg = T2([128, D], BF16, "mrg")
    mT = T2([128, 8, 128], BF16, "mT")
    x1 = T2([128, D], F32, "x1")
    ss2 = T2([128, 1], F32, "ss2")
    rs2 = T2([128, 1], F32, "rs2")
    h2 = T2([128, D], F32, "h2")
    h2T32 = T2([128, 8, 128], F32, "h2T32")
    h2Tb = T2([128, 8, 128], BF16, "h2Tb")
    lg = T2([128, 36], F32, "lg")
    gsm = T2([128, 16], F32, "gsm")
    em = T2([128, 32], F32, "em")
    top8 = T2([128, 8], F32, "top8")
    gt1 = T2([128, 32], F32, "gt1")
    gts = T2([128, 32], F32, "gts")
    attn_mark = A.cur
    KT, B_KT = T([128, 4, LTOK], BF16, "KT")
    V1a, B_V1 = T([128, NJ, 520], BF16, "V1a")
    call, B_call = T([128, NJ, 8], F32, "call")
    bias_all, B_bias = T([128, NJ, 8], F32, "biasall")
    QTg, B_QTg = T([128, 4, 512], BF16, "QTg")
    PT = T2([128, 512], BF16, "PT", 3)
    ofn, B_ofn = T([128, 4, 512], BF16, "ofn")
    rden = T2([128, 4], F32, "rden")
    ofT = T2([128, 4, 128], BF16, "ofT")
    S.op(DVE, lambda e: e.memset(call[:, :, :], 0.0), w=[B_call])
    mcount = {"i": 0}

    def merge_tile(OFT, nt, tok0, xsrc):
        k = mcount["i"] % 2
        mcount["i"] += 1
        SGL, MGL, XL, M1, MR, MT_, X1, SS2, RS2, H2, H32, H2B, LG, GS, EM, T8, G1, GT = (
            sgfl[k], mgl[k], xl[k], m1[k], mrg[k], mT[k], x1[k], ss2[k], rs2[k], h2[k], h2T32[k], h2Tb[k],
            lg[k], gsm[k], em[k], top8[k], gt1[k], gts[k])
        S.dma(QS, SGL[0][0:nt, :], sgf_scr[tok0:tok0 + nt, :], w=[SGL[1]])
        S.dma(QS, MGL[0][0:nt, :], mg_scr[tok0:tok0 + nt, :], w=[MGL[1]])
        S.dma(QS, XL[0][0:nt, :], xsrc, w=[XL[1]])
        for half in range(2):
            p, Bp = ps()
            for c in range(4):
                S.op(PE, lambda e, c=c, p=p, half=half: e.matmul(out=p[0:nt, :], lhsT=OFT[0][:, c, 0:nt],
                                                                 rhs=wfo[:, c, half * 512:(half + 1) * 512],
                                                                 start=(c == 0), stop=(c == 3)),
                     r=[OFT[1], B_wfo], w=[Bp])
            S.op(DVE, lambda e, p=p, half=half: e.tensor_tensor(out=M1[0][0:nt, half * 512:(half + 1) * 512],
                                                                in0=p[0:nt, :],
                                                                in1=SGL[0][0:nt, half * 512:(half + 1) * 512],
                                                                op=ALU.mult), r=[Bp, SGL[1]], w=[M1[1]])
        S.op(POOL, lambda e: e.tensor_tensor(out=MR[0][0:nt, :], in0=M1[0][0:nt, :], in1=MGL[0][0:nt, :], op=ALU.add),
             r=[M1[1], MGL[1]], w=[MR[1]])
        p, Bp = ps()
        pv = bfv(p)
        for c in range(8):
            S.op(PE, lambda e, c=c: e.transpose(out=pv[:, c * 128:c * 128 + nt], in_=MR[0][0:nt, c * 128:(c + 1) * 128],
                                                identity=id_b[0:nt, 0:nt]), r=[MR[1], B_idb], w=[Bp])
        S.op(ACT, lambda e: e.activation(out=MT_[0][:, :, 0:nt],
                                         in_=pv.rearrange("p (c t) -> p c t", c=8)[:, :, 0:nt], func=AF.Copy),
             r=[Bp], w=[MT_[1]])
        for half in range(2):
            p, Bp = ps()
            for c in range(8):
                S.op(PE, lambda e, c=c, p=p, half=half: e.matmul(out=p[0:nt, :], lhsT=MT_[0][:, c, 0:nt],
                                                                 rhs=wo[:, c, half * 512:(half + 1) * 512],
                                                                 start=(c == 0), stop=(c == 7)),
                     r=[MT_[1], B_wo], w=[Bp])
            S.op(DVE, lambda e, p=p, half=half: e.tensor_tensor(out=X1[0][0:nt, half * 512:(half + 1) * 512],
                                                                in0=p[0:nt, :],
                                                                in1=XL[0][0:nt, half * 512:(half + 1) * 512],
                                                                op=ALU.add), r=[Bp, XL[1]], w=[X1[1]])
        S.dma(QS, x1_scr[tok0:tok0 + nt, :], X1[0][0:nt, :], r=[X1[1]])
        rmsnorm_stats(X1[0][0:nt, :], X1[1], nt, SS2, RS2, D)
        S.op(DVE, lambda e: e.scalar_tensor_tensor(out=H2[0][0:nt, :], in0=X1[0][0:nt, :], scalar=RS2[0][0:nt, :],
                                                   in1=gffn_bc[0:nt, :], op0=ALU.mult, op1=ALU.mult),
             r=[X1[1], RS2[1], B_gffn], w=[H2[1]])
        for half in range(2):
            p, Bp = ps()
            for c in range(4):
                cc = half * 4 + c
                S.op(PE, lambda e, c=c, cc=cc, p=p: e.transpose(out=p[:, c * 128:c * 128 + nt],
                                                                in_=H2[0][0:nt, cc * 128:(cc + 1) * 128],
                                                                identity=id_f[0:nt, 0:nt]), r=[H2[1], B_idf], w=[Bp])
            S.op(ACT, lambda e, p=p, half=half: e.activation(
                out=H32[0][:, half * 4:(half + 1) * 4, 0:nt],
                in_=p[:, :].rearrange("p (c t) -> p c t", c=4)[:, :, 0:nt], func=AF.Copy), r=[Bp], w=[H32[1]])
        S.op(POOL, lambda e: e.tensor_copy(out=H2B[0][:, :, 0:nt], in_=H32[0][:, :, 0:nt]), r=[H32[1]], w=[H2B[1]])
        S.dma(QS, h2T_scr[:, :, tok0:tok0 + nt], H2B[0][:, :, 0:nt], r=[H2B[1]])
        p, Bp = ps()
        for c in range(8):
            S.op(PE, lambda e, c=c: e.matmul(out=p[0:nt, 0:36], lhsT=H32[0][:, c, 0:nt], rhs=wr[:, c, :],
                                             start=(c == 0), stop=(c == 7)), r=[H32[1], B_wr], w=[Bp])
        S.op(DVE, lambda e: e.tensor_tensor(out=LG[0][0:nt, :], in0=p[0:nt, 0:36], in1=rb_bc[0:nt, :], op=ALU.add),
             r=[Bp, B_rb], w=[LG[1]])
        g = GS[0]
        S.op(DVE, lambda e: e.tensor_reduce(out=g[0:nt, 0:1], in_=LG[0][0:nt, 0:4], axis=AX.X, op=ALU.max),
             r=[LG[1]], w=[GS[1]])
        S.op(DVE, lambda e: e.tensor_scalar(out=g[0:nt, 1:2], in0=g[0:nt, 0:1], scalar1=-1.0, scalar2=None, op0=ALU.mult),
             r=[GS[1]], w=[GS[1]])
        S.op(DVE, lambda e: e.tensor_scalar(out=g[0:nt, 8:12], in0=LG[0][0:nt, 0:4], scalar1=g[0:nt, 0:1], scalar2=None,
                                            op0=ALU.is_equal), r=[LG[1], GS[1]], w=[GS[1]])
        S.op(DVE, lambda e: e.tensor_scalar(out=g[0:nt, 12:16], in0=g[0:nt, 8:12], scalar1=-1.0, scalar2=1e30,
                                            op0=ALU.add, op1=ALU.mult), r=[GS[1]], w=[GS[1]])
        S.op(ACT, lambda e: e.activation(out=junk[0:nt, 0:4], in_=LG[0][0:nt, 0:4], func=AF.Exp, bias=g[0:nt, 1:2],
                                         accum_out=g[0:nt, 2:3]), r=[LG[1], GS[1]], w=[GS[1]])
        S.op(DVE, lambda e: e.reciprocal(out=g[0:nt, 3:4], in_=g[0:nt, 2:3]), r=[GS[1]], w=[GS[1]])
        S.op(DVE, lambda e: e.tensor_tensor(out=EM[0][0:nt, :].rearrange("p (g k) -> p g k", g=4),
                                            in0=LG[0][0:nt, 4:36].rearrange("p (g k) -> p g k", g=4),
                                            in1=g[0:nt, 12:16].unsqueeze(2).to_broadcast([nt, 4, 8]), op=ALU.add),
             r=[LG[1], GS[1]], w=[EM[1]])
        S.op(DVE, lambda e: e.max(out=T8[0][0:nt, :], in_=EM[0][0:nt, :]), r=[EM[1]], w=[T8[1]])
        S.op(DVE, lambda e: e.tensor_tensor(out=g[0:nt, 4:5], in0=T8[0][0:nt, 1:2], in1=T8[0][0:nt, 0:1], op=ALU.subtract),
             r=[T8[1], GS[1]], w=[GS[1]])
        S.op(ACT, lambda e: e.activation(out=g[0:nt, 5:6], in_=g[0:nt, 4:5], func=AF.Exp), r=[GS[1]], w=[GS[1]])
        S.op(DVE, lambda e: e.tensor_scalar(out=g[0:nt, 5:6], in0=g[0:nt, 5:6], scalar1=1.0, scalar2=None, op0=ALU.add),
             r=[GS[1]], w=[GS[1]])
        S.op(DVE, lambda e: e.reciprocal(out=g[0:nt, 5:6], in_=g[0:nt, 5:6]), r=[GS[1]], w=[GS[1]])
        S.op(DVE, lambda e: e.tensor_tensor(out=g[0:nt, 5:6], in0=g[0:nt, 5:6], in1=g[0:nt, 3:4], op=ALU.mult),
             r=[GS[1]], w=[GS[1]])
        S.op(DVE, lambda e: e.tensor_tensor(out=g[0:nt, 6:7], in0=g[0:nt, 3:4], in1=g[0:nt, 5:6], op=ALU.subtract),
             r=[GS[1]], w=[GS[1]])
        S.op(DVE, lambda e: e.tensor_scalar(out=G1[0][0:nt, :], in0=EM[0][0:nt, :], scalar1=T8[0][0:nt, 0:1],
                                            scalar2=g[0:nt, 5:6], op0=ALU.is_equal, op1=ALU.mult),
             r=[EM[1], T8[1], GS[1]], w=[G1[1]])
        S.op(DVE, lambda e: e.tensor_scalar(out=GT[0][0:nt, :], in0=EM[0][0:nt, :], scalar1=T8[0][0:nt, 1:2],
                                            scalar2=g[0:nt, 6:7], op0=ALU.is_equal, op1=ALU.mult),
             r=[EM[1], T8[1], GS[1]], w=[GT[1]])
        S.op(DVE, lambda e: e.tensor_tensor(out=GT[0][0:nt, :], in0=GT[0][0:nt, :], in1=G1[0][0:nt, :], op=ALU.add),
             r=[GT[1], G1[1]], w=[GT[1]])
        S.dma(QS, gat_scr[tok0:tok0 + nt, :], GT[0][0:nt, :], r=[GT[1]])

    po_banks = [(psb[6], psB[6]), (psb[7], psB[7])]

    def attention_group(gi, tiles, tok0):
        nts = [16 if j == 0 else 128 for j in tiles]
        nq = sum(nts)
        j0 = tiles[0]
        jlast = tiles[-1]
        ntq = len(tiles)
        S.dma(QS, KT[:, :, tok0:tok0 + nq], kT_scr[:, :, tok0:tok0 + nq], w=[B_KT])
        for qi, j in enumerate(tiles):
            t0 = tok0 + sum(nts[:qi])
            S.dma(QS, V1a[0:nts[qi], j, :], v1_scr[t0:t0 + nts[qi], :], w=[B_V1])
            S.dma(QS, call[0:nts[qi], j, :], c_scr[t0:t0 + nts[qi], :], w=[B_call])
        S.dma(QS, QTg[:, :, 0:nq], qT_scr[:, :, tok0:tok0 + nq], w=[B_QTg])
        nj = jlast + 1
        if j0 == 0:
            S.op(DVE, lambda e: e.tensor_scalar(out=bias_all[:, 0:1, :], in0=call[:, 0:1, :], scalar1=-1.0,
                                                scalar2=None, op0=ALU.mult), r=[B_call], w=[B_bias])
        else:
            S.op(DVE, lambda e: e.tensor_tensor(out=bias_all[:, 0:nj, :],
                                                in0=cref[:, gi:gi + 1, :].to_broadcast([128, nj, 8]),
                                                in1=call[:, 0:nj, :], op=ALU.subtract),
                 r=[B_cref, B_call], w=[B_bias])
        for h in range(8):
            r0, c = (h % 2) * 64, h // 2
            po, Bpo = po_banks[h % 2]
            po3 = po[:, 0:260].rearrange("p (q d) -> p q d", q=4)
            S.op(PE, lambda e, po=po: e.matmul(out=po[:, 0:260], lhsT=zer_b[0:1, 0:128], rhs=zer_b[0:1, 0:260],
                                               start=True, stop=True), r=[B_zer], w=[Bpo])
            for j in range(nj):
                nk = 16 if j == 0 else 128
                kt0 = 0 if j == 0 else 16 + (j - 1) * 128
                m = j - j0
                col0 = max(m, 0) * 128 if j0 > 0 else 0
                ncol = nq - col0
                p, Bp = ps()
                S.op(PE, lambda e, p=p, nk=nk, kt0=kt0, col0=col0, ncol=ncol: e.matmul(
                    out=p[0:nk, 0:ncol], lhsT=KT[r0:r0 + 64, c, kt0:kt0 + nk], rhs=QTg[r0:r0 + 64, c, col0:col0 + ncol],
                    start=True, stop=True), r=[B_KT, B_QTg], w=[Bp])
                P_ = PT[(h * 64 + j) % 3]
                S.op(ACT, lambda e, p=p, P_=P_, nk=nk, ncol=ncol, j=j: e.activation(
                    out=P_[0][0:nk, 0:ncol], in_=p[0:nk, 0:ncol], func=AF.Exp, scale=0.125,
                    bias=bias_all[0:nk, j, h:h + 1]), r=[Bp, B_bias], w=[P_[1]])
                if m >= 0:
                    nd = nts[m]
                    S.op(POOL, lambda e, P_=P_, nk=nk, nd=nd: e.tensor_tensor(
                        out=P_[0][0:nk, 0:nd], in0=P_[0][0:nk, 0:nd], in1=tri_b[0:nk, 0:nd], op=ALU.mult),
                        r=[P_[1], B_trib], w=[P_[1]])
                for qt in range(max(m, 0), ntq):
                    qc0 = sum(nts[:qt]) - col0
                    nqt = nts[qt]
                    S.op(PE, lambda e, P_=P_, nk=nk, qc0=qc0, nqt=nqt, qt=qt, j=j: e.matmul(
                        out=po3[0:nqt, qt, :], lhsT=P_[0][0:nk, qc0:qc0 + nqt], rhs=V1a[0:nk, j, h * 65:(h + 1) * 65],
                        start=False, stop=True, skip_group_check=True), r=[P_[1], B_V1], w=[Bpo])
            RD = rden[h % 2]
            nqt = nts[0]
            S.op(DVE, lambda e, po3=po3, RD=RD: e.reciprocal(out=RD[0][0:nqt, 0:ntq], in_=po3[0:nqt, 0:ntq, 64]),
                 r=[Bpo], w=[RD[1]])
            S.op(DVE, lambda e, po3=po3, RD=RD, h=h: e.tensor_tensor(
                out=ofn[0:nqt, 0:ntq, h * 64:(h + 1) * 64], in0=po3[0:nqt, 0:ntq, 0:64],
                in1=RD[0][0:nqt, 0:ntq].unsqueeze(2).to_broadcast([nqt, ntq, 64]), op=ALU.mult),
                r=[Bpo, RD[1]], w=[B_ofn])
        for qi, j in enumerate(tiles):
            nt = nts[qi]
            t0 = tok0 + sum(nts[:qi])
            OF = ofT[qi % 2]
            p, Bp = ps()
            pv = bfv(p)
            for c in range(4):
                S.op(PE, lambda e, c=c, qi=qi: e.transpose(out=pv[:, c * 128:c * 128 + nt],
                                                           in_=ofn[0:nt, qi, c * 128:(c + 1) * 128],
                                                           identity=id_b[0:nt, 0:nt]), r=[B_ofn, B_idb], w=[Bp])
            S.op(ACT, lambda e, OF=OF: e.activation(out=OF[0][:, :, 0:nt],
                                                    in_=pv[:, 0:512].rearrange("p (c t) -> p c t", c=4)[:, :, 0:nt],
                                                    func=AF.Copy), r=[Bp], w=[OF[1]])
            xsrc = meta if j == 0 else xp[(j - 1) * 128:j * 128, :]
            merge_tile(OF, nt, t0, xsrc)

    attention_group(0, [0], 0)
    for g in range(ngroups):
        attention_group(g, [1 + 4 * g + i for i in range(4)], 16 + 512 * g)

    S.barrier()
    if STOP == "1b":
        return nc
    A.cur = attn_mark
    NPGp = NPG
    idxc = T2([128, 1], I32, "idxc")
    lfpg, B_lfpg = T([128, 1024], F32, "lfpg")
    lfT, B_lfT = T([128, 8, NPGp], F32, "lfT")
    pre, B_pre = T([128, 8, NPGp], F32, "pre")
    tot_s, B_tot = T([128, 8, NPGp], F32, "tots")
    wexp, B_wexp = T([128, 8, NPGp], F32, "wexp")
    QTs, B_QTs = T([128, 4, 8], BF16, "QTs")
    KTs, B_KTs = T([128, 4, 8], BF16, "KTs")
    Qbd, B_Qbd = T([128, 4, 16], BF16, "Qbd")
    Vs, B_Vs = T([8, 520], BF16, "Vs")
    cs_s, B_css = T([8, 8], F32, "css")
    wnew, B_wnew = T([8, 8], F32, "wnew")
    kpg = T2([128, 512], F32, "kpg", 3)
    vpg = T2([128, 512], F32, "vpg", 3)
    kTp = T2([128, 4, 128], BF16, "kTp")
    vbp = T2([128, 512], BF16, "vbp")
    Pf = T2([128, 64], F32, "Pf")
    Pw = T2([128, 64], BF16, "Pw")
    Ofn_s, B_Ofn = T([64, 512], BF16, "Ofns")
    rd_s, B_rds = T([64, 1], F32, "rds")
    ofTs, B_ofTs = T([128, 4, 8], BF16, "ofTs")
    Of_b, B_Of = psb[6], psB[6]
    dn_b, B_dn = psb[7], psB[7]

    def sample_seq(sj):
        tok0 = LTOK + sj * 8
        S.dma(QS, QTs[:, :, :], qT_scr[:, :, tok0:tok0 + 8], w=[B_QTs])
        S.dma(QS, KTs[:, :, :], kT_scr[:, :, tok0:tok0 + 8], w=[B_KTs])
        S.dma(QS, Vs[:, :], v1_scr[tok0:tok0 + 8, :], w=[B_Vs])
        S.dma(QS, cs_s[:, :], c_scr[tok0:tok0 + 8, :], w=[B_css])
        IX = idxc[sj % 2]
        S.dma(QS, IX[0][0:NPG, :], ptcol[sj * NPG:(sj + 1) * NPG, :], w=[IX[1]])
        S.op(POOL, lambda e: e.memset(Qbd[:, :, :], 0.0), w=[B_Qbd])
        S.op(POOL, lambda e: e.tensor_copy(out=Qbd[0:64, :, 0:8], in_=QTs[0:64, :, :]), r=[B_QTs], w=[B_Qbd])
        S.op(POOL, lambda e: e.tensor_copy(out=Qbd[64:128, :, 8:16], in_=QTs[64:128, :, :]), r=[B_QTs], w=[B_Qbd])
        S.op(ACT, lambda e: e.activation(out=wnew[:, :], in_=cs_s[:, :], func=AF.Exp, scale=-1.0), r=[B_css], w=[B_wnew])
        S.idma(lfpg[0:NPG, :], clf, IX[0][0:NPG, 0:1], r=[IX[1]], w=[B_lfpg])
        lf3 = lfpg[:, :].rearrange("p (s h) -> p s h", h=8)
        for half in range(2):
            p, Bp = ps()
            for hh in range(4):
                h = half * 4 + hh
                S.op(PE, lambda e, h=h, hh=hh, p=p: e.transpose(out=p[:, hh * NPG:(hh + 1) * NPG], in_=lf3[0:NPG, :, h],
                                                                identity=id_f[0:NPG, 0:NPG]), r=[B_lfpg, B_idf], w=[Bp])
            S.op(ACT, lambda e, p=p, half=half: e.activation(
                out=lfT[:, half * 4:(half + 1) * 4, :], in_=p[:, 0:4 * NPG].rearrange("p (h g) -> p h g", h=4),
                func=AF.Copy), r=[Bp], w=[B_lfT])
        lfTf = lfT[:, :, :].rearrange("p h g -> p (h g)")
        nn = 8 * NPG
        pw_, Bpw = [], []
        pt_, Bpt = [], []
        for c0 in range(0, nn, 512):
            c1 = min(nn, c0 + 512)
            p, Bp = ps()
            S.op(PE, lambda e, p=p, c0=c0, c1=c1: e.matmul(out=p[:, 0:c1 - c0], lhsT=triR_f[:, :], rhs=lfTf[:, c0:c1],
                                                           start=True, stop=True), r=[B_triR, B_lfT], w=[Bp])
            p2, Bp2 = ps()
            S.op(PE, lambda e, p2=p2, c0=c0, c1=c1: e.matmul(out=p2[:, 0:c1 - c0], lhsT=ones_f[:, :], rhs=lfTf[:, c0:c1],
                                                             start=True, stop=True), r=[B_ones, B_lfT], w=[Bp2])
            totf = tot_s[:, :, :].rearrange("p h g -> p (h g)")
            S.op(DVE, lambda e, p2=p2, c0=c0, c1=c1, totf=totf: e.tensor_copy(out=totf[:, c0:c1], in_=p2[:, 0:c1 - c0]),
                 r=[Bp2], w=[B_tot])
            pw_.append((p, Bp, c0, c1))
        for h in range(8):
            S.op(DVE, lambda e, h=h: e.tensor_tensor_scan(out=pre[:, h, :], data0=ones_f[:, 0:NPG], data1=tot_s[:, h, :],
                                                          initial=0.0, op0=ALU.mult, op1=ALU.add),
                 r=[B_tot, B_ones], w=[B_pre])
        S.op(DVE, lambda e: e.tensor_tensor(out=tot_s[:, :, :], in0=pre[:, :, :],
                                            in1=pre[:, :, NPG - 1:NPG].to_broadcast([128, 8, NPG]), op=ALU.subtract),
             r=[B_pre], w=[B_tot])
        wexf = wexp[:, :, :].rearrange("p h g -> p (h g)")
        pref = tot_s[:, :, :].rearrange("p h g -> p (h g)")
        for (p, Bp, c0, c1) in pw_:
            S.op(DVE, lambda e, p=p, c0=c0, c1=c1: e.tensor_tensor(out=wexf[:, c0:c1], in0=p[:, 0:c1 - c0],
                                                                   in1=pref[:, c0:c1], op=ALU.subtract),
                 r=[Bp, B_tot], w=[B_wexp])
        S.op(ACT, lambda e: e.activation(out=wexf[:, :], in_=wexf[:, :], func=AF.Exp), r=[B_wexp], w=[B_wexp])
        S.op(PE, lambda e: e.matmul(out=Of_b[0:64, :], lhsT=zer_b[0:1, 0:64], rhs=zer_b[0:1, 0:512],
                                    start=True, stop=True), r=[B_zer], w=[B_Of])
        S.op(PE, lambda e: e.matmul(out=dn_b[0:64, 0:1], lhsT=zer_b[0:1, 0:64], rhs=zer_b[0:1, 0:1],
                                    start=True, stop=True), r=[B_zer], w=[B_dn])
        for pg in range(NPG):
            KP, VP, KTP, VB, PF, PW_ = kpg[pg % 3], vpg[pg % 3], kTp[pg % 2], vbp[pg % 2], Pf[pg % 2], Pw[pg % 2]
            col = sj * NPG + pg
            S.idma(KP[0][:, :], ck_rows, idx_all[:, col:col + 1], r=[B_idx], w=[KP[1]])
            S.idma(VP[0][:, :], cv_rows, idx_all[:, col:col + 1], r=[B_idx], w=[VP[1]])
            p, Bp = ps()
            for c in range(4):
                S.op(PE, lambda e, c=c, p=p: e.transpose(out=p[:, c * 128:(c + 1) * 128], in_=KP[0][:, c * 128:(c + 1) * 128],
                                                         identity=id_f[:, :]), r=[KP[1], B_idf], w=[Bp])
            S.op(ACT, lambda e, p=p: e.activation(out=KTP[0][:, :, :], in_=p[:, :].rearrange("p (c t) -> p c t", c=4),
                                                  func=AF.Copy), r=[Bp], w=[KTP[1]])
            S.op(DVE, lambda e: e.tensor_copy(out=VB[0][:, :], in_=VP[0][:, :]), r=[VP[1]], w=[VB[1]])
            p, Bp = ps()
            for c in range(4):
                S.op(PE, lambda e, c=c, p=p: e.matmul(out=p[:, c * 16:(c + 1) * 16], lhsT=KTP[0][:, c, :], rhs=Qbd[:, c, :],
                                                      start=True, stop=True), r=[KTP[1], B_Qbd], w=[Bp])
            S.op(ACT, lambda e, p=p: e.activation(out=PF[0][:, :], in_=p[:, 0:64], func=AF.Exp, scale=0.125),
                 r=[Bp], w=[PF[1]])
            S.op(DVE, lambda e, pg=pg: e.tensor_tensor(out=PW_[0][:, :].rearrange("p (h q) -> p h q", h=8),
                                                       in0=PF[0][:, :].rearrange("p (h q) -> p h q", h=8),
                                                       in1=wexp[:, :, pg:pg + 1].to_broadcast([128, 8, 8]), op=ALU.mult),
                 r=[PF[1], B_wexp], w=[PW_[1]])
            S.op(PE, lambda e: e.matmul(out=Of_b[0:64, :], lhsT=PW_[0][:, :], rhs=VB[0][:, :], start=False, stop=True,
                                        skip_group_check=True), r=[PW_[1], VB[1]], w=[B_Of])
            S.op(PE, lambda e: e.matmul(out=dn_b[0:64, 0:1], lhsT=PW_[0][:, :], rhs=ones_b[:, 0:1], start=False, stop=True,
                                        skip_group_check=True), r=[PW_[1], B_onesb], w=[B_dn])
        PF, PW_ = Pf[0], Pw[0]
        p, Bp = ps()
        for c in range(4):
            S.op(PE, lambda e, c=c, p=p: e.matmul(out=p[0:8, c * 16:(c + 1) * 16], lhsT=KTs[:, c, :], rhs=Qbd[:, c, :],
                                                  start=True, stop=True), r=[B_KTs, B_Qbd], w=[Bp])
        S.op(ACT, lambda e: e.activation(out=PF[0][0:8, :], in_=p[0:8, 0:64], func=AF.Exp, scale=0.125), r=[Bp], w=[PF[1]])
        S.op(DVE, lambda e: e.tensor_tensor(out=PF[0][0:8, :].rearrange("p (h q) -> p h q", h=8),
                                            in0=PF[0][0:8, :].rearrange("p (h q) -> p h q", h=8),
                                            in1=wnew[:, :].unsqueeze(2).to_broadcast([8, 8, 8]), op=ALU.mult),
             r=[PF[1], B_wnew], w=[PF[1]])
        S.op(DVE, lambda e: e.tensor_tensor(out=PW_[0][0:8, :].rearrange("p (h q) -> p h q", h=8),
                                            in0=PF[0][0:8, :].rearrange("p (h q) -> p h q", h=8),
                                            in1=tri_f[0:8, 0:8].unsqueeze(1).to_broadcast([8, 8, 8]), op=ALU.mult),
             r=[PF[1], B_tri], w=[PW_[1]])
        Vs3 = Vs[:, :].rearrange("p (h d) -> p h d", h=8)
        for h in range(8):
            S.op(PE, lambda e, h=h: e.matmul(out=Of_b[0:64, h * 64:(h + 1) * 64], lhsT=PW_[0][0:8, :], rhs=Vs3[:, h, 0:64],
                                             start=False, stop=True, skip_group_check=True), r=[PW_[1], B_Vs], w=[B_Of])
        S.op(PE, lambda e: e.matmul(out=dn_b[0:64, 0:1], lhsT=PW_[0][0:8, :], rhs=ones_b[0:8, 0:1], start=False, stop=True,
                                    skip_group_check=True), r=[PW_[1], B_onesb], w=[B_dn])
        S.op(DVE, lambda e: e.reciprocal(out=rd_s[:, :], in_=dn_b[0:64, 0:1]), r=[B_dn], w=[B_rds])
        S.op(DVE, lambda e: e.tensor_scalar(out=Ofn_s[:, :], in0=Of_b[0:64, :], scalar1=rd_s[:, 0:1], scalar2=None,
                                            op0=ALU.mult), r=[B_Of, B_rds], w=[B_Ofn])
        p, Bp = ps()
        for h in range(8):
            r0, c = (h % 2) * 64, h // 2
            S.op(PE, lambda e, h=h, r0=r0, c=c, p=p: e.matmul(out=p[r0:r0 + 64, c * 8:(c + 1) * 8],
                                                              lhsT=Ofn_s[0:64, h * 64:(h + 1) * 64],
                                                              rhs=id_b[0:64, h * 8:(h + 1) * 8], start=True, stop=True),
                 r=[B_Ofn, B_idb], w=[Bp])
        S.op(ACT, lambda e: e.activation(out=ofTs[:, :, :], in_=p[:, 0:32].rearrange("p (c t) -> p c t", c=4),
                                         func=AF.Copy), r=[Bp], w=[B_ofTs])
        merge_tile((ofTs, B_ofTs), 8, tok0, xs[sj * 8:(sj + 1) * 8, :])

    for sj in range(NSS):
        sample_seq(sj)

    S.barrier()
    if STOP == "1s":
        return nc
    A.cur = SB_BASE
    rot["n"] = 8
    gfin_bc, B_gfin = T([128, D], F32, "gfin")
    S.dma(QS, gfin_bc[:, :], g_fin.partition_broadcast(128), w=[B_gfin])
    eps2, B_eps2 = T([128, 1], F32, "eps2")
    S.op(DVE, lambda e: e.memset(eps2[:, :], EPS), w=[B_eps2])
    junk2 = A.t([128, 1024], BF16, "junk2")
    ntile2 = (TOK + 127) // 128
    nsb = max(1, ntile2 // 8)
    sb_tiles = [list(range(i * 8, (i + 1) * 8 if i < nsb - 1 else ntile2)) for i in range(nsb)]
    maxt = max(len(x) for x in sb_tiles)
    yacc, B_y = T([128, maxt, D], F32, "yacc")
    gat, B_gat = T([128, maxt, 32], F32, "gat")
    h2s, B_h2s = T([128, 8, maxt * 128], BF16, "h2s")
    NWB = 3
    wgs = T2([128, 8, 256], BF16, "wgs", NWB)
    wus = T2([128, 8, 256], BF16, "wus", NWB)
    wds = T2([128, 2, D], BF16, "wds", NWB)
    ytmp = T2([128, D], F32, "ytmp")

    def load_expert(gidx):
        ex_ = gidx % NE
        k_ = gidx % NWB
        S.dma(QP, wgs[k_][0][:, :, :], w_eg[ex_].rearrange("(c p) f -> p c f", p=128), w=[wgs[k_][1]])
        S.dma(QP, wus[k_][0][:, :, :], w_eu[ex_].rearrange("(c p) f -> p c f", p=128), w=[wus[k_][1]])
        S.dma(QP, wds[k_][0][:, :, :], w_ed[ex_].rearrange("(c p) f -> p c f", p=128), w=[wds[k_][1]])

    n_exp_total = NE * len(sb_tiles)
    load_expert(0)
    if n_exp_total > 1:
        load_expert(1)
    eg = T2([128, 2, 512], F32, "eg")
    sgs = T2([128, 2, 512], F32, "sgs")
    heT = T2([128, 2, 512], BF16, "heT")
    yo = T2([128, D], F32, "yo")
    ssf = T2([128, 1], F32, "ssf")
    rsf = T2([128, 1], F32, "rsf")
    B_ytile = [Buf(f"y{i}") for i in range(maxt)]
    ecount = 0
    for tl in sb_tiles:
        t0 = tl[0] * 128
        t1 = min(TOK, (tl[-1] + 1) * 128)
        ntk = t1 - t0
        for li, ti2 in enumerate(tl):
            a0 = ti2 * 128
            n = min(128, TOK - a0)
            S.dma(QS, yacc[0:n, li, :], x1_scr[a0:a0 + n, :], w=[B_ytile[li]])
            S.dma(QS, gat[0:n, li, :], gat_scr[a0:a0 + n, :], w=[B_gat])
        S.dma(QS, h2s[:, :, 0:ntk], h2T_scr[:, :, t0:t1], w=[B_h2s])
        for ex in range(NE):
            k = ecount % NWB
            if ecount + 2 < n_exp_total:
                load_expert(ecount + 2)
            ecount += 1
            WG, WU, WD = wgs[k], wus[k], wds[k]
            for n0 in range(0, ntk, 512):
                nn_ = min(512, ntk - n0)
                kk = (n0 // 512) % 2
                EG, SGS, HE = eg[kk], sgs[kk], heT[kk]
                banks = [ps() for _ in range(4)]
                for c in range(8):
                    for bi, (wsb, fc) in enumerate(((WG, 0), (WG, 1), (WU, 0), (WU, 1))):
                        p, Bp = banks[bi]
                        S.op(PE, lambda e, p=p, wsb=wsb, fc=fc, c=c: e.matmul(
                            out=p[:, 0:nn_], lhsT=wsb[0][:, c, fc * 128:(fc + 1) * 128], rhs=h2s[:, c, n0:n0 + nn_],
                            start=(c == 0), stop=(c == 7)), r=[wsb[1], B_h2s], w=[Bp])
                for fc in range(2):
                    p, Bp = banks[fc]
                    S.op(ACT, lambda e, p=p, fc=fc: e.activation(out=EG[0][:, fc, 0:nn_], in_=p[:, 0:nn_], func=AF.Exp,
                                                                 scale=-1.0), r=[Bp], w=[EG[1]])
                S.op(POOL, lambda e: e.tensor_scalar(out=EG[0][:, :, 0:nn_], in0=EG[0][:, :, 0:nn_], scalar1=1.0,
                                                     scalar2=None, op0=ALU.add), r=[EG[1]], w=[EG[1]])
                S.op(DVE, lambda e: e.reciprocal(out=EG[0][:, :, 0:nn_], in_=EG[0][:, :, 0:nn_]), r=[EG[1]], w=[EG[1]])
                for fc in range(2):
                    p, Bp = banks[fc]
                    S.op(DVE, lambda e, p=p, fc=fc: e.tensor_tensor(out=SGS[0][:, fc, 0:nn_], in0=p[:, 0:nn_],
                                                                    in1=EG[0][:, fc, 0:nn_], op=ALU.mult),
                         r=[Bp, EG[1]], w=[SGS[1]])
                for fc in range(2):
                    p, Bp = banks[2 + fc]
                    S.op(DVE, lambda e, p=p, fc=fc: e.tensor_tensor(out=HE[0][:, fc, 0:nn_], in0=p[:, 0:nn_],
                                                                    in1=SGS[0][:, fc, 0:nn_], op=ALU.mult),
                         r=[Bp, SGS[1]], w=[HE[1]])
                for q0 in range(0, nn_, 128):
                    nq_ = min(128, nn_ - q0)
                    li = (n0 + q0) // 128
                    for half in range(2):
                        p, Bp = ps()
                        for fc in range(2):
                            S.op(PE, lambda e, p=p, fc=fc, half=half: e.matmul(
                                out=p[0:nq_, :], lhsT=HE[0][:, fc, q0:q0 + nq_], rhs=WD[0][:, fc, half * 512:(half + 1) * 512],
                                start=(fc == 0), stop=(fc == 1)), r=[HE[1], WD[1]], w=[Bp])
                        if (q0 // 128) % 2 == 0:
                            S.op(DVE, lambda e, p=p, half=half, li=li: e.scalar_tensor_tensor(
                                out=yacc[0:nq_, li, half * 512:(half + 1) * 512], in0=p[0:nq_, :],
                                scalar=gat[0:nq_, li, ex:ex + 1], in1=yacc[0:nq_, li, half * 512:(half + 1) * 512],
                                op0=ALU.mult, op1=ALU.add), r=[Bp, B_gat, B_ytile[li]], w=[B_ytile[li]])
                        else:
                            YT = ytmp[half]
                            S.op(ACT, lambda e, p=p, YT=YT, li=li: e.activation(
                                out=YT[0][0:nq_, 0:512], in_=p[0:nq_, :], func=AF.Identity,
                                scale=gat[0:nq_, li, ex:ex + 1]), r=[Bp, B_gat], w=[YT[1]])
                            S.op(POOL, lambda e, YT=YT, half=half, li=li: e.tensor_tensor(
                                out=yacc[0:nq_, li, half * 512:(half + 1) * 512], in0=YT[0][0:nq_, 0:512],
                                in1=yacc[0:nq_, li, half * 512:(half + 1) * 512], op=ALU.add),
                                r=[YT[1], B_ytile[li]], w=[B_ytile[li]])
        for li, ti2 in enumerate(tl):
            a0 = ti2 * 128
            n = min(128, TOK - a0)
            k = li % 2
            YO, SF, RF = yo[k], ssf[k], rsf[k]
            S.op(ACT, lambda e, li=li: e.activation(out=junk2[0:n, :], in_=yacc[0:n, li, :], func=AF.Square,
                                                    accum_out=SF[0][0:n, :]), r=[B_ytile[li]], w=[SF[1]])
            S.op(ACT, lambda e: e.activation(out=RF[0][0:n, :], in_=SF[0][0:n, :], func=AF.Ln, scale=1.0 / D,
                                             bias=eps2[0:n, :]), r=[SF[1], B_eps2], w=[RF[1]])
            S.op(ACT, lambda e: e.activation(out=RF[0][0:n, :], in_=RF[0][0:n, :], func=AF.Exp, scale=-0.5),
                 r=[RF[1]], w=[RF[1]])
            S.op(DVE, lambda e, li=li: e.scalar_tensor_tensor(out=YO[0][0:n, :], in0=yacc[0:n, li, :], scalar=RF[0][0:n, :],
                                                              in1=gfin_bc[0:n, :], op0=ALU.mult, op1=ALU.mult),
                 r=[B_ytile[li], RF[1], B_gfin], w=[YO[1]])
            lo, hi = max(a0, 16), min(a0 + n, LTOK)
            if hi > lo:
                S.dma(QS, y_p[lo - 16:hi - 16, :], YO[0][lo - a0:hi - a0, :], r=[YO[1]])
            lo, hi = max(a0, LTOK), min(a0 + n, TOK)
            if hi > lo:
                S.dma(QS, y_s[lo - LTOK:hi - LTOK, :], YO[0][lo - a0:hi - a0, :], r=[YO[1]])
    S.barrier()
    return nc


_CACHE = {}


def kernel(x_prompt, x_sample, cache_k, cache_v, cache_log_f, state_gla, page_table,
           meta_tokens, norm_mix_g, w_in, fox_f_bias, gla_w_a2, gla_b_a, gla_norm_g,
           w_fox_out, w_gla_out, w_o, norm_ffn_g, w_group_router, b_group_router,
           w_expert_router, b_expert_router, w_expert_gate, w_expert_up, w_expert_down,
           norm_final_g):
    f = lambda a: np.ascontiguousarray(np.asarray(a), dtype=np.float32)
    x_prompt, x_sample = f(x_prompt), f(x_sample)
    B, SEQ, _ = x_prompt.shape
    DB, DS, _ = x_sample.shape
    NC = 8
    NPT = SEQ // 128
    NSS = DB // NC
    NPG = page_table.shape[1]
    NPHYS = cache_k.shape[1]
    LTOK = 16 + SEQ
    key = (NPT, NSS, NPG, NPHYS)
    if key not in _CACHE:
        _CACHE[key] = build(*key)
    nc = _CACHE[key]
    ck = f(cache_k)[0].reshape(NPHYS, 128, 512)
    cv = f(cache_v)[0].reshape(NPHYS, 128, 512)
    clf = f(cache_log_f)[0].reshape(NPHYS, 1024)
    pt = np.ascontiguousarray(np.asarray(page_table), dtype=np.int32)
    shared = dict(
        ck=ck, cv=cv, clf=clf, meta=f(meta_tokens), g_mix=f(norm_mix_g)[0], w_in=f(w_in)[0],
        f_bias=f(fox_f_bias)[0], w_a2=f(gla_w_a2)[0], b_a=f(gla_b_a)[0], g_gla=f(gla_norm_g)[0],
        w_fo=f(w_fox_out)[0], w_go=f(w_gla_out)[0], w_o=f(w_o)[0], g_ffn=f(norm_ffn_g)[0],
        w_gr=f(w_group_router)[0], b_gr=f(b_group_router)[0], w_er=f(w_expert_router)[0],
        b_er=f(b_expert_router)[0], w_eg=f(w_expert_gate)[0], w_eu=f(w_expert_up)[0],
        w_ed=f(w_expert_down)[0], g_fin=f(norm_final_g))
    in_maps = []
    for c in range(NC):
        m = dict(shared)
        m["xp"] = x_prompt[c]
        m["xs"] = x_sample[c * NSS:(c + 1) * NSS].reshape(NSS * DS, D)
        m["sgl"] = f(state_gla)[0, c * NSS:(c + 1) * NSS]
        ptc = pt[c * NSS:(c + 1) * NSS].reshape(1, NSS * NPG)
        m["ptab"] = ptc
        m["ptcol"] = np.ascontiguousarray(ptc.reshape(NSS * NPG, 1))
        in_maps.append(m)
    res = run_bass_kernel_spmd(nc, in_maps, core_ids=list(range(NC))).results
    cat = lambda k: np.stack([np.asarray(r[k]) for r in res])
    y_prompt = cat("y_p").reshape(B, SEQ, D)
    y_sample = cat("y_s").reshape(DB, DS, D)
    nk_p = cat("nk_p").reshape(1, B, LTOK, 8, 64)
    nv_p = cat("nv_p").reshape(1, B, LTOK, 8, 64)
    nlf_p = cat("nlf_p").reshape(1, B, LTOK, 8)
    ngl_p = cat("ngl_p").reshape(1, B, 4, 64, 128)
    nk_s = cat("nk_s").reshape(1, DB, DS, 8, 64)
    nv_s = cat("nv_s").reshape(1, DB, DS, 8, 64)
    nlf_s = cat("nlf_s").reshape(1, DB, DS, 8)
    ngl_s = cat("ngl_s").reshape(1, DB, 4, 64, 128)
    return (y_prompt.astype(np.float32), y_sample.astype(np.float32), nk_p, nv_p, nlf_p, ngl_p,
            nk_s, nv_s, nlf_s, ngl_s)
```

```python
import numpy as np
import concourse.bass as bass
import concourse.mybir as mybir
from concourse.bass_utils import run_bass_kernel_spmd

F32 = mybir.dt.float32
BF16 = mybir.dt.bfloat16
I32 = mybir.dt.int32
AF = mybir.ActivationFunctionType
ALU = mybir.AluOpType
AX = mybir.AxisListType
ET = mybir.EngineType

D = 1024
PW = 5144
C_QF, C_KF, C_VF, C_FF, C_QG, C_KG, C_VG, C_LR, C_RG, C_GF, C_GG = (
    0, 512, 1024, 1536, 1544, 1800, 2056, 2568, 2584, 3096, 4120)
EPS = 1e-6
NE = 32
NSD = 12
SB_BASE = 16640
DEBUG = False
STOP = ''
DEBUG_TILE = 0


class StopBuild(Exception):
    pass


class Buf:
    __slots__ = ("w", "r", "name", "excl")

    def __init__(self, name="", excl=False):
        self.w = None
        self.r = {}
        self.name = name
        self.excl = excl


class Eng:
    def __init__(self, name, e, sem):
        self.name, self.e, self.sem, self.cnt, self.waited = name, e, sem, 0, {}


class Queue:
    def __init__(self, E, sems):
        self.E, self.sems, self.k = E, sems, 0


class Sched:
    def __init__(self, nc):
        self.nc = nc
        mk = lambda n: nc.alloc_semaphore(n)
        self.PE = Eng("pe", nc.tensor, mk("s_pe"))
        self.ACT = Eng("act", nc.scalar, mk("s_act"))
        self.DVE = Eng("dve", nc.vector, mk("s_dve"))
        self.POOL = Eng("pool", nc.gpsimd, mk("s_pool"))
        self.SP = Eng("sp", nc.sync, mk("s_sp"))
        self.engs = [self.PE, self.ACT, self.DVE, self.POOL, self.SP]
        self.QS = Queue(self.SP, [mk(f"s_qs{i}") for i in range(NSD)])
        self.QP = Queue(self.POOL, [mk(f"s_qp{i}") for i in range(NSD)])

    def _wait(self, E, need):
        for s, v in need.items():
            if E is self.PE and s is self.PE.sem:
                continue
            if E.waited.get(s, 0) < v:
                E.e.wait_ge(s, v)
                E.waited[s] = v

    @staticmethod
    def _need(r, w, own=None):
        need = {}

        def add(tok):
            if tok is None:
                return
            s, v = tok
            if need.get(s, 0) < v:
                need[s] = v
        for b in r:
            add(b.w)
            if b.excl:
                for s, v in b.r.items():
                    if s is not own:
                        add((s, v))
        for b in w:
            add(b.w)
            for s, v in b.r.items():
                add((s, v))
        return need

    @staticmethod
    def _mark(tok, r, w):
        s, v = tok
        for b in r:
            if b.r.get(s, 0) < v:
                b.r[s] = v
        for b in w:
            b.w = tok
            b.r = {}

    def op(self, E, fn, r=(), w=()):
        self._wait(E, self._need(r, w, E.sem))
        ins = fn(E.e)
        E.cnt += 1
        ins.then_inc(E.sem, 1)
        self._mark((E.sem, E.cnt), r, w)
        return ins

    def dma(self, Q, out, in_, r=(), w=(), **kw):
        E = Q.E
        k = Q.k
        Q.k += 1
        s = Q.sems[k % NSD]
        base = 16 * (k // NSD)
        need = self._need(r, w)
        if base > 0 and need.get(s, 0) < base:
            need[s] = base
        self._wait(E, need)
        ins = E.e.dma_start(out=out, in_=in_, **kw)
        ins.then_inc(s, 16)
        self._mark((s, base + 16), r, w)
        return ins

    def idma(self, out, in_, idx_ap, r=(), w=()):
        Q = self.QP
        E = Q.E
        k = Q.k
        Q.k += 1
        s = Q.sems[k % NSD]
        base = 16 * (k // NSD)
        need = self._need(r, w)
        if base > 0 and need.get(s, 0) < base:
            need[s] = base
        self._wait(E, need)
        ins = E.e.indirect_dma_start(out=out, out_offset=None, in_=in_,
                                     in_offset=bass.IndirectOffsetOnAxis(idx_ap, 0))
        ins.then_inc(s, 16)
        self._mark((s, base + 16), r, w)
        return ins

    def barrier(self):
        toks = {}
        for E in self.engs:
            if E.cnt:
                toks[E.sem] = E.cnt
        for Q in (self.QS, self.QP):
            for i, s in enumerate(Q.sems):
                n = (Q.k - i + NSD - 1) // NSD if Q.k > i else 0
                if n:
                    toks[s] = 16 * n
        for E in self.engs:
            need = {s: v for s, v in toks.items() if s is not E.sem}
            for s, v in need.items():
                if E.waited.get(s, 0) < v:
                    E.e.wait_ge(s, v)
                    E.waited[s] = v


class Alloc:
    def __init__(self, nc):
        self.nc, self.cur, self.n = nc, SB_BASE, 0

    def t(self, shape, dt, name=None):
        isz = {F32: 4, BF16: 2, I32: 4}[dt]
        per = int(np.prod(shape[1:])) * isz
        per = (per + 63) // 64 * 64
        self.n += 1
        h = self.nc.alloc_sbuf_tensor_at(f"{name or 't'}_{self.n}", list(shape), dt, offset=self.cur)
        self.cur += per
        assert self.cur <= 229000, f"SBUF overflow {self.cur}"
        return h


def build(NPT, NSS, NPG, NPHYS):
    nc = bass.Bass("TRN2", target_bir_lowering=False)
    st = {}
    try:
        _build(nc, st, NPT, NSS, NPG, NPHYS)
    except StopBuild:
        st['S'].barrier()
    return nc


def _build(nc, st, NPT, NSS, NPG, NPHYS):
    LTOK = 16 + NPT * 128
    TOK = LTOK + NSS * 8
    NJ = NPT + 1

    def din(name, shape, dt=F32):
        return nc.dram_tensor(name, list(shape), dt, kind="ExternalInput").ap()

    def dout(name, shape, dt=F32):
        return nc.dram_tensor(name, list(shape), dt, kind="ExternalOutput").ap()

    def dscr(name, shape, dt=F32):
        return nc.dram_tensor(name, list(shape), dt, kind="Internal").ap()

    xp = din("xp", [NPT * 128, D])
    xs = din("xs", [NSS * 8, D])
    ck = din("ck", [NPHYS, 128, 512])
    cv = din("cv", [NPHYS, 128, 512])
    clf = din("clf", [NPHYS, 1024])
    sgl = din("sgl", [NSS, 4, 64, 128])
    ptab = din("ptab", [1, NSS * NPG], I32)
    ptcol = din("ptcol", [NSS * NPG, 1], I32)
    meta = din("meta", [16, D])
    g_mix = din("g_mix", [D])
    w_in = din("w_in", [D, PW])
    f_bias = din("f_bias", [8])
    w_a2 = din("w_a2", [16, 256])
    b_a = din("b_a", [256])
    g_gla = din("g_gla", [512])
    w_fo = din("w_fo", [512, D])
    w_go = din("w_go", [512, D])
    w_o = din("w_o", [D, D])
    g_ffn = din("g_ffn", [D])
    w_gr = din("w_gr", [D, 4])
    b_gr = din("b_gr", [4])
    w_er = din("w_er", [D, 32])
    b_er = din("b_er", [32])
    w_eg = din("w_eg", [NE, D, 256])
    w_eu = din("w_eu", [NE, D, 256])
    w_ed = din("w_ed", [NE, 256, D])
    g_fin = din("g_fin", [D])

    y_p = dout("y_p", [NPT * 128, D])
    y_s = dout("y_s", [NSS * 8, D])
    nk_p = dout("nk_p", [LTOK, 512])
    nv_p = dout("nv_p", [LTOK, 512])
    nlf_p = dout("nlf_p", [LTOK, 8])
    ngl_p = dout("ngl_p", [4, 64, 128])
    nk_s = dout("nk_s", [NSS * 8, 512])
    nv_s = dout("nv_s", [NSS * 8, 512])
    nlf_s = dout("nlf_s", [NSS * 8, 8])
    ngl_s = dout("ngl_s", [NSS, 4, 64, 128])

    qT_scr = dscr("qT_scr", [128, 4, TOK], BF16)
    kT_scr = dscr("kT_scr", [128, 4, TOK], BF16)
    v1_scr = dscr("v1_scr", [TOK, 520], BF16)
    c_scr = dscr("c_scr", [TOK, 8])
    sgf_scr = dscr("sgf_scr", [TOK, D], BF16)
    mg_scr = dscr("mg_scr", [TOK, D], BF16)
    x1_scr = dscr("x1_scr", [TOK, D])
    h2T_scr = dscr("h2T_scr", [128, 8, TOK], BF16)
    gat_scr = dscr("gat_scr", [TOK, 32])

    _lp = nc.allow_low_precision(reason="bf16 matmul operands by design; fp32 accumulation")
    _lp.__enter__()
    S = Sched(nc)
    st['S'] = S
    PE, ACT, DVE, POOL, SP = S.PE, S.ACT, S.DVE, S.POOL, S.SP
    QS, QP = S.QS, S.QP
    A = Alloc(nc)

    psb = [nc.alloc_psum_tensor(f"ps{i}", [128, 512], F32) for i in range(8)]
    psB = [Buf(f"ps{i}", excl=True) for i in range(8)]
    rot = {"i": 0, "n": 8}

    def ps():
        i = rot["i"] % rot["n"]
        rot["i"] += 1
        return psb[i], psB[i]

    def bfv(p):
        return p[:, :].bitcast(BF16)

    cnt = {"i": 0}

    def T(shape, dt, name="t"):
        return A.t(shape, dt, name), Buf(name)

    def T2(shape, dt, name="t", n=2):
        return [T(shape, dt, f"{name}{i}") for i in range(n)]

    ones_f, B_ones = T([128, 128], F32, "ones")
    tri_f, B_tri = T([128, 128], F32, "tri")
    triR_f, B_triR = T([128, 128], F32, "triR")
    id_f, B_idf = T([128, 128], F32, "idf")
    id_b, B_idb = T([128, 128], BF16, "idb")
    tri_b, B_trib = T([128, 128], BF16, "trib")
    ones_b, B_onesb = T([128, 128], BF16, "onesb")
    zer_b, B_zer = T([128, 512], BF16, "zer")
    junk = A.t([128, 1024], BF16, "junk")

    S.op(POOL, lambda e: e.memset(ones_f[:, :], 1.0), w=[B_ones])
    S.op(POOL, lambda e: e.memset(zer_b[:, :], 0.0), w=[B_zer])
    S.op(POOL, lambda e: e.affine_select(out=tri_f[:, :], in_=ones_f[:, :], pattern=[[1, 128]],
                                         compare_op=ALU.is_ge, fill=0.0, base=0, channel_multiplier=-1),
         r=[B_ones], w=[B_tri])
    S.op(POOL, lambda e: e.affine_select(out=triR_f[:, :], in_=ones_f[:, :], pattern=[[-1, 128]],
                                         compare_op=ALU.is_ge, fill=0.0, base=-1, channel_multiplier=1),
         r=[B_ones], w=[B_triR])
    S.op(POOL, lambda e: e.affine_select(out=id_f[:, :], in_=ones_f[:, :], pattern=[[1, 128]],
                                         compare_op=ALU.is_equal, fill=0.0, base=0, channel_multiplier=-1),
         r=[B_ones], w=[B_idf])
    S.op(POOL, lambda e: e.tensor_copy(out=id_b[:, :], in_=id_f[:, :]), r=[B_idf], w=[B_idb])
    S.op(POOL, lambda e: e.tensor_copy(out=tri_b[:, :], in_=tri_f[:, :]), r=[B_tri], w=[B_trib])
    S.op(POOL, lambda e: e.tensor_copy(out=ones_b[:, :], in_=ones_f[:, :]), r=[B_ones], w=[B_onesb])

    def bc_load(src1d, n, name):
        t, b = T([128, n], F32, name)
        S.dma(QS, t[:, :], src1d.partition_broadcast(128), w=[b])
        return t, b

    gmix_bc, B_gmix = bc_load(g_mix, D, "gmix")
    gffn_bc, B_gffn = bc_load(g_ffn, D, "gffn")
    ggla_bc, B_ggla = bc_load(g_gla, 512, "ggla")
    fb_bc, B_fb = bc_load(f_bias, 8, "fb")
    rb_bc, B_rb = T([128, 36], F32, "rb")
    S.dma(QS, rb_bc[:, 0:4], b_gr.partition_broadcast(128), w=[B_rb])
    S.dma(QS, rb_bc[:, 4:36], b_er.partition_broadcast(128), w=[B_rb])
    nba, B_nba = T([128, 2], F32, "nba")
    with nc.allow_non_contiguous_dma(reason="tiny bias column load"):
        S.dma(QS, nba[:, :], b_a.rearrange("(c p) -> p c", p=128), w=[B_nba])
    S.op(DVE, lambda e: e.tensor_scalar(out=nba[:, :], in0=nba[:, :], scalar1=-1.0, scalar2=None,
                                        op0=ALU.mult), r=[B_nba], w=[B_nba])
    wa2, B_wa2 = T([16, 256], F32, "wa2")
    S.dma(QS, wa2[:, :], w_a2, w=[B_wa2])
    wr, B_wr = T([128, 8, 36], F32, "wr")
    S.dma(QS, wr[:, :, 0:4], w_gr.rearrange("(c p) n -> p c n", p=128), w=[B_wr])
    S.dma(QS, wr[:, :, 4:36], w_er.rearrange("(c p) n -> p c n", p=128), w=[B_wr])
    NPGT = NSS * NPG
    ptb_i, B_ptbi = T([128, NPGT], I32, "ptbi")
    S.dma(QS, ptb_i[:, :], ptab[0].partition_broadcast(128), w=[B_ptbi])
    slot_i, B_sloti = T([128, 1], I32, "sloti")
    S.op(POOL, lambda e: e.iota(out=slot_i[:, :], pattern=[[0, 1]], base=0, channel_multiplier=1), w=[B_sloti])
    slot_f, B_slotf = T([128, 1], F32, "slotf")
    S.op(DVE, lambda e: e.tensor_copy(out=slot_f[:, :], in_=slot_i[:, :]), r=[B_sloti], w=[B_slotf])
    ptb_f, B_ptbf = T([128, NPGT], F32, "ptbf")
    S.op(DVE, lambda e: e.tensor_copy(out=ptb_f[:, :], in_=ptb_i[:, :]), r=[B_ptbi], w=[B_ptbf])
    S.op(DVE, lambda e: e.tensor_scalar(out=ptb_f[:, :], in0=ptb_f[:, :], scalar1=128.0, scalar2=slot_f[:, 0:1],
                                        op0=ALU.mult, op1=ALU.add), r=[B_ptbf, B_slotf], w=[B_ptbf])
    idx_all, B_idx = T([128, NPGT], I32, "idxall")
    S.op(DVE, lambda e: e.tensor_copy(out=idx_all[:, :], in_=ptb_f[:, :]), r=[B_ptbf], w=[B_idx])
    ck_rows = ck.rearrange("n s f -> (n s) f")
    cv_rows = cv.rearrange("n s f -> (n s) f")
    cref, B_cref = T([128, NPT // 4 + 2, 8], F32, "cref")
    carry, B_carry = T([128, 8], F32, "carry")
    S.op(DVE, lambda e: e.memset(carry[:, :], 0.0), w=[B_carry])
    wgo, B_wgo = T([128, 4, D], BF16, "wgo")
    for c in range(4):
        S.dma(QP, wgo[:, c, :], w_go[c * 128:(c + 1) * 128, :], w=[B_wgo])
    eps_c, B_eps = T([128, 1], F32, "eps")
    S.op(DVE, lambda e: e.memset(eps_c[:, :], EPS), w=[B_eps])
    one_c, B_one = T([128, 1], F32, "onec")
    S.op(DVE, lambda e: e.memset(one_c[:, :], 1.0), w=[B_one])
    hm = []
    for par in range(2):
        t_, b_ = T([128, 1], F32, f"hm{par}")
        S.op(DVE, lambda e, t_=t_: e.memset(t_[:, :], 0.0), w=[b_])
        S.op(DVE, lambda e, t_=t_, par=par: e.memset(t_[par * 64:(par + 1) * 64, :], 0.125), w=[b_])
        hm.append((t_, b_))
    persist_mark = A.cur

    if STOP == "0":
        S.barrier()
        return nc
    win, B_win = T([128, 8, PW], BF16, "win")
    w_in3 = w_in.rearrange("(c p) n -> p c n", p=128)
    for c0 in range(0, PW, 2048):
        c1 = min(PW, c0 + 2048)
        S.dma(QP, win[:, :, c0:c1], w_in3[:, :, c0:c1], w=[B_win])

    xt = T2([128, D], F32, "xt")
    ssq = T2([128, 1], F32, "ssq")
    rstd = T2([128, 1], F32, "rstd")
    hb = T2([128, D], BF16, "hb")
    hT = T2([128, 8, 128], BF16, "hT")
    qTt = T2([128, 4, 128], BF16, "qTt")
    kTt = T2([128, 4, 128], BF16, "kTt")
    lrgT = T2([16, 128], F32, "lrgT")
    kout = T2([128, 512], F32, "kout")
    vout = T2([128, 512], F32, "vout")
    v1t = T2([128, 8, 65], BF16, "v1t")
    vgb = T2([128, 512], BF16, "vgb")
    etmp = T2([128, D], F32, "etmp", 3)
    rsil = T2([128, 512], F32, "rsil")
    sgf = T2([128, D], BF16, "sgf")
    sgg = T2([128, D], BF16, "sgg")
    lft = T2([128, 8], F32, "lft")
    ltmp = T2([128, 8], F32, "ltmp")
    ctile = T2([128, 8], F32, "ctile")
    ea = T2([128, 2, 128], F32, "ea")
    csum = T2([128, 2, 128], F32, "csum")
    ebt = T2([128, 2, 128], F32, "ebt")
    enbt = T2([128, 2, 128], F32, "enbt")
    qtl = T2([128, 2, 2, 128], BF16, "qtl")
    ktl = T2([128, 2, 128], BF16, "ktl")
    ktok = T2([128, 256], BF16, "ktok")
    Am = T2([128, 4, 128], BF16, "Am")
    Sst, B_S = T([128, 2, 128], F32, "S")
    Sb, B_Sb = T([128, 2, 128], BF16, "Sb")
    ssg = T2([128, 4], F32, "ssg")
    rsg = T2([128, 4], F32, "rsg")
    o1 = T2([128, 512], F32, "o1")
    ogb = T2([128, 4, 128], BF16, "ogb")
    ogT = T2([128, 4, 128], BF16, "ogT")
    mgt = T2([128, D], BF16, "mgt")
    for i in range(2):
        S.op(POOL, lambda e, i=i: e.memset(v1t[i][0][:, :, 64:65], 1.0), w=[v1t[i][1]])

    def rmsnorm_stats(x_ap, Bx, nt, ss_, rs_, n):
        S.op(ACT, lambda e: e.activation(out=junk[0:nt, 0:n], in_=x_ap, func=AF.Square,
                                         accum_out=ss_[0][0:nt, :]), r=[Bx], w=[ss_[1]])
        S.op(ACT, lambda e: e.activation(out=rs_[0][0:nt, :], in_=ss_[0][0:nt, :], func=AF.Ln,
                                         scale=1.0 / n, bias=eps_c[0:nt, :]), r=[ss_[1], B_eps], w=[rs_[1]])
        S.op(ACT, lambda e: e.activation(out=rs_[0][0:nt, :], in_=rs_[0][0:nt, :], func=AF.Exp,
                                         scale=-0.5), r=[rs_[1]], w=[rs_[1]])

    def sigmoid_from(ps_ap, Bp, nt, n, tmp, out_ap, Bout):
        S.op(ACT, lambda e: e.activation(out=tmp[0][0:nt, 0:n], in_=ps_ap, func=AF.Exp, scale=-1.0),
             r=[Bp], w=[tmp[1]])
        S.op(POOL, lambda e: e.tensor_scalar(out=tmp[0][0:nt, 0:n], in0=tmp[0][0:nt, 0:n], scalar1=1.0,
                                             scalar2=None, op0=ALU.add), r=[tmp[1]], w=[tmp[1]])
        S.op(DVE, lambda e: e.reciprocal(out=out_ap, in_=tmp[0][0:nt, 0:n]), r=[tmp[1]], w=[Bout])

    def proj_tok(hTs, nt, col0, ncols=512):
        p, Bp = ps()
        for c in range(8):
            S.op(PE, lambda e, c=c: e.matmul(out=p[0:nt, 0:ncols], lhsT=hTs[0][:, c, 0:nt],
                                             rhs=win[:, c, col0:col0 + ncols], start=(c == 0), stop=(c == 7)),
                 r=[hTs[1], B_win], w=[Bp])
        return p, Bp

    def proj_fm(hTs, nt, col0, nblk, m=128):
        p, Bp = ps()
        for b in range(nblk):
            for c in range(8):
                S.op(PE, lambda e, c=c, b=b: e.matmul(out=p[0:m, b * 128:b * 128 + nt],
                                                      lhsT=win[:, c, col0 + b * 128:col0 + b * 128 + m],
                                                      rhs=hTs[0][:, c, 0:nt], start=(c == 0), stop=(c == 7)),
                     r=[hTs[1], B_win], w=[Bp])
        return p, Bp

    def chk(tag):
        if STOP == tag:
            raise StopBuild()

    def phase1a(ti, kind, nt, idx, tok0):
        k = ti % 2
        X, SS, RS, HB, HT = xt[k], ssq[k], rstd[k], hb[k], hT[k]
        src = meta if kind == "m" else (xp[idx * 128:(idx + 1) * 128, :] if kind == "p"
                                        else xs[idx * 8:(idx + 1) * 8, :])
        S.dma(QS, X[0][0:nt, :], src, w=[X[1]])
        rmsnorm_stats(X[0][0:nt, :], X[1], nt, SS, RS, D)
        S.op(DVE, lambda e: e.scalar_tensor_tensor(out=HB[0][0:nt, :], in0=X[0][0:nt, :], scalar=RS[0][0:nt, :],
                                                   in1=gmix_bc[0:nt, :], op0=ALU.mult, op1=ALU.mult),
             r=[X[1], RS[1], B_gmix], w=[HB[1]])
        p, Bp = ps()
        pv = bfv(p)
        for c in range(8):
            S.op(PE, lambda e, c=c: e.transpose(out=pv[:, c * 128:c * 128 + nt], in_=HB[0][0:nt, c * 128:(c + 1) * 128],
                                                identity=id_b[0:nt, 0:nt]), r=[HB[1], B_idb], w=[Bp])
        S.op(ACT, lambda e: e.activation(out=HT[0][:, :, 0:nt],
                                         in_=pv.rearrange("p (c t) -> p c t", c=8)[:, :, 0:nt], func=AF.Copy),
             r=[Bp], w=[HT[1]])
        chk("a1")
        for (col0, TT, scr) in ((C_QF, qTt[k], qT_scr), (C_KF, kTt[k], kT_scr)):
            p, Bp = proj_fm(HT, nt, col0, 4)
            S.op(ACT, lambda e, p=p, TT=TT: e.activation(out=TT[0][:, :, 0:nt],
                                                         in_=p[:, :].rearrange("p (c t) -> p c t", c=4)[:, :, 0:nt],
                                                         func=AF.Copy), r=[Bp], w=[TT[1]])
            S.dma(QS, scr[:, :, tok0:tok0 + nt], TT[0][:, :, 0:nt], r=[TT[1]])
        p, Bp = proj_fm(HT, nt, C_LR, 1, m=16)
        LR = lrgT[k]
        S.op(ACT, lambda e: e.activation(out=LR[0][0:16, 0:nt], in_=p[0:16, 0:nt], func=AF.Copy), r=[Bp], w=[LR[1]])
        chk("a2")
        dk, dv, dlf = (nk_p, nv_p, nlf_p) if kind != "s" else (nk_s, nv_s, nlf_s)
        orow = tok0 if kind != "s" else idx * 8
        p, Bp = proj_tok(HT, nt, C_KF)
        KO = kout[k]
        S.op(ACT, lambda e: e.activation(out=KO[0][0:nt, :], in_=p[0:nt, :], func=AF.Copy), r=[Bp], w=[KO[1]])
        chk("b0")
        S.dma(QS, dk[orow:orow + nt, :], KO[0][0:nt, :], r=[KO[1]])
        chk("b1")
        p, Bp = proj_tok(HT, nt, C_VF)
        VO, V1 = vout[k], v1t[k]
        S.op(ACT, lambda e: e.activation(out=VO[0][0:nt, :], in_=p[0:nt, :], func=AF.Copy), r=[Bp], w=[VO[1]])
        S.op(DVE, lambda e: e.tensor_copy(out=V1[0][0:nt, :, 0:64],
                                          in_=p[0:nt, :].rearrange("p (h d) -> p h d", h=8)), r=[Bp], w=[V1[1]])
        chk("b2")
        S.dma(QS, dv[orow:orow + nt, :], VO[0][0:nt, :], r=[VO[1]])
        S.dma(QS, v1_scr[tok0:tok0 + nt, :], V1[0][0:nt, :, :].rearrange("p h d -> p (h d)"), r=[V1[1]])
        chk("b3")
        p, Bp = proj_tok(HT, nt, C_VG)
        VG = vgb[k]
        S.op(DVE, lambda e: e.tensor_copy(out=VG[0][0:nt, :], in_=p[0:nt, :]), r=[Bp], w=[VG[1]])
        chk("a3")
        p, Bp = proj_tok(HT, nt, C_RG)
        E0, RSL = etmp[0], rsil[k]
        sigmoid_from(p[0:nt, :], Bp, nt, 512, E0, E0[0][0:nt, 0:512], E0[1])
        S.op(DVE, lambda e: e.tensor_tensor(out=RSL[0][0:nt, :], in0=p[0:nt, :], in1=E0[0][0:nt, 0:512], op=ALU.mult),
             r=[Bp, E0[1]], w=[RSL[1]])
        S.op(POOL, lambda e: e.tensor_tensor(out=RSL[0][0:nt, :], in0=RSL[0][0:nt, :], in1=ggla_bc[0:nt, :],
                                             op=ALU.mult), r=[RSL[1], B_ggla], w=[RSL[1]])
        chk("a4")
        SGF, SGG = sgf[k], sgg[k]
        for (col0, SG, ei) in ((C_GF, SGF, 1), (C_GG, SGG, 2)):
            for half in range(2):
                p, Bp = proj_tok(HT, nt, col0 + half * 512)
                ET_ = etmp[ei]
                S.op(ACT, lambda e, p=p, ET_=ET_, half=half: e.activation(
                    out=ET_[0][0:nt, half * 512:(half + 1) * 512], in_=p[0:nt, :], func=AF.Exp, scale=-1.0),
                    r=[Bp], w=[ET_[1]])
            S.op(POOL, lambda e, ET_=ET_: e.tensor_scalar(out=ET_[0][0:nt, :], in0=ET_[0][0:nt, :], scalar1=1.0,
                                                          scalar2=None, op0=ALU.add), r=[ET_[1]], w=[ET_[1]])
            S.op(DVE, lambda e, ET_=ET_, SG=SG: e.reciprocal(out=SG[0][0:nt, :], in_=ET_[0][0:nt, :]),
                 r=[ET_[1]], w=[SG[1]])
        S.dma(QS, sgf_scr[tok0:tok0 + nt, :], SGF[0][0:nt, :], r=[SGF[1]])
        chk("a5")
        p, Bp = proj_tok(HT, nt, C_FF, 8)
        LF, LT, CT = lft[k], ltmp[k], ctile[k]
        S.op(DVE, lambda e: e.tensor_tensor(out=LT[0][0:nt, :], in0=p[0:nt, 0:8], in1=fb_bc[0:nt, :], op=ALU.add),
             r=[Bp, B_fb], w=[LT[1]])
        S.op(ACT, lambda e: e.activation(out=LT[0][0:nt, :], in_=LT[0][0:nt, :], func=AF.Exp, scale=-1.0),
             r=[LT[1]], w=[LT[1]])
        S.op(ACT, lambda e: e.activation(out=LT[0][0:nt, :], in_=LT[0][0:nt, :], func=AF.Ln, bias=one_c[0:nt, :]),
             r=[LT[1]], w=[LT[1]])
        S.op(DVE, lambda e: e.tensor_scalar(out=LF[0][0:nt, :], in0=LT[0][0:nt, :], scalar1=-1.0, scalar2=None,
                                            op0=ALU.mult), r=[LT[1]], w=[LF[1]])
        S.dma(QS, dlf[orow:orow + nt, :], LF[0][0:nt, :], r=[LF[1]])
        p, Bp = ps()
        S.op(PE, lambda e: e.matmul(out=p[0:nt, 0:8], lhsT=tri_f[0:nt, 0:nt], rhs=LF[0][0:nt, :], start=True, stop=True),
             r=[B_tri, LF[1]], w=[Bp])
        if kind == "s":
            S.op(DVE, lambda e: e.tensor_copy(out=CT[0][0:nt, :], in_=p[0:nt, 0:8]), r=[Bp], w=[CT[1]])
        else:
            S.op(DVE, lambda e: e.tensor_tensor(out=CT[0][0:nt, :], in0=p[0:nt, 0:8], in1=carry[0:nt, :], op=ALU.add),
                 r=[Bp, B_carry], w=[CT[1]])
            p2, Bp2 = ps()
            S.op(PE, lambda e: e.matmul(out=p2[:, 0:8], lhsT=ones_f[0:nt, :], rhs=LF[0][0:nt, :], start=True, stop=True),
                 r=[B_ones, LF[1]], w=[Bp2])
            S.op(DVE, lambda e: e.tensor_tensor(out=carry[:, :], in0=p2[:, 0:8], in1=carry[:, :], op=ALU.add),
                 r=[Bp2, B_carry], w=[B_carry])
        S.dma(QS, c_scr[tok0:tok0 + nt, :], CT[0][0:nt, :], r=[CT[1]])
        chk("a6")
        return

    def phase1a_gla(ti, kind, nt, idx, tok0):
        k = ti % 2
        HT, LR, VG, RSL, SGG = hT[k], lrgT[k], vgb[k], rsil[k], sgg[k]
        EA, CS, EB, ENB, QTL, KTL, KTK, AM = ea[k], csum[k], ebt[k], enbt[k], qtl[k], ktl[k], ktok[k], Am[k]
        p, Bp = ps()
        for c in range(2):
            S.op(PE, lambda e, c=c: e.matmul(out=p[:, c * 128:c * 128 + nt], lhsT=wa2[0:16, c * 128:(c + 1) * 128],
                                             rhs=LR[0][0:16, 0:nt], start=True, stop=True), r=[B_wa2, LR[1]], w=[Bp])
        for c in range(2):
            S.op(ACT, lambda e, c=c: e.activation(out=EA[0][:, c, 0:nt], in_=p[:, c * 128:c * 128 + nt], func=AF.Exp,
                                                  scale=-1.0, bias=nba[:, c:c + 1]), r=[Bp, B_nba], w=[EA[1]])
        S.op(ACT, lambda e: e.activation(out=EA[0][:, :, 0:nt], in_=EA[0][:, :, 0:nt], func=AF.Ln, bias=one_c[:, :]),
             r=[EA[1]], w=[EA[1]])
        for c in range(2):
            S.op(DVE, lambda e, c=c: e.tensor_tensor_scan(out=CS[0][:, c, 0:nt], data0=ones_f[:, 0:nt],
                                                          data1=EA[0][:, c, 0:nt], initial=0.0,
                                                          op0=ALU.mult, op1=ALU.add), r=[EA[1], B_ones], w=[CS[1]])
        S.op(ACT, lambda e: e.activation(out=EB[0][:, :, 0:nt], in_=CS[0][:, :, 0:nt], func=AF.Exp, scale=-1.0 / 16),
             r=[CS[1]], w=[EB[1]])
        S.op(ACT, lambda e: e.activation(out=ENB[0][:, :, 0:nt], in_=CS[0][:, :, 0:nt], func=AF.Exp, scale=1.0 / 16),
             r=[CS[1]], w=[ENB[1]])
        pqk, Bpqk = ps()
        for b in range(4):
            col0 = C_QG + b * 128
            for c in range(8):
                S.op(PE, lambda e, c=c, b=b, col0=col0: e.matmul(out=pqk[:, b * 128:b * 128 + nt],
                                                                 lhsT=win[:, c, col0:col0 + 128],
                                                                 rhs=HT[0][:, c, 0:nt], start=(c == 0), stop=(c == 7)),
                     r=[HT[1], B_win], w=[Bpqk])
        pq3 = pqk[:, :].rearrange("p (b t) -> p b t", b=4)
        for par in range(2):
            S.op(DVE, lambda e, par=par: e.scalar_tensor_tensor(out=QTL[0][:, par, :, 0:nt], in0=pq3[:, 0:2, 0:nt],
                                                                scalar=hm[par][0][:, 0:1], in1=EB[0][:, :, 0:nt],
                                                                op0=ALU.mult, op1=ALU.mult),
                 r=[Bpqk, EB[1], hm[par][1]], w=[QTL[1]])
        S.op(DVE, lambda e: e.tensor_tensor(out=KTL[0][:, :, 0:nt], in0=pq3[:, 2:4, 0:nt], in1=ENB[0][:, :, 0:nt],
                                            op=ALU.mult), r=[Bpqk, ENB[1]], w=[KTL[1]])
        p, Bp = ps()
        pv = bfv(p)
        for c in range(2):
            S.op(PE, lambda e, c=c: e.transpose(out=pv[0:nt, c * 128:(c + 1) * 128], in_=KTL[0][:, c, 0:nt],
                                                identity=id_b[:, :]), r=[KTL[1], B_idb], w=[Bp])
        S.op(ACT, lambda e: e.activation(out=KTK[0][0:nt, :], in_=pv[0:nt, 0:256], func=AF.Copy), r=[Bp], w=[KTK[1]])
        chk("a7")
        pa, Bpa = ps()
        for h in range(4):
            r0, c = (h % 2) * 64, h // 2
            S.op(PE, lambda e, h=h, c=c: e.matmul(out=pa[0:nt, h * 128:h * 128 + nt],
                                                  lhsT=KTL[0][:, c, 0:nt],
                                                  rhs=QTL[0][:, h % 2, c, 0:nt], start=True, stop=True),
                 r=[KTL[1], QTL[1]], w=[Bpa])
        S.op(DVE, lambda e: e.tensor_tensor(out=AM[0][0:nt, :, 0:nt],
                                            in0=pa[:, :].rearrange("p (h t) -> p h t", h=4)[0:nt, :, 0:nt],
                                            in1=tri_b[0:nt, 0:nt].unsqueeze(1).to_broadcast([nt, 4, nt]),
                                            op=ALU.mult), r=[Bpa, B_trib], w=[AM[1]])
        po_, Bpo = ps()
        for h in range(4):
            r0, c = (h % 2) * 64, h // 2
            S.op(PE, lambda e, h=h: e.matmul(out=po_[0:nt, h * 128:(h + 1) * 128], lhsT=AM[0][0:nt, h, 0:nt],
                                             rhs=VG[0][0:nt, h * 128:(h + 1) * 128], start=True, stop=False),
                 r=[AM[1], VG[1]], w=[Bpo])
            S.op(PE, lambda e, h=h, c=c: e.matmul(out=po_[0:nt, h * 128:(h + 1) * 128],
                                                  lhsT=QTL[0][:, h % 2, c, 0:nt],
                                                  rhs=Sb[:, c, :], start=False, stop=True),
                 r=[QTL[1], B_Sb], w=[Bpo])
        chk("a8")
        pd, Bpd = ps()
        for h in range(4):
            r0, c = (h % 2) * 64, h // 2
            S.op(PE, lambda e, h=h, r0=r0, c=c: e.matmul(out=pd[r0:r0 + 64, c * 128:(c + 1) * 128],
                                                         lhsT=KTK[0][0:nt, h * 64:(h + 1) * 64],
                                                         rhs=VG[0][0:nt, h * 128:(h + 1) * 128], start=True, stop=True),
                 r=[KTK[1], VG[1]], w=[Bpd])
        S.op(DVE, lambda e: e.tensor_tensor(out=Sst[:, :, :], in0=pd[:, 0:256].rearrange("p (c v) -> p c v", c=2),
                                            in1=Sst[:, :, :], op=ALU.add), r=[Bpd, B_S], w=[B_S])
        for c in range(2):
            S.op(DVE, lambda e, c=c: e.tensor_scalar(out=Sst[:, c, :], in0=Sst[:, c, :], scalar1=EB[0][:, c, nt - 1:nt],
                                                     scalar2=None, op0=ALU.mult), r=[B_S, EB[1]], w=[B_S])
        S.op(POOL, lambda e: e.tensor_copy(out=Sb[:, :, :], in_=Sst[:, :, :]), r=[B_S], w=[B_Sb])
        if DEBUG and ti == DEBUG_TILE:
            def dbg(name, t, B, shape, dt=F32):
                d = dout("dbg_" + name, shape, dt)
                S.dma(QS, d, t, r=[B])
            dbg("sp", EA[0][:, :, :], EA[1], [128, 2, 128])
            dbg("cs", CS[0][:, :, :], CS[1], [128, 2, 128])
            dbg("ktok", KTK[0][:, :], KTK[1], [128, 256], BF16)
            dbg("vgb", VG[0][:, :], VG[1], [128, 512], BF16)
            dbg("qtl", QTL[0][:, 0, :, :], QTL[1], [128, 2, 128], BF16)
            dbg("ktl", KTL[0][:, :, :], KTL[1], [128, 2, 128], BF16)
            dbg("am", AM[0][:, :, :], AM[1], [128, 4, 128], BF16)
            dbg("S", Sst[:, :, :], B_S, [128, 2, 128])
            dbg("lrg", LR[0][:, :], LR[1], [16, 128])
        chk("a9")
        SG_, RG_, O1, OGB, OGT, MG = ssg[k], rsg[k], o1[k], ogb[k], ogT[k], mgt[k]
        for h in range(4):
            S.op(ACT, lambda e, h=h: e.activation(out=junk[0:nt, 0:128], in_=po_[0:nt, h * 128:(h + 1) * 128],
                                                  func=AF.Square, accum_out=SG_[0][0:nt, h:h + 1]), r=[Bpo], w=[SG_[1]])
        S.op(ACT, lambda e: e.activation(out=RG_[0][0:nt, :], in_=SG_[0][0:nt, :], func=AF.Ln, scale=1.0 / 128,
                                         bias=eps_c[0:nt, :]), r=[SG_[1]], w=[RG_[1]])
        S.op(ACT, lambda e: e.activation(out=RG_[0][0:nt, :], in_=RG_[0][0:nt, :], func=AF.Exp, scale=-0.5),
             r=[RG_[1]], w=[RG_[1]])
        S.op(DVE, lambda e: e.tensor_tensor(out=O1[0][0:nt, :], in0=po_[0:nt, :], in1=RSL[0][0:nt, :], op=ALU.mult),
             r=[Bpo, RSL[1]], w=[O1[1]])
        S.op(POOL, lambda e: e.tensor_tensor(out=OGB[0][0:nt, :, :],
                                             in0=O1[0][0:nt, :].rearrange("p (h v) -> p h v", h=4),
                                             in1=RG_[0][0:nt, :].unsqueeze(2).to_broadcast([nt, 4, 128]),
                                             op=ALU.mult), r=[O1[1], RG_[1]], w=[OGB[1]])
        p, Bp = ps()
        pv = bfv(p)
        for c in range(4):
            S.op(PE, lambda e, c=c: e.transpose(out=pv[:, c * 128:c * 128 + nt], in_=OGB[0][0:nt, c, :],
                                                identity=id_b[0:nt, 0:nt]), r=[OGB[1], B_idb], w=[Bp])
        S.op(ACT, lambda e: e.activation(out=OGT[0][:, :, 0:nt],
                                         in_=pv[:, 0:512].rearrange("p (c t) -> p c t", c=4)[:, :, 0:nt], func=AF.Copy),
             r=[Bp], w=[OGT[1]])
        for half in range(2):
            p, Bp = ps()
            for c in range(4):
                S.op(PE, lambda e, c=c, p=p, half=half: e.matmul(out=p[0:nt, :], lhsT=OGT[0][:, c, 0:nt],
                                                                 rhs=wgo[:, c, half * 512:(half + 1) * 512],
                                                                 start=(c == 0), stop=(c == 3)),
                     r=[OGT[1], B_wgo], w=[Bp])
            S.op(DVE, lambda e, p=p, half=half: e.tensor_tensor(out=MG[0][0:nt, half * 512:(half + 1) * 512],
                                                                in0=p[0:nt, :],
                                                                in1=SGG[0][0:nt, half * 512:(half + 1) * 512],
                                                                op=ALU.mult), r=[Bp, SGG[1]], w=[MG[1]])
        S.dma(QS, mg_scr[tok0:tok0 + nt, :], MG[0][0:nt, :], r=[MG[1]])

    S.op(DVE, lambda e: e.memset(Sst[:, :, :], 0.0), w=[B_S])
    S.op(POOL, lambda e: e.memset(Sb[:, :, :], 0.0), w=[B_Sb])
    if STOP == "0w":
        S.barrier()
        return nc
    tiles1 = [("m", 16, 0, 0)] + [("p", 128, i, 16 + i * 128) for i in range(NPT)] + \
             [("s", 8, j, LTOK + j * 8) for j in range(NSS)]
    ngroups = NPT // 4

    def run_gla(n):
        kind, nt, idx, tok0 = tiles1[n]
        if kind == "s":
            S.dma(QS, Sst[:, :, :], sgl[idx].rearrange("(c t) k v -> (t k) c v", t=2), w=[B_S])
            S.op(POOL, lambda e: e.tensor_copy(out=Sb[:, :, :], in_=Sst[:, :, :]), r=[B_S], w=[B_Sb])
        phase1a_gla(n, kind, nt, idx, tok0)
        if kind == "p" and idx == NPT - 1:
            S.dma(QS, ngl_p.rearrange("(c t) k v -> (t k) c v", t=2), Sst[:, :, :], r=[B_S])
        if kind == "s":
            S.dma(QS, ngl_s[idx].rearrange("(c t) k v -> (t k) c v", t=2), Sst[:, :, :], r=[B_S])

    for n, (kind, nt, idx, tok0) in enumerate(tiles1):
        phase1a(n, kind, nt, idx, tok0)
        if kind == "m":
            S.op(DVE, lambda e: e.tensor_copy(out=cref[:, 0, :], in_=carry[:, :]), r=[B_carry], w=[B_cref])
        if kind == "p" and idx % 4 == 3 and idx // 4 + 1 < ngroups:
            g = idx // 4 + 1
            S.op(DVE, lambda e, g=g: e.tensor_copy(out=cref[:, g, :], in_=carry[:, :]), r=[B_carry], w=[B_cref])
        if n >= 1:
            run_gla(n - 1)
        if n == 0 and STOP == "1am":
            run_gla(0)
            S.barrier()
            return nc
    run_gla(len(tiles1) - 1)

    S.barrier()
    if STOP == "1a":
        return nc
    A.cur = persist_mark
    rot["n"] = 6
    wfo, B_wfo = T([128, 4, D], BF16, "wfo")
    wo, B_wo = T([128, 8, D], BF16, "wo")
    for c in range(4):
        S.dma(QP, wfo[:, c, :], w_fo[c * 128:(c + 1) * 128, :], w=[B_wfo])
    for c in range(8):
        S.dma(QP, wo[:, c, :], w_o[c * 128:(c + 1) * 128, :], w=[B_wo])
    sgfl = T2([128, D], BF16, "sgfl")
    mgl = T2([128, D], BF16, "mgl")
    xl = T2([128, D], F32, "xl")
    m1 = T2([128, D], F32, "m1")
    mrg = T2([128, D], BF16, "mrg")
    mT = T2([128, 8, 128], BF16, "mT")
    x1 = T2([128, D], F32, "x1")
    ss2 = T2([128, 1], F32, "ss2")
    rs2 = T2([128, 1], F32, "rs2")
    h2 = T2([128, D], F32, "h2")
    h2T32 = T2([128, 8, 128], F32, "h2T32")
    h2Tb = T2([128, 8, 128], BF16, "h2Tb")
    lg = T2([128, 36], F32, "lg")
    gsm = T2([128, 16], F32, "gsm")
    em = T2([128, 32], F32, "em")
    top8 = T2([128, 8], F32, "top8")
    gt1 = T2([128, 32], F32, "gt1")
    gts = T2([128, 32], F32, "gts")
    attn_mark = A.cur
    KT, B_KT = T([128, 4, LTOK], BF16, "KT")
    V1a, B_V1 = T([128, NJ, 520], BF16, "V1a")
    call, B_call = T([128, NJ, 8], F32, "call")
    bias_all, B_bias = T([128, NJ, 8], F32, "biasall")
    QTg, B_QTg = T([128, 4, 512], BF16, "QTg")
    PT = T2([128, 512], BF16, "PT", 3)
    ofn, B_ofn = T([128, 4, 512], BF16, "ofn")
    rden = T2([128, 4], F32, "rden")
    ofT = T2([128, 4, 128], BF16, "ofT")
    S.op(DVE, lambda e: e.memset(call[:, :, :], 0.0), w=[B_call])
    mcount = {"i": 0}

    def merge_tile(OFT, nt, tok0, xsrc):
        k = mcount["i"] % 2
        mcount["i"] += 1
        SGL, MGL, XL, M1, MR, MT_, X1, SS2, RS2, H2, H32, H2B, LG, GS, EM, T8, G1, GT = (
            sgfl[k], mgl[k], xl[k], m1[k], mrg[k], mT[k], x1[k], ss2[k], rs2[k], h2[k], h2T32[k], h2Tb[k],
            lg[k], gsm[k], em[k], top8[k], gt1[k], gts[k])
        S.dma(QS, SGL[0][0:nt, :], sgf_scr[tok0:tok0 + nt, :], w=[SGL[1]])
        S.dma(QS, MGL[0][0:nt, :], mg_scr[tok0:tok0 + nt, :], w=[MGL[1]])
        S.dma(QS, XL[0][0:nt, :], xsrc, w=[XL[1]])
        for half in range(2):
            p, Bp = ps()
            for c in range(4):
                S.op(PE, lambda e, c=c, p=p, half=half: e.matmul(out=p[0:nt, :], lhsT=OFT[0][:, c, 0:nt],
                                                                 rhs=wfo[:, c, half * 512:(half + 1) * 512],
                                                                 start=(c == 0), stop=(c == 3)),
                     r=[OFT[1], B_wfo], w=[Bp])
            S.op(DVE, lambda e, p=p, half=half: e.tensor_tensor(out=M1[0][0:nt, half * 512:(half + 1) * 512],
                                                                in0=p[0:nt, :],
                                                                in1=SGL[0][0:nt, half * 512:(half + 1) * 512],
                                                                op=ALU.mult), r=[Bp, SGL[1]], w=[M1[1]])
        S.op(POOL, lambda e: e.tensor_tensor(out=MR[0][0:nt, :], in0=M1[0][0:nt, :], in1=MGL[0][0:nt, :], op=ALU.add),
             r=[M1[1], MGL[1]], w=[MR[1]])
        p, Bp = ps()
        pv = bfv(p)
        for c in range(8):
            S.op(PE, lambda e, c=c: e.transpose(out=pv[:, c * 128:c * 128 + nt], in_=MR[0][0:nt, c * 128:(c + 1) * 128],
                                                identity=id_b[0:nt, 0:nt]), r=[MR[1], B_idb], w=[Bp])
        S.op(ACT, lambda e: e.activation(out=MT_[0][:, :, 0:nt],
                                         in_=pv.rearrange("p (c t) -> p c t", c=8)[:, :, 0:nt], func=AF.Copy),
             r=[Bp], w=[MT_[1]])
        for half in range(2):
            p, Bp = ps()
            for c in range(8):
                S.op(PE, lambda e, c=c, p=p, half=half: e.matmul(out=p[0:nt, :], lhsT=MT_[0][:, c, 0:nt],
                                                                 rhs=wo[:, c, half * 512:(half + 1) * 512],
                                                                 start=(c == 0), stop=(c == 7)),
                     r=[MT_[1], B_wo], w=[Bp])
            S.op(DVE, lambda e, p=p, half=half: e.tensor_tensor(out=X1[0][0:nt, half * 512:(half + 1) * 512],
                                                                in0=p[0:nt, :],
                                                                in1=XL[0][0:nt, half * 512:(half + 1) * 512],
                                                                op=ALU.add), r=[Bp, XL[1]], w=[X1[1]])
        S.dma(QS, x1_scr[tok0:tok0 + nt, :], X1[0][0:nt, :], r=[X1[1]])
        rmsnorm_stats(X1[0][0:nt, :], X1[1], nt, SS2, RS2, D)
        S.op(DVE, lambda e: e.scalar_tensor_tensor(out=H2[0][0:nt, :], in0=X1[0][0:nt, :], scalar=RS2[0][0:nt, :],
                                                   in1=gffn_bc[0:nt, :], op0=ALU.mult, op1=ALU.mult),
             r=[X1[1], RS2[1], B_gffn], w=[H2[1]])
        for half in range(2):
            p, Bp = ps()
            for c in range(4):
                cc = half * 4 + c
                S.op(PE, lambda e, c=c, cc=cc, p=p: e.transpose(out=p[:, c * 128:c * 128 + nt],
                                                                in_=H2[0][0:nt, cc * 128:(cc + 1) * 128],
                                                                identity=id_f[0:nt, 0:nt]), r=[H2[1], B_idf], w=[Bp])
            S.op(ACT, lambda e, p=p, half=half: e.activation(
                out=H32[0][:, half * 4:(half + 1) * 4, 0:nt],
                in_=p[:, :].rearrange("p (c t) -> p c t", c=4)[:, :, 0:nt], func=AF.Copy), r=[Bp], w=[H32[1]])
        S.op(POOL, lambda e: e.tensor_copy(out=H2B[0][:, :, 0:nt], in_=H32[0][:, :, 0:nt]), r=[H32[1]], w=[H2B[1]])
        S.dma(QS, h2T_scr[:, :, tok0:tok0 + nt], H2B[0][:, :, 0:nt], r=[H2B[1]])
        p, Bp = ps()
        for c in range(8):
            S.op(PE, lambda e, c=c: e.matmul(out=p[0:nt, 0:36], lhsT=H32[0][:, c, 0:nt], rhs=wr[:, c, :],
                                             start=(c == 0), stop=(c == 7)), r=[H32[1], B_wr], w=[Bp])
        S.op(DVE, lambda e: e.tensor_tensor(out=LG[0][0:nt, :], in0=p[0:nt, 0:36], in1=rb_bc[0:nt, :], op=ALU.add),
             r=[Bp, B_rb], w=[LG[1]])
        g = GS[0]
        S.op(DVE, lambda e: e.tensor_reduce(out=g[0:nt, 0:1], in_=LG[0][0:nt, 0:4], axis=AX.X, op=ALU.max),
             r=[LG[1]], w=[GS[1]])
        S.op(DVE, lambda e: e.tensor_scalar(out=g[0:nt, 1:2], in0=g[0:nt, 0:1], scalar1=-1.0, scalar2=None, op0=ALU.mult),
             r=[GS[1]], w=[GS[1]])
        S.op(DVE, lambda e: e.tensor_scalar(out=g[0:nt, 8:12], in0=LG[0][0:nt, 0:4], scalar1=g[0:nt, 0:1], scalar2=None,
                                            op0=ALU.is_equal), r=[LG[1], GS[1]], w=[GS[1]])
        S.op(DVE, lambda e: e.tensor_scalar(out=g[0:nt, 12:16], in0=g[0:nt, 8:12], scalar1=-1.0, scalar2=1e30,
                                            op0=ALU.add, op1=ALU.mult), r=[GS[1]], w=[GS[1]])
        S.op(ACT, lambda e: e.activation(out=junk[0:nt, 0:4], in_=LG[0][0:nt, 0:4], func=AF.Exp, bias=g[0:nt, 1:2],
                                         accum_out=g[0:nt, 2:3]), r=[LG[1], GS[1]], w=[GS[1]])
        S.op(DVE, lambda e: e.reciprocal(out=g[0:nt, 3:4], in_=g[0:nt, 2:3]), r=[GS[1]], w=[GS[1]])
        S.op(DVE, lambda e: e.tensor_tensor(out=EM[0][0:nt, :].rearrange("p (g k) -> p g k", g=4),
                                            in0=LG[0][0:nt, 4:36].rearrange("p (g k) -> p g k", g=4),
                                            in1=g[0:nt, 12:16].unsqueeze(2).to_broadcast([nt, 4, 8]), op=ALU.add),
             r=[LG[1], GS[1]], w=[EM[1]])
        S.op(DVE, lambda e: e.max(out=T8[0][0:nt, :], in_=EM[0][0:nt, :]), r=[EM[1]], w=[T8[1]])
        S.op(DVE, lambda e: e.tensor_tensor(out=g[0:nt, 4:5], in0=T8[0][0:nt, 1:2], in1=T8[0][0:nt, 0:1], op=ALU.subtract),
             r=[T8[1], GS[1]], w=[GS[1]])
        S.op(ACT, lambda e: e.activation(out=g[0:nt, 5:6], in_=g[0:nt, 4:5], func=AF.Exp), r=[GS[1]], w=[GS[1]])
        S.op(DVE, lambda e: e.tensor_scalar(out=g[0:nt, 5:6], in0=g[0:nt, 5:6], scalar1=1.0, scalar2=None, op0=ALU.add),
             r=[GS[1]], w=[GS[1]])
        S.op(DVE, lambda e: e.reciprocal(out=g[0:nt, 5:6], in_=g[0:nt, 5:6]), r=[GS[1]], w=[GS[1]])
        S.op(DVE, lambda e: e.tensor_tensor(out=g[0:nt, 5:6], in0=g[0:nt, 5:6], in1=g[0:nt, 3:4], op=ALU.mult),
             r=[GS[1]], w=[GS[1]])
        S.op(DVE, lambda e: e.tensor_tensor(out=g[0:nt, 6:7], in0=g[0:nt, 3:4], in1=g[0:nt, 5:6], op=ALU.subtract),
             r=[GS[1]], w=[GS[1]])
        S.op(DVE, lambda e: e.tensor_scalar(out=G1[0][0:nt, :], in0=EM[0][0:nt, :], scalar1=T8[0][0:nt, 0:1],
                                            scalar2=g[0:nt, 5:6], op0=ALU.is_equal, op1=ALU.mult),
             r=[EM[1], T8[1], GS[1]], w=[G1[1]])
        S.op(DVE, lambda e: e.tensor_scalar(out=GT[0][0:nt, :], in0=EM[0][0:nt, :], scalar1=T8[0][0:nt, 1:2],
                                            scalar2=g[0:nt, 6:7], op0=ALU.is_equal, op1=ALU.mult),
             r=[EM[1], T8[1], GS[1]], w=[GT[1]])
        S.op(DVE, lambda e: e.tensor_tensor(out=GT[0][0:nt, :], in0=GT[0][0:nt, :], in1=G1[0][0:nt, :], op=ALU.add),
             r=[GT[1], G1[1]], w=[GT[1]])
        S.dma(QS, gat_scr[tok0:tok0 + nt, :], GT[0][0:nt, :], r=[GT[1]])

    po_banks = [(psb[6], psB[6]), (psb[7], psB[7])]

    def attention_group(gi, tiles, tok0):
        nts = [16 if j == 0 else 128 for j in tiles]
        nq = sum(nts)
        j0 = tiles[0]
        jlast = tiles[-1]
        ntq = len(tiles)
        S.dma(QS, KT[:, :, tok0:tok0 + nq], kT_scr[:, :, tok0:tok0 + nq], w=[B_KT])
        for qi, j in enumerate(tiles):
            t0 = tok0 + sum(nts[:qi])
            S.dma(QS, V1a[0:nts[qi], j, :], v1_scr[t0:t0 + nts[qi], :], w=[B_V1])
            S.dma(QS, call[0:nts[qi], j, :], c_scr[t0:t0 + nts[qi], :], w=[B_call])
        S.dma(QS, QTg[:, :, 0:nq], qT_scr[:, :, tok0:tok0 + nq], w=[B_QTg])
        nj = jlast + 1
        if j0 == 0:
            S.op(DVE, lambda e: e.tensor_scalar(out=bias_all[:, 0:1, :], in0=call[:, 0:1, :], scalar1=-1.0,
                                                scalar2=None, op0=ALU.mult), r=[B_call], w=[B_bias])
        else:
            S.op(DVE, lambda e: e.tensor_tensor(out=bias_all[:, 0:nj, :],
                                                in0=cref[:, gi:gi + 1, :].to_broadcast([128, nj, 8]),
                                                in1=call[:, 0:nj, :], op=ALU.subtract),
                 r=[B_cref, B_call], w=[B_bias])
        for h in range(8):
            r0, c = (h % 2) * 64, h // 2
            po, Bpo = po_banks[h % 2]
            po3 = po[:, 0:260].rearrange("p (q d) -> p q d", q=4)
            S.op(PE, lambda e, po=po: e.matmul(out=po[:, 0:260], lhsT=zer_b[0:1, 0:128], rhs=zer_b[0:1, 0:260],
                                               start=True, stop=True), r=[B_zer], w=[Bpo])
            for j in range(nj):
                nk = 16 if j == 0 else 128
                kt0 = 0 if j == 0 else 16 + (j - 1) * 128
                m = j - j0
                col0 = max(m, 0) * 128 if j0 > 0 else 0
                ncol = nq - col0
                p, Bp = ps()
                S.op(PE, lambda e, p=p, nk=nk, kt0=kt0, col0=col0, ncol=ncol: e.matmul(
                    out=p[0:nk, 0:ncol], lhsT=KT[r0:r0 + 64, c, kt0:kt0 + nk], rhs=QTg[r0:r0 + 64, c, col0:col0 + ncol],
                    start=True, stop=True), r=[B_KT, B_QTg], w=[Bp])
                P_ = PT[(h * 64 + j) % 3]
                S.op(ACT, lambda e, p=p, P_=P_, nk=nk, ncol=ncol, j=j: e.activation(
                    out=P_[0][0:nk, 0:ncol], in_=p[0:nk, 0:ncol], func=AF.Exp, scale=0.125,
                    bias=bias_all[0:nk, j, h:h + 1]), r=[Bp, B_bias], w=[P_[1]])
                if m >= 0:
                    nd = nts[m]
                    S.op(POOL, lambda e, P_=P_, nk=nk, nd=nd: e.tensor_tensor(
                        out=P_[0][0:nk, 0:nd], in0=P_[0][0:nk, 0:nd], in1=tri_b[0:nk, 0:nd], op=ALU.mult),
                        r=[P_[1], B_trib], w=[P_[1]])
                for qt in range(max(m, 0), ntq):
                    qc0 = sum(nts[:qt]) - col0
                    nqt = nts[qt]
                    S.op(PE, lambda e, P_=P_, nk=nk, qc0=qc0, nqt=nqt, qt=qt, j=j: e.matmul(
                        out=po3[0:nqt, qt, :], lhsT=P_[0][0:nk, qc0:qc0 + nqt], rhs=V1a[0:nk, j, h * 65:(h + 1) * 65],
                        start=False, stop=True, skip_group_check=True), r=[P_[1], B_V1], w=[Bpo])
            RD = rden[h % 2]
            nqt = nts[0]
            S.op(DVE, lambda e, po3=po3, RD=RD: e.reciprocal(out=RD[0][0:nqt, 0:ntq], in_=po3[0:nqt, 0:ntq, 64]),
                 r=[Bpo], w=[RD[1]])
            S.op(DVE, lambda e, po3=po3, RD=RD, h=h: e.tensor_tensor(
                out=ofn[0:nqt, 0:ntq, h * 64:(h + 1) * 64], in0=po3[0:nqt, 0:ntq, 0:64],
                in1=RD[0][0:nqt, 0:ntq].unsqueeze(2).to_broadcast([nqt, ntq, 64]), op=ALU.mult),
                r=[Bpo, RD[1]], w=[B_ofn])
        for qi, j in enumerate(tiles):
            nt = nts[qi]
            t0 = tok0 + sum(nts[:qi])
            OF = ofT[qi % 2]
            p, Bp = ps()
            pv = bfv(p)
            for c in range(4):
                S.op(PE, lambda e, c=c, qi=qi: e.transpose(out=pv[:, c * 128:c * 128 + nt],
                                                           in_=ofn[0:nt, qi, c * 128:(c + 1) * 128],
                                                           identity=id_b[0:nt, 0:nt]), r=[B_ofn, B_idb], w=[Bp])
            S.op(ACT, lambda e, OF=OF: e.activation(out=OF[0][:, :, 0:nt],
                                                    in_=pv[:, 0:512].rearrange("p (c t) -> p c t", c=4)[:, :, 0:nt],
                                                    func=AF.Copy), r=[Bp], w=[OF[1]])
            xsrc = meta if j == 0 else xp[(j - 1) * 128:j * 128, :]
            merge_tile(OF, nt, t0, xsrc)

    attention_group(0, [0], 0)
    for g in range(ngroups):
        attention_group(g, [1 + 4 * g + i for i in range(4)], 16 + 512 * g)

    S.barrier()
    if STOP == "1b":
        return nc
    A.cur = attn_mark
    NPGp = NPG
    idxc = T2([128, 1], I32, "idxc")
    lfpg, B_lfpg = T([128, 1024], F32, "lfpg")
    lfT, B_lfT = T([128, 8, NPGp], F32, "lfT")
    pre, B_pre = T([128, 8, NPGp], F32, "pre")
    tot_s, B_tot = T([128, 8, NPGp], F32, "tots")
    wexp, B_wexp = T([128, 8, NPGp], F32, "wexp")
    QTs, B_QTs = T([128, 4, 8], BF16, "QTs")
    KTs, B_KTs = T([128, 4, 8], BF16, "KTs")
    Qbd, B_Qbd = T([128, 4, 16], BF16, "Qbd")
    Vs, B_Vs = T([8, 520], BF16, "Vs")
    cs_s, B_css = T([8, 8], F32, "css")
    wnew, B_wnew = T([8, 8], F32, "wnew")
    kpg = T2([128, 512], F32, "kpg", 3)
    vpg = T2([128, 512], F32, "vpg", 3)
    kTp = T2([128, 4, 128], BF16, "kTp")
    vbp = T2([128, 512], BF16, "vbp")
    Pf = T2([128, 64], F32, "Pf")
    Pw = T2([128, 64], BF16, "Pw")
    Ofn_s, B_Ofn = T([64, 512], BF16, "Ofns")
    rd_s, B_rds = T([64, 1], F32, "rds")
    ofTs, B_ofTs = T([128, 4, 8], BF16, "ofTs")
    Of_b, B_Of = psb[6], psB[6]
    dn_b, B_dn = psb[7], psB[7]

    def sample_seq(sj):
        tok0 = LTOK + sj * 8
        S.dma(QS, QTs[:, :, :], qT_scr[:, :, tok0:tok0 + 8], w=[B_QTs])
        S.dma(QS, KTs[:, :, :], kT_scr[:, :, tok0:tok0 + 8], w=[B_KTs])
        S.dma(QS, Vs[:, :], v1_scr[tok0:tok0 + 8, :], w=[B_Vs])
        S.dma(QS, cs_s[:, :], c_scr[tok0:tok0 + 8, :], w=[B_css])
        IX = idxc[sj % 2]
        S.dma(QS, IX[0][0:NPG, :], ptcol[sj * NPG:(sj + 1) * NPG, :], w=[IX[1]])
        S.op(POOL, lambda e: e.memset(Qbd[:, :, :], 0.0), w=[B_Qbd])
        S.op(POOL, lambda e: e.tensor_copy(out=Qbd[0:64, :, 0:8], in_=QTs[0:64, :, :]), r=[B_QTs], w=[B_Qbd])
        S.op(POOL, lambda e: e.tensor_copy(out=Qbd[64:128, :, 8:16], in_=QTs[64:128, :, :]), r=[B_QTs], w=[B_Qbd])
        S.op(ACT, lambda e: e.activation(out=wnew[:, :], in_=cs_s[:, :], func=AF.Exp, scale=-1.0), r=[B_css], w=[B_wnew])
        S.idma(lfpg[0:NPG, :], clf, IX[0][0:NPG, 0:1], r=[IX[1]], w=[B_lfpg])
        lf3 = lfpg[:, :].rearrange("p (s h) -> p s h", h=8)
        for half in range(2):
            p, Bp = ps()
            for hh in range(4):
                h = half * 4 + hh
                S.op(PE, lambda e, h=h, hh=hh, p=p: e.transpose(out=p[:, hh * NPG:(hh + 1) * NPG], in_=lf3[0:NPG, :, h],
                                                                identity=id_f[0:NPG, 0:NPG]), r=[B_lfpg, B_idf], w=[Bp])
            S.op(ACT, lambda e, p=p, half=half: e.activation(
                out=lfT[:, half * 4:(half + 1) * 4, :], in_=p[:, 0:4 * NPG].rearrange("p (h g) -> p h g", h=4),
                func=AF.Copy), r=[Bp], w=[B_lfT])
        lfTf = lfT[:, :, :].rearrange("p h g -> p (h g)")
        nn = 8 * NPG
        pw_, Bpw = [], []
        pt_, Bpt = [], []
        for c0 in range(0, nn, 512):
            c1 = min(nn, c0 + 512)
            p, Bp = ps()
            S.op(PE, lambda e, p=p, c0=c0, c1=c1: e.matmul(out=p[:, 0:c1 - c0], lhsT=triR_f[:, :], rhs=lfTf[:, c0:c1],
                                                           start=True, stop=True), r=[B_triR, B_lfT], w=[Bp])
            p2, Bp2 = ps()
            S.op(PE, lambda e, p2=p2, c0=c0, c1=c1: e.matmul(out=p2[:, 0:c1 - c0], lhsT=ones_f[:, :], rhs=lfTf[:, c0:c1],
                                                             start=True, stop=True), r=[B_ones, B_lfT], w=[Bp2])
            totf = tot_s[:, :, :].rearrange("p h g -> p (h g)")
            S.op(DVE, lambda e, p2=p2, c0=c0, c1=c1, totf=totf: e.tensor_copy(out=totf[:, c0:c1], in_=p2[:, 0:c1 - c0]),
                 r=[Bp2], w=[B_tot])
            pw_.append((p, Bp, c0, c1))
        for h in range(8):
            S.op(DVE, lambda e, h=h: e.tensor_tensor_scan(out=pre[:, h, :], data0=ones_f[:, 0:NPG], data1=tot_s[:, h, :],
                                                          initial=0.0, op0=ALU.mult, op1=ALU.add),
                 r=[B_tot, B_ones], w=[B_pre])
        S.op(DVE, lambda e: e.tensor_tensor(out=tot_s[:, :, :], in0=pre[:, :, :],
                                            in1=pre[:, :, NPG - 1:NPG].to_broadcast([128, 8, NPG]), op=ALU.subtract),
             r=[B_pre], w=[B_tot])
        wexf = wexp[:, :, :].rearrange("p h g -> p (h g)")
        pref = tot_s[:, :, :].rearrange("p h g -> p (h g)")
        for (p, Bp, c0, c1) in pw_:
            S.op(DVE, lambda e, p=p, c0=c0, c1=c1: e.tensor_tensor(out=wexf[:, c0:c1], in0=p[:, 0:c1 - c0],
                                                                   in1=pref[:, c0:c1], op=ALU.subtract),
                 r=[Bp, B_tot], w=[B_wexp])
        S.op(ACT, lambda e: e.activation(out=wexf[:, :], in_=wexf[:, :], func=AF.Exp), r=[B_wexp], w=[B_wexp])
        S.op(PE, lambda e: e.matmul(out=Of_b[0:64, :], lhsT=zer_b[0:1, 0:64], rhs=zer_b[0:1, 0:512],
                                    start=True, stop=True), r=[B_zer], w=[B_Of])
        S.op(PE, lambda e: e.matmul(out=dn_b[0:64, 0:1], lhsT=zer_b[0:1, 0:64], rhs=zer_b[0:1, 0:1],
                                    start=True, stop=True), r=[B_zer], w=[B_dn])
        for pg in range(NPG):
            KP, VP, KTP, VB, PF, PW_ = kpg[pg % 3], vpg[pg % 3], kTp[pg % 2], vbp[pg % 2], Pf[pg % 2], Pw[pg % 2]
            col = sj * NPG + pg
            S.idma(KP[0][:, :], ck_rows, idx_all[:, col:col + 1], r=[B_idx], w=[KP[1]])
            S.idma(VP[0][:, :], cv_rows, idx_all[:, col:col + 1], r=[B_idx], w=[VP[1]])
            p, Bp = ps()
            for c in range(4):
                S.op(PE, lambda e, c=c, p=p: e.transpose(out=p[:, c * 128:(c + 1) * 128], in_=KP[0][:, c * 128:(c + 1) * 128],
                                                         identity=id_f[:, :]), r=[KP[1], B_idf], w=[Bp])
            S.op(ACT, lambda e, p=p: e.activation(out=KTP[0][:, :, :], in_=p[:, :].rearrange("p (c t) -> p c t", c=4),
                                                  func=AF.Copy), r=[Bp], w=[KTP[1]])
            S.op(DVE, lambda e: e.tensor_copy(out=VB[0][:, :], in_=VP[0][:, :]), r=[VP[1]], w=[VB[1]])
            p, Bp = ps()
            for c in range(4):
                S.op(PE, lambda e, c=c, p=p: e.matmul(out=p[:, c * 16:(c + 1) * 16], lhsT=KTP[0][:, c, :], rhs=Qbd[:, c, :],
                                                      start=True, stop=True), r=[KTP[1], B_Qbd], w=[Bp])
            S.op(ACT, lambda e, p=p: e.activation(out=PF[0][:, :], in_=p[:, 0:64], func=AF.Exp, scale=0.125),
                 r=[Bp], w=[PF[1]])
            S.op(DVE, lambda e, pg=pg: e.tensor_tensor(out=PW_[0][:, :].rearrange("p (h q) -> p h q", h=8),
                                                       in0=PF[0][:, :].rearrange("p (h q) -> p h q", h=8),
                                                       in1=wexp[:, :, pg:pg + 1].to_broadcast([128, 8, 8]), op=ALU.mult),
                 r=[PF[1], B_wexp], w=[PW_[1]])
            S.op(PE, lambda e: e.matmul(out=Of_b[0:64, :], lhsT=PW_[0][:, :], rhs=VB[0][:, :], start=False, stop=True,
                                        skip_group_check=True), r=[PW_[1], VB[1]], w=[B_Of])
            S.op(PE, lambda e: e.matmul(out=dn_b[0:64, 0:1], lhsT=PW_[0][:, :], rhs=ones_b[:, 0:1], start=False, stop=True,
                                        skip_group_check=True), r=[PW_[1], B_onesb], w=[B_dn])
        PF, PW_ = Pf[0], Pw[0]
        p, Bp = ps()
        for c in range(4):
            S.op(PE, lambda e, c=c, p=p: e.matmul(out=p[0:8, c * 16:(c + 1) * 16], lhsT=KTs[:, c, :], rhs=Qbd[:, c, :],
                                                  start=True, stop=True), r=[B_KTs, B_Qbd], w=[Bp])
        S.op(ACT, lambda e: e.activation(out=PF[0][0:8, :], in_=p[0:8, 0:64], func=AF.Exp, scale=0.125), r=[Bp], w=[PF[1]])
        S.op(DVE, lambda e: e.tensor_tensor(out=PF[0][0:8, :].rearrange("p (h q) -> p h q", h=8),
                                            in0=PF[0][0:8, :].rearrange("p (h q) -> p h q", h=8),
                                            in1=wnew[:, :].unsqueeze(2).to_broadcast([8, 8, 8]), op=ALU.mult),
             r=[PF[1], B_wnew], w=[PF[1]])
        S.op(DVE, lambda e: e.tensor_tensor(out=PW_[0][0:8, :].rearrange("p (h q) -> p h q", h=8),
                                            in0=PF[0][0:8, :].rearrange("p (h q) -> p h q", h=8),
                                            in1=tri_f[0:8, 0:8].unsqueeze(1).to_broadcast([8, 8, 8]), op=ALU.mult),
             r=[PF[1], B_tri], w=[PW_[1]])
        Vs3 = Vs[:, :].rearrange("p (h d) -> p h d", h=8)
        for h in range(8):
            S.op(PE, lambda e, h=h: e.matmul(out=Of_b[0:64, h * 64:(h + 1) * 64], lhsT=PW_[0][0:8, :], rhs=Vs3[:, h, 0:64],
                                             start=False, stop=True, skip_group_check=True), r=[PW_[1], B_Vs], w=[B_Of])
        S.op(PE, lambda e: e.matmul(out=dn_b[0:64, 0:1], lhsT=PW_[0][0:8, :], rhs=ones_b[0:8, 0:1], start=False, stop=True,
                                    skip_group_check=True), r=[PW_[1], B_onesb], w=[B_dn])
        S.op(DVE, lambda e: e.reciprocal(out=rd_s[:, :], in_=dn_b[0:64, 0:1]), r=[B_dn], w=[B_rds])
        S.op(DVE, lambda e: e.tensor_scalar(out=Ofn_s[:, :], in0=Of_b[0:64, :], scalar1=rd_s[:, 0:1], scalar2=None,
                                            op0=ALU.mult), r=[B_Of, B_rds], w=[B_Ofn])
        p, Bp = ps()
        for h in range(8):
            r0, c = (h % 2) * 64, h // 2
            S.op(PE, lambda e, h=h, r0=r0, c=c, p=p: e.matmul(out=p[r0:r0 + 64, c * 8:(c + 1) * 8],
                                                              lhsT=Ofn_s[0:64, h * 64:(h + 1) * 64],
                                                              rhs=id_b[0:64, h * 8:(h + 1) * 8], start=True, stop=True),
                 r=[B_Ofn, B_idb], w=[Bp])
        S.op(ACT, lambda e: e.activation(out=ofTs[:, :, :], in_=p[:, 0:32].rearrange("p (c t) -> p c t", c=4),
                                         func=AF.Copy), r=[Bp], w=[B_ofTs])
        merge_tile((ofTs, B_ofTs), 8, tok0, xs[sj * 8:(sj + 1) * 8, :])

    for sj in range(NSS):
        sample_seq(sj)

    S.barrier()
    if STOP == "1s":
        return nc
    A.cur = SB_BASE
    rot["n"] = 8
    gfin_bc, B_gfin = T([128, D], F32, "gfin")
    S.dma(QS, gfin_bc[:, :], g_fin.partition_broadcast(128), w=[B_gfin])
    eps2, B_eps2 = T([128, 1], F32, "eps2")
    S.op(DVE, lambda e: e.memset(eps2[:, :], EPS), w=[B_eps2])
    junk2 = A.t([128, 1024], BF16, "junk2")
    ntile2 = (TOK + 127) // 128
    nsb = max(1, ntile2 // 8)
    sb_tiles = [list(range(i * 8, (i + 1) * 8 if i < nsb - 1 else ntile2)) for i in range(nsb)]
    maxt = max(len(x) for x in sb_tiles)
    yacc, B_y = T([128, maxt, D], F32, "yacc")
    gat, B_gat = T([128, maxt, 32], F32, "gat")
    h2s, B_h2s = T([128, 8, maxt * 128], BF16, "h2s")
    NWB = 3
    wgs = T2([128, 8, 256], BF16, "wgs", NWB)
    wus = T2([128, 8, 256], BF16, "wus", NWB)
    wds = T2([128, 2, D], BF16, "wds", NWB)
    ytmp = T2([128, D], F32, "ytmp")

    def load_expert(gidx):
        ex_ = gidx % NE
        k_ = gidx % NWB
        S.dma(QP, wgs[k_][0][:, :, :], w_eg[ex_].rearrange("(c p) f -> p c f", p=128), w=[wgs[k_][1]])
        S.dma(QP, wus[k_][0][:, :, :], w_eu[ex_].rearrange("(c p) f -> p c f", p=128), w=[wus[k_][1]])
        S.dma(QP, wds[k_][0][:, :, :], w_ed[ex_].rearrange("(c p) f -> p c f", p=128), w=[wds[k_][1]])

    n_exp_total = NE * len(sb_tiles)
    load_expert(0)
    if n_exp_total > 1:
        load_expert(1)
    eg = T2([128, 2, 512], F32, "eg")
    sgs = T2([128, 2, 512], F32, "sgs")
    heT = T2([128, 2, 512], BF16, "heT")
    yo = T2([128, D], F32, "yo")
    ssf_all, B_ssf = T([128, 16], F32, "ssfall")
    rsf_all, B_rsf = T([128, 16], F32, "rsfall")
    S.op(DVE, lambda e: e.memset(ssf_all[:, :], 1.0), w=[B_ssf])
    B_ytile = [Buf(f"y{i}") for i in range(maxt)]
    ecount = 0
    for tl in sb_tiles:
        t0 = tl[0] * 128
        t1 = min(TOK, (tl[-1] + 1) * 128)
        ntk = t1 - t0
        for li, ti2 in enumerate(tl):
            a0 = ti2 * 128
            n = min(128, TOK - a0)
            S.dma(QS, yacc[0:n, li, :], x1_scr[a0:a0 + n, :], w=[B_ytile[li]])
            S.dma(QS, gat[0:n, li, :], gat_scr[a0:a0 + n, :], w=[B_gat])
        S.dma(QS, h2s[:, :, 0:ntk], h2T_scr[:, :, t0:t1], w=[B_h2s])
        for ex in range(NE):
            k = ecount % NWB
            if ecount + 2 < n_exp_total:
                load_expert(ecount + 2)
            ecount += 1
            WG, WU, WD = wgs[k], wus[k], wds[k]
            for n0 in range(0, ntk, 512):
                nn_ = min(512, ntk - n0)
                kk = (n0 // 512) % 2
                EG, SGS, HE = eg[kk], sgs[kk], heT[kk]
                banks = [ps() for _ in range(4)]
                for c in range(8):
                    for bi, (wsb, fc) in enumerate(((WG, 0), (WG, 1), (WU, 0), (WU, 1))):
                        p, Bp = banks[bi]
                        S.op(PE, lambda e, p=p, wsb=wsb, fc=fc, c=c: e.matmul(
                            out=p[:, 0:nn_], lhsT=wsb[0][:, c, fc * 128:(fc + 1) * 128], rhs=h2s[:, c, n0:n0 + nn_],
                            start=(c == 0), stop=(c == 7)), r=[wsb[1], B_h2s], w=[Bp])
                for fc in range(2):
                    p, Bp = banks[fc]
                    S.op(ACT, lambda e, p=p, fc=fc: e.activation(out=SGS[0][:, fc, 0:nn_], in_=p[:, 0:nn_], func=AF.Silu),
                         r=[Bp], w=[SGS[1]])
                for fc in range(2):
                    p, Bp = banks[2 + fc]
                    S.op(DVE, lambda e, p=p, fc=fc: e.tensor_tensor(out=HE[0][:, fc, 0:nn_], in0=p[:, 0:nn_],
                                                                    in1=SGS[0][:, fc, 0:nn_], op=ALU.mult),
                         r=[Bp, SGS[1]], w=[HE[1]])
                for q0 in range(0, nn_, 128):
                    nq_ = min(128, nn_ - q0)
                    li = (n0 + q0) // 128
                    for half in range(2):
                        p, Bp = ps()
                        for fc in range(2):
                            S.op(PE, lambda e, p=p, fc=fc, half=half: e.matmul(
                                out=p[0:nq_, :], lhsT=HE[0][:, fc, q0:q0 + nq_], rhs=WD[0][:, fc, half * 512:(half + 1) * 512],
                                start=(fc == 0), stop=(fc == 1)), r=[HE[1], WD[1]], w=[Bp])
                        if (q0 // 128) % 2 == 0:
                            S.op(DVE, lambda e, p=p, half=half, li=li: e.scalar_tensor_tensor(
                                out=yacc[0:nq_, li, half * 512:(half + 1) * 512], in0=p[0:nq_, :],
                                scalar=gat[0:nq_, li, ex:ex + 1], in1=yacc[0:nq_, li, half * 512:(half + 1) * 512],
                                op0=ALU.mult, op1=ALU.add), r=[Bp, B_gat, B_ytile[li]], w=[B_ytile[li]])
                        else:
                            YT = ytmp[half]
                            S.op(ACT, lambda e, p=p, YT=YT, li=li: e.activation(
                                out=YT[0][0:nq_, 0:512], in_=p[0:nq_, :], func=AF.Identity,
                                scale=gat[0:nq_, li, ex:ex + 1]), r=[Bp, B_gat], w=[YT[1]])
                            S.op(POOL, lambda e, YT=YT, half=half, li=li: e.tensor_tensor(
                                out=yacc[0:nq_, li, half * 512:(half + 1) * 512], in0=YT[0][0:nq_, 0:512],
                                in1=yacc[0:nq_, li, half * 512:(half + 1) * 512], op=ALU.add),
                                r=[YT[1], B_ytile[li]], w=[B_ytile[li]])
        infos = []
        for li, ti2 in enumerate(tl):
            a0 = ti2 * 128
            infos.append((li, a0, min(128, TOK - a0)))
        for li, a0, n in infos:
            S.op(ACT, lambda e, li=li, n=n: e.activation(out=junk2[0:n, :], in_=yacc[0:n, li, :], func=AF.Square,
                                                         accum_out=ssf_all[0:n, li:li + 1]), r=[B_ytile[li]], w=[B_ssf])
        S.op(ACT, lambda e: e.activation(out=rsf_all[:, 0:len(tl)], in_=ssf_all[:, 0:len(tl)], func=AF.Ln, scale=1.0 / D,
                                         bias=eps2[:, :]), r=[B_ssf, B_eps2], w=[B_rsf])
        S.op(ACT, lambda e: e.activation(out=rsf_all[:, 0:len(tl)], in_=rsf_all[:, 0:len(tl)], func=AF.Exp, scale=-0.5),
             r=[B_rsf], w=[B_rsf])
        for li, a0, n in infos:
            YO = yo[li % 2]
            S.op(DVE, lambda e, li=li, n=n, YO=YO: e.scalar_tensor_tensor(
                out=YO[0][0:n, :], in0=yacc[0:n, li, :], scalar=rsf_all[0:n, li:li + 1], in1=gfin_bc[0:n, :],
                op0=ALU.mult, op1=ALU.mult), r=[B_ytile[li], B_rsf, B_gfin], w=[YO[1]])
            lo, hi = max(a0, 16), min(a0 + n, LTOK)
            if hi > lo:
                S.dma(QS, y_p[lo - 16:hi - 16, :], YO[0][lo - a0:hi - a0, :], r=[YO[1]])
            lo, hi = max(a0, LTOK), min(a0 + n, TOK)
            if hi > lo:
                S.dma(QS, y_s[lo - LTOK:hi - LTOK, :], YO[0][lo - a0:hi - a0, :], r=[YO[1]])
    S.barrier()
    return nc


_CACHE = {}


def kernel(x_prompt, x_sample, cache_k, cache_v, cache_log_f, state_gla, page_table,
           meta_tokens, norm_mix_g, w_in, fox_f_bias, gla_w_a2, gla_b_a, gla_norm_g,
           w_fox_out, w_gla_out, w_o, norm_ffn_g, w_group_router, b_group_router,
           w_expert_router, b_expert_router, w_expert_gate, w_expert_up, w_expert_down,
           norm_final_g):
    f = lambda a: np.ascontiguousarray(np.asarray(a), dtype=np.float32)
    x_prompt, x_sample = f(x_prompt), f(x_sample)
    B, SEQ, _ = x_prompt.shape
    DB, DS, _ = x_sample.shape
    NC = 8
    NPT = SEQ // 128
    NSS = DB // NC
    NPG = page_table.shape[1]
    NPHYS = cache_k.shape[1]
    LTOK = 16 + SEQ
    key = (NPT, NSS, NPG, NPHYS)
    if key not in _CACHE:
        _CACHE[key] = build(*key)
    nc = _CACHE[key]
    ck = f(cache_k)[0].reshape(NPHYS, 128, 512)
    cv = f(cache_v)[0].reshape(NPHYS, 128, 512)
    clf = f(cache_log_f)[0].reshape(NPHYS, 1024)
    pt = np.ascontiguousarray(np.asarray(page_table), dtype=np.int32)
    shared = dict(
        ck=ck, cv=cv, clf=clf, meta=f(meta_tokens), g_mix=f(norm_mix_g)[0], w_in=f(w_in)[0],
        f_bias=f(fox_f_bias)[0], w_a2=f(gla_w_a2)[0], b_a=f(gla_b_a)[0], g_gla=f(gla_norm_g)[0],
        w_fo=f(w_fox_out)[0], w_go=f(w_gla_out)[0], w_o=f(w_o)[0], g_ffn=f(norm_ffn_g)[0],
        w_gr=f(w_group_router)[0], b_gr=f(b_group_router)[0], w_er=f(w_expert_router)[0],
        b_er=f(b_expert_router)[0], w_eg=f(w_expert_gate)[0], w_eu=f(w_expert_up)[0],
        w_ed=f(w_expert_down)[0], g_fin=f(norm_final_g))
    in_maps = []
    for c in range(NC):
        m = dict(shared)
        m["xp"] = x_prompt[c]
        m["xs"] = x_sample[c * NSS:(c + 1) * NSS].reshape(NSS * DS, D)
        m["sgl"] = f(state_gla)[0, c * NSS:(c + 1) * NSS]
        ptc = pt[c * NSS:(c + 1) * NSS].reshape(1, NSS * NPG)
        m["ptab"] = ptc
        m["ptcol"] = np.ascontiguousarray(ptc.reshape(NSS * NPG, 1))
        in_maps.append(m)
    res = run_bass_kernel_spmd(nc, in_maps, core_ids=list(range(NC))).results
    cat = lambda k: np.stack([np.asarray(r[k]) for r in res])
    y_prompt = cat("y_p").reshape(B, SEQ, D)
    y_sample = cat("y_s").reshape(DB, DS, D)
    nk_p = cat("nk_p").reshape(1, B, LTOK, 8, 64)
    nv_p = cat("nv_p").reshape(1, B, LTOK, 8, 64)
    nlf_p = cat("nlf_p").reshape(1, B, LTOK, 8)
    ngl_p = cat("ngl_p").reshape(1, B, 4, 64, 128)
    nk_s = cat("nk_s").reshape(1, DB, DS, 8, 64)
    nv_s = cat("nv_s").reshape(1, DB, DS, 8, 64)
    nlf_s = cat("nlf_s").reshape(1, DB, DS, 8)
    ngl_s = cat("ngl_s").reshape(1, DB, 4, 64, 128)
    return (y_prompt.astype(np.float32), y_sample.astype(np.float32), nk_p, nv_p, nlf_p, ngl_p,
            nk_s, nv_s, nlf_s, ngl_s)
```

```python
import numpy as np
import concourse.bass as bass
import concourse.mybir as mybir
from concourse.bass_utils import run_bass_kernel_spmd

F32 = mybir.dt.float32
BF16 = mybir.dt.bfloat16
I32 = mybir.dt.int32
AF = mybir.ActivationFunctionType
ALU = mybir.AluOpType
AX = mybir.AxisListType
ET = mybir.EngineType

D = 1024
PW = 5144
C_QF, C_KF, C_VF, C_FF, C_QG, C_KG, C_VG, C_LR, C_RG, C_GF, C_GG = (
    0, 512, 1024, 1536, 1544, 1800, 2056, 2568, 2584, 3096, 4120)
EPS = 1e-6
NE = 32
NSD = 12
SB_BASE = 16640
DEBUG = False
STOP = ''
DEBUG_TILE = 0


class StopBuild(Exception):
    pass


class Buf:
    __slots__ = ("w", "r", "name", "excl")

    def __init__(self, name="", excl=False):
        self.w = None
        self.r = {}
        self.name = name
        self.excl = excl


class Eng:
    def __init__(self, name, e, sem):
        self.name, self.e, self.sem, self.cnt, self.waited = name, e, sem, 0, {}


class Queue:
    def __init__(self, E, sems):
        self.E, self.sems, self.k = E, sems, 0


class Sched:
    def __init__(self, nc):
        self.nc = nc
        mk = lambda n: nc.alloc_semaphore(n)
        self.PE = Eng("pe", nc.tensor, mk("s_pe"))
        self.ACT = Eng("act", nc.scalar, mk("s_act"))
        self.DVE = Eng("dve", nc.vector, mk("s_dve"))
        self.POOL = Eng("pool", nc.gpsimd, mk("s_pool"))
        self.SP = Eng("sp", nc.sync, mk("s_sp"))
        self.engs = [self.PE, self.ACT, self.DVE, self.POOL, self.SP]
        self.QS = Queue(self.SP, [mk(f"s_qs{i}") for i in range(NSD)])
        self.QP = Queue(self.POOL, [mk(f"s_qp{i}") for i in range(NSD)])

    def _wait(self, E, need):
        for s, v in need.items():
            if E is self.PE and s is self.PE.sem:
                continue
            if E.waited.get(s, 0) < v:
                E.e.wait_ge(s, v)
                E.waited[s] = v

    @staticmethod
    def _need(r, w, own=None):
        need = {}

        def add(tok):
            if tok is None:
                return
            s, v = tok
            if need.get(s, 0) < v:
                need[s] = v
        for b in r:
            add(b.w)
            if b.excl:
                for s, v in b.r.items():
                    if s is not own:
                        add((s, v))
        for b in w:
            add(b.w)
            for s, v in b.r.items():
                add((s, v))
        return need

    @staticmethod
    def _mark(tok, r, w):
        s, v = tok
        for b in r:
            if b.r.get(s, 0) < v:
                b.r[s] = v
        for b in w:
            b.w = tok
            b.r = {}

    def op(self, E, fn, r=(), w=()):
        self._wait(E, self._need(r, w, E.sem))
        ins = fn(E.e)
        E.cnt += 1
        ins.then_inc(E.sem, 1)
        self._mark((E.sem, E.cnt), r, w)
        return ins

    def dma(self, Q, out, in_, r=(), w=(), **kw):
        E = Q.E
        k = Q.k
        Q.k += 1
        s = Q.sems[k % NSD]
        base = 16 * (k // NSD)
        need = self._need(r, w)
        if base > 0 and need.get(s, 0) < base:
            need[s] = base
        self._wait(E, need)
        ins = E.e.dma_start(out=out, in_=in_, **kw)
        ins.then_inc(s, 16)
        self._mark((s, base + 16), r, w)
        return ins

    def idma(self, out, in_, idx_ap, r=(), w=()):
        Q = self.QP
        E = Q.E
        k = Q.k
        Q.k += 1
        s = Q.sems[k % NSD]
        base = 16 * (k // NSD)
        need = self._need(r, w)
        if base > 0 and need.get(s, 0) < base:
            need[s] = base
        self._wait(E, need)
        ins = E.e.indirect_dma_start(out=out, out_offset=None, in_=in_,
                                     in_offset=bass.IndirectOffsetOnAxis(idx_ap, 0))
        ins.then_inc(s, 16)
        self._mark((s, base + 16), r, w)
        return ins

    def barrier(self):
        toks = {}
        for E in self.engs:
            if E.cnt:
                toks[E.sem] = E.cnt
        for Q in (self.QS, self.QP):
            for i, s in enumerate(Q.sems):
                n = (Q.k - i + NSD - 1) // NSD if Q.k > i else 0
                if n:
                    toks[s] = 16 * n
        for E in self.engs:
            need = {s: v for s, v in toks.items() if s is not E.sem}
            for s, v in need.items():
                if E.waited.get(s, 0) < v:
                    E.e.wait_ge(s, v)
                    E.waited[s] = v


class Alloc:
    def __init__(self, nc):
        self.nc, self.cur, self.n = nc, SB_BASE, 0

    def t(self, shape, dt, name=None):
        isz = {F32: 4, BF16: 2, I32: 4}[dt]
        per = int(np.prod(shape[1:])) * isz
        per = (per + 63) // 64 * 64
        self.n += 1
        h = self.nc.alloc_sbuf_tensor_at(f"{name or 't'}_{self.n}", list(shape), dt, offset=self.cur)
        self.cur += per
        assert self.cur <= 229000, f"SBUF overflow {self.cur}"
        return h


def build(NPT, NSS, NPG, NPHYS):
    nc = bass.Bass("TRN2", target_bir_lowering=False)
    st = {}
    try:
        _build(nc, st, NPT, NSS, NPG, NPHYS)
    except StopBuild:
        st['S'].barrier()
    return nc


def _build(nc, st, NPT, NSS, NPG, NPHYS):
    LTOK = 16 + NPT * 128
    TOK = LTOK + NSS * 8
    NJ = NPT + 1

    def din(name, shape, dt=F32):
        return nc.dram_tensor(name, list(shape), dt, kind="ExternalInput").ap()

    def dout(name, shape, dt=F32):
        return nc.dram_tensor(name, list(shape), dt, kind="ExternalOutput").ap()

    def dscr(name, shape, dt=F32):
        return nc.dram_tensor(name, list(shape), dt, kind="Internal").ap()

    xp = din("xp", [NPT * 128, D])
    xs = din("xs", [NSS * 8, D])
    ck = din("ck", [NPHYS, 128, 512])
    cv = din("cv", [NPHYS, 128, 512])
    clf = din("clf", [NPHYS, 1024])
    sgl = din("sgl", [NSS, 4, 64, 128])
    ptab = din("ptab", [1, NSS * NPG], I32)
    ptcol = din("ptcol", [NSS * NPG, 1], I32)
    meta = din("meta", [16, D])
    g_mix = din("g_mix", [D])
    w_in = din("w_in", [D, PW])
    f_bias = din("f_bias", [8])
    w_a2 = din("w_a2", [16, 256])
    b_a = din("b_a", [256])
    g_gla = din("g_gla", [512])
    w_fo = din("w_fo", [512, D])
    w_go = din("w_go", [512, D])
    w_o = din("w_o", [D, D])
    g_ffn = din("g_ffn", [D])
    w_gr = din("w_gr", [D, 4])
    b_gr = din("b_gr", [4])
    w_er = din("w_er", [D, 32])
    b_er = din("b_er", [32])
    w_eg = din("w_eg", [NE, D, 256])
    w_eu = din("w_eu", [NE, D, 256])
    w_ed = din("w_ed", [NE, 256, D])
    g_fin = din("g_fin", [D])

    y_p = dout("y_p", [NPT * 128, D])
    y_s = dout("y_s", [NSS * 8, D])
    nk_p = dout("nk_p", [LTOK, 512])
    nv_p = dout("nv_p", [LTOK, 512])
    nlf_p = dout("nlf_p", [LTOK, 8])
    ngl_p = dout("ngl_p", [4, 64, 128])
    nk_s = dout("nk_s", [NSS * 8, 512])
    nv_s = dout("nv_s", [NSS * 8, 512])
    nlf_s = dout("nlf_s", [NSS * 8, 8])
    ngl_s = dout("ngl_s", [NSS, 4, 64, 128])

    qT_scr = dscr("qT_scr", [128, 4, TOK], BF16)
    kT_scr = dscr("kT_scr", [128, 4, TOK], BF16)
    v1_scr = dscr("v1_scr", [TOK, 520], BF16)
    c_scr = dscr("c_scr", [TOK, 8])
    sgf_scr = dscr("sgf_scr", [TOK, D], BF16)
    mg_scr = dscr("mg_scr", [TOK, D], BF16)
    x1_scr = dscr("x1_scr", [TOK, D])
    h2T_scr = dscr("h2T_scr", [128, 8, TOK], BF16)
    gat_scr = dscr("gat_scr", [TOK, 32])

    _lp = nc.allow_low_precision(reason="bf16 matmul operands by design; fp32 accumulation")
    _lp.__enter__()
    S = Sched(nc)
    st['S'] = S
    PE, ACT, DVE, POOL, SP = S.PE, S.ACT, S.DVE, S.POOL, S.SP
    QS, QP = S.QS, S.QP
    A = Alloc(nc)

    psb = [nc.alloc_psum_tensor(f"ps{i}", [128, 512], F32) for i in range(8)]
    psB = [Buf(f"ps{i}", excl=True) for i in range(8)]
    rot = {"i": 0, "n": 8}

    def ps():
        i = rot["i"] % rot["n"]
        rot["i"] += 1
        return psb[i], psB[i]

    def bfv(p):
        return p[:, :].bitcast(BF16)

    cnt = {"i": 0}

    def T(shape, dt, name="t"):
        return A.t(shape, dt, name), Buf(name)

    def T2(shape, dt, name="t", n=2):
        return [T(shape, dt, f"{name}{i}") for i in range(n)]

    ones_f, B_ones = T([128, 128], F32, "ones")
    tri_f, B_tri = T([128, 128], F32, "tri")
    triR_f, B_triR = T([128, 128], F32, "triR")
    id_f, B_idf = T([128, 128], F32, "idf")
    id_b, B_idb = T([128, 128], BF16, "idb")
    tri_b, B_trib = T([128, 128], BF16, "trib")
    ones_b, B_onesb = T([128, 128], BF16, "onesb")
    zer_b, B_zer = T([128, 512], BF16, "zer")
    junk = A.t([128, 1024], BF16, "junk")

    S.op(POOL, lambda e: e.memset(ones_f[:, :], 1.0), w=[B_ones])
    S.op(POOL, lambda e: e.memset(zer_b[:, :], 0.0), w=[B_zer])
    S.op(POOL, lambda e: e.affine_select(out=tri_f[:, :], in_=ones_f[:, :], pattern=[[1, 128]],
                                         compare_op=ALU.is_ge, fill=0.0, base=0, channel_multiplier=-1),
         r=[B_ones], w=[B_tri])
    S.op(POOL, lambda e: e.affine_select(out=triR_f[:, :], in_=ones_f[:, :], pattern=[[-1, 128]],
                                         compare_op=ALU.is_ge, fill=0.0, base=-1, channel_multiplier=1),
         r=[B_ones], w=[B_triR])
    S.op(POOL, lambda e: e.affine_select(out=id_f[:, :], in_=ones_f[:, :], pattern=[[1, 128]],
                                         compare_op=ALU.is_equal, fill=0.0, base=0, channel_multiplier=-1),
         r=[B_ones], w=[B_idf])
    S.op(POOL, lambda e: e.tensor_copy(out=id_b[:, :], in_=id_f[:, :]), r=[B_idf], w=[B_idb])
    S.op(POOL, lambda e: e.tensor_copy(out=tri_b[:, :], in_=tri_f[:, :]), r=[B_tri], w=[B_trib])
    S.op(POOL, lambda e: e.tensor_copy(out=ones_b[:, :], in_=ones_f[:, :]), r=[B_ones], w=[B_onesb])

    def bc_load(src1d, n, name):
        t, b = T([128, n], F32, name)
        S.dma(QS, t[:, :], src1d.partition_broadcast(128), w=[b])
        return t, b

    gmix_bc, B_gmix = bc_load(g_mix, D, "gmix")
    gffn_bc, B_gffn = bc_load(g_ffn, D, "gffn")
    ggla_bc, B_ggla = bc_load(g_gla, 512, "ggla")
    fb_bc, B_fb = bc_load(f_bias, 8, "fb")
    rb_bc, B_rb = T([128, 36], F32, "rb")
    S.dma(QS, rb_bc[:, 0:4], b_gr.partition_broadcast(128), w=[B_rb])
    S.dma(QS, rb_bc[:, 4:36], b_er.partition_broadcast(128), w=[B_rb])
    nba, B_nba = T([128, 2], F32, "nba")
    with nc.allow_non_contiguous_dma(reason="tiny bias column load"):
        S.dma(QS, nba[:, :], b_a.rearrange("(c p) -> p c", p=128), w=[B_nba])
    S.op(DVE, lambda e: e.tensor_scalar(out=nba[:, :], in0=nba[:, :], scalar1=-1.0, scalar2=None,
                                        op0=ALU.mult), r=[B_nba], w=[B_nba])
    wa2, B_wa2 = T([16, 256], F32, "wa2")
    S.dma(QS, wa2[:, :], w_a2, w=[B_wa2])
    wr, B_wr = T([128, 8, 36], F32, "wr")
    S.dma(QS, wr[:, :, 0:4], w_gr.rearrange("(c p) n -> p c n", p=128), w=[B_wr])
    S.dma(QS, wr[:, :, 4:36], w_er.rearrange("(c p) n -> p c n", p=128), w=[B_wr])
    NPGT = NSS * NPG
    ptb_i, B_ptbi = T([128, NPGT], I32, "ptbi")
    S.dma(QS, ptb_i[:, :], ptab[0].partition_broadcast(128), w=[B_ptbi])
    slot_i, B_sloti = T([128, 1], I32, "sloti")
    S.op(POOL, lambda e: e.iota(out=slot_i[:, :], pattern=[[0, 1]], base=0, channel_multiplier=1), w=[B_sloti])
    slot_f, B_slotf = T([128, 1], F32, "slotf")
    S.op(DVE, lambda e: e.tensor_copy(out=slot_f[:, :], in_=slot_i[:, :]), r=[B_sloti], w=[B_slotf])
    ptb_f, B_ptbf = T([128, NPGT], F32, "ptbf")
    S.op(DVE, lambda e: e.tensor_copy(out=ptb_f[:, :], in_=ptb_i[:, :]), r=[B_ptbi], w=[B_ptbf])
    S.op(DVE, lambda e: e.tensor_scalar(out=ptb_f[:, :], in0=ptb_f[:, :], scalar1=128.0, scalar2=slot_f[:, 0:1],
                                        op0=ALU.mult, op1=ALU.add), r=[B_ptbf, B_slotf], w=[B_ptbf])
    idx_all, B_idx = T([128, NPGT], I32, "idxall")
    S.op(DVE, lambda e: e.tensor_copy(out=idx_all[:, :], in_=ptb_f[:, :]), r=[B_ptbf], w=[B_idx])
    ck_rows = ck.rearrange("n s f -> (n s) f")
    cv_rows = cv.rearrange("n s f -> (n s) f")
    cref, B_cref = T([128, NPT // 4 + 2, 8], F32, "cref")
    carry, B_carry = T([128, 8], F32, "carry")
    S.op(DVE, lambda e: e.memset(carry[:, :], 0.0), w=[B_carry])
    wgo, B_wgo = T([128, 4, D], BF16, "wgo")
    for c in range(4):
        S.dma(QP, wgo[:, c, :], w_go[c * 128:(c + 1) * 128, :], w=[B_wgo])
    eps_c, B_eps = T([128, 1], F32, "eps")
    S.op(DVE, lambda e: e.memset(eps_c[:, :], EPS), w=[B_eps])
    one_c, B_one = T([128, 1], F32, "onec")
    S.op(DVE, lambda e: e.memset(one_c[:, :], 1.0), w=[B_one])
    hm = []
    for par in range(2):
        t_, b_ = T([128, 1], F32, f"hm{par}")
        S.op(DVE, lambda e, t_=t_: e.memset(t_[:, :], 0.0), w=[b_])
        S.op(DVE, lambda e, t_=t_, par=par: e.memset(t_[par * 64:(par + 1) * 64, :], 0.125), w=[b_])
        hm.append((t_, b_))
    persist_mark = A.cur

    if STOP == "0":
        S.barrier()
        return nc
    win, B_win = T([128, 8, PW], BF16, "win")
    w_in3 = w_in.rearrange("(c p) n -> p c n", p=128)
    for c0 in range(0, PW, 2048):
        c1 = min(PW, c0 + 2048)
        S.dma(QP, win[:, :, c0:c1], w_in3[:, :, c0:c1], w=[B_win])

    xt = T2([128, D], F32, "xt")
    ssq = T2([128, 1], F32, "ssq")
    rstd = T2([128, 1], F32, "rstd")
    hb = T2([128, D], BF16, "hb")
    hT = T2([128, 8, 128], BF16, "hT")
    qTt = T2([128, 4, 128], BF16, "qTt")
    kTt = T2([128, 4, 128], BF16, "kTt")
    lrgT = T2([16, 128], F32, "lrgT")
    kout = T2([128, 512], F32, "kout")
    vout = T2([128, 512], F32, "vout")
    v1t = T2([128, 8, 65], BF16, "v1t")
    vgb = T2([128, 512], BF16, "vgb")
    etmp = T2([128, D], F32, "etmp", 3)
    rsil = T2([128, 512], F32, "rsil")
    sgf = T2([128, D], BF16, "sgf")
    sgg = T2([128, D], BF16, "sgg")
    lft = T2([128, 8], F32, "lft")
    ltmp = T2([128, 8], F32, "ltmp")
    ctile = T2([128, 8], F32, "ctile")
    ea = T2([128, 2, 128], F32, "ea")
    csum = T2([128, 2, 128], F32, "csum")
    ebt = T2([128, 2, 128], F32, "ebt")
    enbt = T2([128, 2, 128], F32, "enbt")
    qtl = T2([128, 2, 2, 128], BF16, "qtl")
    ktl = T2([128, 2, 128], BF16, "ktl")
    ktok = T2([128, 256], BF16, "ktok")
    Am = T2([128, 4, 128], BF16, "Am")
    Sst, B_S = T([128, 2, 128], F32, "S")
    Sb, B_Sb = T([128, 2, 128], BF16, "Sb")
    ssg = T2([128, 4], F32, "ssg")
    rsg = T2([128, 4], F32, "rsg")
    o1 = T2([128, 512], F32, "o1")
    ogb = T2([128, 4, 128], BF16, "ogb")
    ogT = T2([128, 4, 128], BF16, "ogT")
    mgt = T2([128, D], BF16, "mgt")
    for i in range(2):
        S.op(POOL, lambda e, i=i: e.memset(v1t[i][0][:, :, 64:65], 1.0), w=[v1t[i][1]])

    def rmsnorm_stats(x_ap, Bx, nt, ss_, rs_, n):
        S.op(ACT, lambda e: e.activation(out=junk[0:nt, 0:n], in_=x_ap, func=AF.Square,
                                         accum_out=ss_[0][0:nt, :]), r=[Bx], w=[ss_[1]])
        S.op(ACT, lambda e: e.activation(out=rs_[0][0:nt, :], in_=ss_[0][0:nt, :], func=AF.Ln,
                                         scale=1.0 / n, bias=eps_c[0:nt, :]), r=[ss_[1], B_eps], w=[rs_[1]])
        S.op(ACT, lambda e: e.activation(out=rs_[0][0:nt, :], in_=rs_[0][0:nt, :], func=AF.Exp,
                                         scale=-0.5), r=[rs_[1]], w=[rs_[1]])

    def sigmoid_from(ps_ap, Bp, nt, n, tmp, out_ap, Bout):
        S.op(ACT, lambda e: e.activation(out=tmp[0][0:nt, 0:n], in_=ps_ap, func=AF.Exp, scale=-1.0),
             r=[Bp], w=[tmp[1]])
        S.op(ACT, lambda e: e.activation(out=tmp[0][0:nt, 0:n], in_=tmp[0][0:nt, 0:n], func=AF.Ln, bias=one_c[0:nt, :]),
             r=[tmp[1], B_one], w=[tmp[1]])
        S.op(ACT, lambda e: e.activation(out=out_ap, in_=tmp[0][0:nt, 0:n], func=AF.Exp, scale=-1.0),
             r=[tmp[1]], w=[Bout])

    def proj_tok(hTs, nt, col0, ncols=512):
        p, Bp = ps()
        for c in range(8):
            S.op(PE, lambda e, c=c: e.matmul(out=p[0:nt, 0:ncols], lhsT=hTs[0][:, c, 0:nt],
                                             rhs=win[:, c, col0:col0 + ncols], start=(c == 0), stop=(c == 7)),
                 r=[hTs[1], B_win], w=[Bp])
        return p, Bp

    def proj_fm(hTs, nt, col0, nblk, m=128):
        p, Bp = ps()
        for b in range(nblk):
            for c in range(8):
                S.op(PE, lambda e, c=c, b=b: e.matmul(out=p[0:m, b * 128:b * 128 + nt],
                                                      lhsT=win[:, c, col0 + b * 128:col0 + b * 128 + m],
                                                      rhs=hTs[0][:, c, 0:nt], start=(c == 0), stop=(c == 7)),
                     r=[hTs[1], B_win], w=[Bp])
        return p, Bp

    def chk(tag):
        if STOP == tag:
            raise StopBuild()

    def phase1a(ti, kind, nt, idx, tok0):
        k = ti % 2
        X, SS, RS, HB, HT = xt[k], ssq[k], rstd[k], hb[k], hT[k]
        src = meta if kind == "m" else (xp[idx * 128:(idx + 1) * 128, :] if kind == "p"
                                        else xs[idx * 8:(idx + 1) * 8, :])
        S.dma(QS, X[0][0:nt, :], src, w=[X[1]])
        rmsnorm_stats(X[0][0:nt, :], X[1], nt, SS, RS, D)
        S.op(DVE, lambda e: e.scalar_tensor_tensor(out=HB[0][0:nt, :], in0=X[0][0:nt, :], scalar=RS[0][0:nt, :],
                                                   in1=gmix_bc[0:nt, :], op0=ALU.mult, op1=ALU.mult),
             r=[X[1], RS[1], B_gmix], w=[HB[1]])
        p, Bp = ps()
        pv = bfv(p)
        for c in range(8):
            S.op(PE, lambda e, c=c: e.transpose(out=pv[:, c * 128:c * 128 + nt], in_=HB[0][0:nt, c * 128:(c + 1) * 128],
                                                identity=id_b[0:nt, 0:nt]), r=[HB[1], B_idb], w=[Bp])
        S.op(ACT, lambda e: e.activation(out=HT[0][:, :, 0:nt],
                                         in_=pv.rearrange("p (c t) -> p c t", c=8)[:, :, 0:nt], func=AF.Copy),
             r=[Bp], w=[HT[1]])
        chk("a1")
        for (col0, TT, scr) in ((C_QF, qTt[k], qT_scr), (C_KF, kTt[k], kT_scr)):
            p, Bp = proj_fm(HT, nt, col0, 4)
            S.op(ACT, lambda e, p=p, TT=TT: e.activation(out=TT[0][:, :, 0:nt],
                                                         in_=p[:, :].rearrange("p (c t) -> p c t", c=4)[:, :, 0:nt],
                                                         func=AF.Copy), r=[Bp], w=[TT[1]])
            S.dma(QS, scr[:, :, tok0:tok0 + nt], TT[0][:, :, 0:nt], r=[TT[1]])
        p, Bp = proj_fm(HT, nt, C_LR, 1, m=16)
        LR = lrgT[k]
        S.op(ACT, lambda e: e.activation(out=LR[0][0:16, 0:nt], in_=p[0:16, 0:nt], func=AF.Copy), r=[Bp], w=[LR[1]])
        chk("a2")
        dk, dv, dlf = (nk_p, nv_p, nlf_p) if kind != "s" else (nk_s, nv_s, nlf_s)
        orow = tok0 if kind != "s" else idx * 8
        p, Bp = proj_tok(HT, nt, C_KF)
        KO = kout[k]
        S.op(ACT, lambda e: e.activation(out=KO[0][0:nt, :], in_=p[0:nt, :], func=AF.Copy), r=[Bp], w=[KO[1]])
        chk("b0")
        S.dma(QS, dk[orow:orow + nt, :], KO[0][0:nt, :], r=[KO[1]])
        chk("b1")
        p, Bp = proj_tok(HT, nt, C_VF)
        VO, V1 = vout[k], v1t[k]
        S.op(ACT, lambda e: e.activation(out=VO[0][0:nt, :], in_=p[0:nt, :], func=AF.Copy), r=[Bp], w=[VO[1]])
        S.op(DVE, lambda e: e.tensor_copy(out=V1[0][0:nt, :, 0:64],
                                          in_=p[0:nt, :].rearrange("p (h d) -> p h d", h=8)), r=[Bp], w=[V1[1]])
        chk("b2")
        S.dma(QS, dv[orow:orow + nt, :], VO[0][0:nt, :], r=[VO[1]])
        S.dma(QS, v1_scr[tok0:tok0 + nt, :], V1[0][0:nt, :, :].rearrange("p h d -> p (h d)"), r=[V1[1]])
        chk("b3")
        p, Bp = proj_tok(HT, nt, C_VG)
        VG = vgb[k]
        S.op(DVE, lambda e: e.tensor_copy(out=VG[0][0:nt, :], in_=p[0:nt, :]), r=[Bp], w=[VG[1]])
        chk("a3")
        p, Bp = proj_tok(HT, nt, C_RG)
        E0, RSL = etmp[0], rsil[k]
        sigmoid_from(p[0:nt, :], Bp, nt, 512, E0, E0[0][0:nt, 0:512], E0[1])
        S.op(DVE, lambda e: e.tensor_tensor(out=RSL[0][0:nt, :], in0=p[0:nt, :], in1=E0[0][0:nt, 0:512], op=ALU.mult),
             r=[Bp, E0[1]], w=[RSL[1]])
        S.op(DVE, lambda e: e.tensor_tensor(out=RSL[0][0:nt, :], in0=RSL[0][0:nt, :], in1=ggla_bc[0:nt, :],
                                             op=ALU.mult), r=[RSL[1], B_ggla], w=[RSL[1]])
        chk("a4")
        SGF, SGG = sgf[k], sgg[k]
        for (col0, SG, ei) in ((C_GF, SGF, 1), (C_GG, SGG, 2)):
            for half in range(2):
                p, Bp = proj_tok(HT, nt, col0 + half * 512)
                ET_ = etmp[ei]
                S.op(ACT, lambda e, p=p, ET_=ET_, half=half: e.activation(
                    out=ET_[0][0:nt, half * 512:(half + 1) * 512], in_=p[0:nt, :], func=AF.Exp, scale=-1.0),
                    r=[Bp], w=[ET_[1]])
            S.op(ACT, lambda e, ET_=ET_: e.activation(out=ET_[0][0:nt, :], in_=ET_[0][0:nt, :], func=AF.Ln,
                                                      bias=one_c[0:nt, :]), r=[ET_[1], B_one], w=[ET_[1]])
            S.op(ACT, lambda e, ET_=ET_, SG=SG: e.activation(out=SG[0][0:nt, :], in_=ET_[0][0:nt, :], func=AF.Exp,
                                                             scale=-1.0), r=[ET_[1]], w=[SG[1]])
        S.dma(QS, sgf_scr[tok0:tok0 + nt, :], SGF[0][0:nt, :], r=[SGF[1]])
        chk("a5")
        p, Bp = proj_tok(HT, nt, C_FF, 8)
        LF, LT, CT = lft[k], ltmp[k], ctile[k]
        S.op(DVE, lambda e: e.tensor_tensor(out=LT[0][0:nt, :], in0=p[0:nt, 0:8], in1=fb_bc[0:nt, :], op=ALU.add),
             r=[Bp, B_fb], w=[LT[1]])
        S.op(ACT, lambda e: e.activation(out=LT[0][0:nt, :], in_=LT[0][0:nt, :], func=AF.Exp, scale=-1.0),
             r=[LT[1]], w=[LT[1]])
        S.op(ACT, lambda e: e.activation(out=LT[0][0:nt, :], in_=LT[0][0:nt, :], func=AF.Ln, bias=one_c[0:nt, :]),
             r=[LT[1]], w=[LT[1]])
        S.op(DVE, lambda e: e.tensor_scalar(out=LF[0][0:nt, :], in0=LT[0][0:nt, :], scalar1=-1.0, scalar2=None,
                                            op0=ALU.mult), r=[LT[1]], w=[LF[1]])
        S.dma(QS, dlf[orow:orow + nt, :], LF[0][0:nt, :], r=[LF[1]])
        p, Bp = ps()
        S.op(PE, lambda e: e.matmul(out=p[0:nt, 0:8], lhsT=tri_f[0:nt, 0:nt], rhs=LF[0][0:nt, :], start=True, stop=True),
             r=[B_tri, LF[1]], w=[Bp])
        if kind == "s":
            S.op(DVE, lambda e: e.tensor_copy(out=CT[0][0:nt, :], in_=p[0:nt, 0:8]), r=[Bp], w=[CT[1]])
        else:
            S.op(DVE, lambda e: e.tensor_tensor(out=CT[0][0:nt, :], in0=p[0:nt, 0:8], in1=carry[0:nt, :], op=ALU.add),
                 r=[Bp, B_carry], w=[CT[1]])
            p2, Bp2 = ps()
            S.op(PE, lambda e: e.matmul(out=p2[:, 0:8], lhsT=ones_f[0:nt, :], rhs=LF[0][0:nt, :], start=True, stop=True),
                 r=[B_ones, LF[1]], w=[Bp2])
            S.op(DVE, lambda e: e.tensor_tensor(out=carry[:, :], in0=p2[:, 0:8], in1=carry[:, :], op=ALU.add),
                 r=[Bp2, B_carry], w=[B_carry])
        S.dma(QS, c_scr[tok0:tok0 + nt, :], CT[0][0:nt, :], r=[CT[1]])
        chk("a6")
        return

    def phase1a_gla(ti, kind, nt, idx, tok0):
        k = ti % 2
        HT, LR, VG, RSL, SGG = hT[k], lrgT[k], vgb[k], rsil[k], sgg[k]
        EA, CS, EB, ENB, QTL, KTL, KTK, AM = ea[k], csum[k], ebt[k], enbt[k], qtl[k], ktl[k], ktok[k], Am[k]
        p, Bp = ps()
        for c in range(2):
            S.op(PE, lambda e, c=c: e.matmul(out=p[:, c * 128:c * 128 + nt], lhsT=wa2[0:16, c * 128:(c + 1) * 128],
                                             rhs=LR[0][0:16, 0:nt], start=True, stop=True), r=[B_wa2, LR[1]], w=[Bp])
        for c in range(2):
            S.op(ACT, lambda e, c=c: e.activation(out=EA[0][:, c, 0:nt], in_=p[:, c * 128:c * 128 + nt], func=AF.Exp,
                                                  scale=-1.0, bias=nba[:, c:c + 1]), r=[Bp, B_nba], w=[EA[1]])
        S.op(ACT, lambda e: e.activation(out=EA[0][:, :, 0:nt], in_=EA[0][:, :, 0:nt], func=AF.Ln, bias=one_c[:, :]),
             r=[EA[1]], w=[EA[1]])
        for c in range(2):
            S.op(DVE, lambda e, c=c: e.tensor_tensor_scan(out=CS[0][:, c, 0:nt], data0=ones_f[:, 0:nt],
                                                          data1=EA[0][:, c, 0:nt], initial=0.0,
                                                          op0=ALU.mult, op1=ALU.add), r=[EA[1], B_ones], w=[CS[1]])
        S.op(ACT, lambda e: e.activation(out=EB[0][:, :, 0:nt], in_=CS[0][:, :, 0:nt], func=AF.Exp, scale=-1.0 / 16),
             r=[CS[1]], w=[EB[1]])
        S.op(ACT, lambda e: e.activation(out=ENB[0][:, :, 0:nt], in_=CS[0][:, :, 0:nt], func=AF.Exp, scale=1.0 / 16),
             r=[CS[1]], w=[ENB[1]])
        pqk, Bpqk = ps()
        for b in range(4):
            col0 = C_QG + b * 128
            for c in range(8):
                S.op(PE, lambda e, c=c, b=b, col0=col0: e.matmul(out=pqk[:, b * 128:b * 128 + nt],
                                                                 lhsT=win[:, c, col0:col0 + 128],
                                                                 rhs=HT[0][:, c, 0:nt], start=(c == 0), stop=(c == 7)),
                     r=[HT[1], B_win], w=[Bpqk])
        pq3 = pqk[:, :].rearrange("p (b t) -> p b t", b=4)
        for par in range(2):
            S.op(DVE, lambda e, par=par: e.scalar_tensor_tensor(out=QTL[0][:, par, :, 0:nt], in0=pq3[:, 0:2, 0:nt],
                                                                scalar=hm[par][0][:, 0:1], in1=EB[0][:, :, 0:nt],
                                                                op0=ALU.mult, op1=ALU.mult),
                 r=[Bpqk, EB[1], hm[par][1]], w=[QTL[1]])
        S.op(DVE, lambda e: e.tensor_tensor(out=KTL[0][:, :, 0:nt], in0=pq3[:, 2:4, 0:nt], in1=ENB[0][:, :, 0:nt],
                                            op=ALU.mult), r=[Bpqk, ENB[1]], w=[KTL[1]])
        p, Bp = ps()
        pv = bfv(p)
        for c in range(2):
            S.op(PE, lambda e, c=c: e.transpose(out=pv[0:nt, c * 128:(c + 1) * 128], in_=KTL[0][:, c, 0:nt],
                                                identity=id_b[:, :]), r=[KTL[1], B_idb], w=[Bp])
        S.op(ACT, lambda e: e.activation(out=KTK[0][0:nt, :], in_=pv[0:nt, 0:256], func=AF.Copy), r=[Bp], w=[KTK[1]])
        chk("a7")
        pa, Bpa = ps()
        for h in range(4):
            r0, c = (h % 2) * 64, h // 2
            S.op(PE, lambda e, h=h, c=c: e.matmul(out=pa[0:nt, h * 128:h * 128 + nt],
                                                  lhsT=KTL[0][:, c, 0:nt],
                                                  rhs=QTL[0][:, h % 2, c, 0:nt], start=True, stop=True),
                 r=[KTL[1], QTL[1]], w=[Bpa])
        S.op(DVE, lambda e: e.tensor_tensor(out=AM[0][0:nt, :, 0:nt],
                                            in0=pa[:, :].rearrange("p (h t) -> p h t", h=4)[0:nt, :, 0:nt],
                                            in1=tri_b[0:nt, 0:nt].unsqueeze(1).to_broadcast([nt, 4, nt]),
                                            op=ALU.mult), r=[Bpa, B_trib], w=[AM[1]])
        po_, Bpo = ps()
        for h in range(4):
            r0, c = (h % 2) * 64, h // 2
            S.op(PE, lambda e, h=h: e.matmul(out=po_[0:nt, h * 128:(h + 1) * 128], lhsT=AM[0][0:nt, h, 0:nt],
                                             rhs=VG[0][0:nt, h * 128:(h + 1) * 128], start=True, stop=False),
                 r=[AM[1], VG[1]], w=[Bpo])
            S.op(PE, lambda e, h=h, c=c: e.matmul(out=po_[0:nt, h * 128:(h + 1) * 128],
                                                  lhsT=QTL[0][:, h % 2, c, 0:nt],
                                                  rhs=Sb[:, c, :], start=False, stop=True),
                 r=[QTL[1], B_Sb], w=[Bpo])
        chk("a8")
        pd, Bpd = ps()
        for h in range(4):
            r0, c = (h % 2) * 64, h // 2
            S.op(PE, lambda e, h=h, r0=r0, c=c: e.matmul(out=pd[r0:r0 + 64, c * 128:(c + 1) * 128],
                                                         lhsT=KTK[0][0:nt, h * 64:(h + 1) * 64],
                                                         rhs=VG[0][0:nt, h * 128:(h + 1) * 128], start=True, stop=True),
                 r=[KTK[1], VG[1]], w=[Bpd])
        S.op(DVE, lambda e: e.tensor_tensor(out=Sst[:, :, :], in0=pd[:, 0:256].rearrange("p (c v) -> p c v", c=2),
                                            in1=Sst[:, :, :], op=ALU.add), r=[Bpd, B_S], w=[B_S])
        for c in range(2):
            S.op(DVE, lambda e, c=c: e.tensor_scalar(out=Sst[:, c, :], in0=Sst[:, c, :], scalar1=EB[0][:, c, nt - 1:nt],
                                                     scalar2=None, op0=ALU.mult), r=[B_S, EB[1]], w=[B_S])
        S.op(POOL, lambda e: e.tensor_copy(out=Sb[:, :, :], in_=Sst[:, :, :]), r=[B_S], w=[B_Sb])
        if DEBUG and ti == DEBUG_TILE:
            def dbg(name, t, B, shape, dt=F32):
                d = dout("dbg_" + name, shape, dt)
                S.dma(QS, d, t, r=[B])
            dbg("sp", EA[0][:, :, :], EA[1], [128, 2, 128])
            dbg("cs", CS[0][:, :, :], CS[1], [128, 2, 128])
            dbg("ktok", KTK[0][:, :], KTK[1], [128, 256], BF16)
            dbg("vgb", VG[0][:, :], VG[1], [128, 512], BF16)
            dbg("qtl", QTL[0][:, 0, :, :], QTL[1], [128, 2, 128], BF16)
            dbg("ktl", KTL[0][:, :, :], KTL[1], [128, 2, 128], BF16)
            dbg("am", AM[0][:, :, :], AM[1], [128, 4, 128], BF16)
            dbg("S", Sst[:, :, :], B_S, [128, 2, 128])
            dbg("lrg", LR[0][:, :], LR[1], [16, 128])
        chk("a9")
        SG_, RG_, O1, OGB, OGT, MG = ssg[k], rsg[k], o1[k], ogb[k], ogT[k], mgt[k]
        for h in range(4):
            S.op(ACT, lambda e, h=h: e.activation(out=junk[0:nt, 0:128], in_=po_[0:nt, h * 128:(h + 1) * 128],
                                                  func=AF.Square, accum_out=SG_[0][0:nt, h:h + 1]), r=[Bpo], w=[SG_[1]])
        S.op(ACT, lambda e: e.activation(out=RG_[0][0:nt, :], in_=SG_[0][0:nt, :], func=AF.Ln, scale=1.0 / 128,
                                         bias=eps_c[0:nt, :]), r=[SG_[1]], w=[RG_[1]])
        S.op(ACT, lambda e: e.activation(out=RG_[0][0:nt, :], in_=RG_[0][0:nt, :], func=AF.Exp, scale=-0.5),
             r=[RG_[1]], w=[RG_[1]])
        S.op(DVE, lambda e: e.tensor_tensor(out=O1[0][0:nt, :], in0=po_[0:nt, :], in1=RSL[0][0:nt, :], op=ALU.mult),
             r=[Bpo, RSL[1]], w=[O1[1]])
        S.op(DVE, lambda e: e.tensor_tensor(out=OGB[0][0:nt, :, :],
                                             in0=O1[0][0:nt, :].rearrange("p (h v) -> p h v", h=4),
                                             in1=RG_[0][0:nt, :].unsqueeze(2).to_broadcast([nt, 4, 128]),
                                             op=ALU.mult), r=[O1[1], RG_[1]], w=[OGB[1]])
        p, Bp = ps()
        pv = bfv(p)
        for c in range(4):
            S.op(PE, lambda e, c=c: e.transpose(out=pv[:, c * 128:c * 128 + nt], in_=OGB[0][0:nt, c, :],
                                                identity=id_b[0:nt, 0:nt]), r=[OGB[1], B_idb], w=[Bp])
        S.op(ACT, lambda e: e.activation(out=OGT[0][:, :, 0:nt],
                                         in_=pv[:, 0:512].rearrange("p (c t) -> p c t", c=4)[:, :, 0:nt], func=AF.Copy),
             r=[Bp], w=[OGT[1]])
        for half in range(2):
            p, Bp = ps()
            for c in range(4):
                S.op(PE, lambda e, c=c, p=p, half=half: e.matmul(out=p[0:nt, :], lhsT=OGT[0][:, c, 0:nt],
                                                                 rhs=wgo[:, c, half * 512:(half + 1) * 512],
                                                                 start=(c == 0), stop=(c == 3)),
                     r=[OGT[1], B_wgo], w=[Bp])
            S.op(DVE, lambda e, p=p, half=half: e.tensor_tensor(out=MG[0][0:nt, half * 512:(half + 1) * 512],
                                                                in0=p[0:nt, :],
                                                                in1=SGG[0][0:nt, half * 512:(half + 1) * 512],
                                                                op=ALU.mult), r=[Bp, SGG[1]], w=[MG[1]])
        S.dma(QS, mg_scr[tok0:tok0 + nt, :], MG[0][0:nt, :], r=[MG[1]])

    S.op(DVE, lambda e: e.memset(Sst[:, :, :], 0.0), w=[B_S])
    S.op(POOL, lambda e: e.memset(Sb[:, :, :], 0.0), w=[B_Sb])
    if STOP == "0w":
        S.barrier()
        return nc
    tiles1 = [("m", 16, 0, 0)] + [("p", 128, i, 16 + i * 128) for i in range(NPT)] + \
             [("s", 8, j, LTOK + j * 8) for j in range(NSS)]
    ngroups = NPT // 4

    def run_gla(n):
        kind, nt, idx, tok0 = tiles1[n]
        if kind == "s":
            S.dma(QS, Sst[:, :, :], sgl[idx].rearrange("(c t) k v -> (t k) c v", t=2), w=[B_S])
            S.op(POOL, lambda e: e.tensor_copy(out=Sb[:, :, :], in_=Sst[:, :, :]), r=[B_S], w=[B_Sb])
        phase1a_gla(n, kind, nt, idx, tok0)
        if kind == "p" and idx == NPT - 1:
            S.dma(QS, ngl_p.rearrange("(c t) k v -> (t k) c v", t=2), Sst[:, :, :], r=[B_S])
        if kind == "s":
            S.dma(QS, ngl_s[idx].rearrange("(c t) k v -> (t k) c v", t=2), Sst[:, :, :], r=[B_S])

    for n, (kind, nt, idx, tok0) in enumerate(tiles1):
        phase1a(n, kind, nt, idx, tok0)
        if kind == "m":
            S.op(DVE, lambda e: e.tensor_copy(out=cref[:, 0, :], in_=carry[:, :]), r=[B_carry], w=[B_cref])
        if kind == "p" and idx % 4 == 3 and idx // 4 + 1 < ngroups:
            g = idx // 4 + 1
            S.op(DVE, lambda e, g=g: e.tensor_copy(out=cref[:, g, :], in_=carry[:, :]), r=[B_carry], w=[B_cref])
        if n >= 1:
            run_gla(n - 1)
        if n == 0 and STOP == "1am":
            run_gla(0)
            S.barrier()
            return nc
    run_gla(len(tiles1) - 1)

    S.barrier()
    if STOP == "1a":
        return nc
    A.cur = persist_mark
    rot["n"] = 6
    wfo, B_wfo = T([128, 4, D], BF16, "wfo")
    wo, B_wo = T([128, 8, D], BF16, "wo")
    for c in range(4):
        S.dma(QP, wfo[:, c, :], w_fo[c * 128:(c + 1) * 128, :], w=[B_wfo])
    for c in range(8):
        S.dma(QP, wo[:, c, :], w_o[c * 128:(c + 1) * 128, :], w=[B_wo])
    sgfl = T2([128, D], BF16, "sgfl")
    mgl = T2([128, D], BF16, "mgl")
    xl = T2([128, D], F32, "xl")
    m1 = T2([128, D], F32, "m1")
    mrg = T2([128, D], BF16, "mrg")
    mT = T2([128, 8, 128], BF16, "mT")
    x1 = T2([128, D], F32, "x1")
    ss2 = T2([128, 1], F32, "ss2")
    rs2 = T2([128, 1], F32, "rs2")
    h2 = T2([128, D], F32, "h2")
    h2T32 = T2([128, 8, 128], F32, "h2T32")
    h2Tb = T2([128, 8, 128], BF16, "h2Tb")
    lg = T2([128, 36], F32, "lg")
    gsm = T2([128, 16], F32, "gsm")
    em = T2([128, 32], F32, "em")
    top8 = T2([128, 8], F32, "top8")
    gt1 = T2([128, 32], F32, "gt1")
    gts = T2([128, 32], F32, "gts")
    attn_mark = A.cur
    KT, B_KT = T([128, 4, LTOK], BF16, "KT")
    V1a, B_V1 = T([128, NJ, 520], BF16, "V1a")
    call, B_call = T([128, NJ, 8], F32, "call")
    bias_all, B_bias = T([128, NJ, 8], F32, "biasall")
    QTg, B_QTg = T([128, 4, 512], BF16, "QTg")
    PT = T2([128, 512], BF16, "PT", 3)
    ofn, B_ofn = T([128, 4, 512], BF16, "ofn")
    rden = T2([128, 4], F32, "rden")
    ofT = T2([128, 4, 128], BF16, "ofT")
    S.op(DVE, lambda e: e.memset(call[:, :, :], 0.0), w=[B_call])
    mcount = {"i": 0}

    def merge_tile(OFT, nt, tok0, xsrc):
        k = mcount["i"] % 2
        mcount["i"] += 1
        SGL, MGL, XL, M1, MR, MT_, X1, SS2, RS2, H2, H32, H2B, LG, GS, EM, T8, G1, GT = (
            sgfl[k], mgl[k], xl[k], m1[k], mrg[k], mT[k], x1[k], ss2[k], rs2[k], h2[k], h2T32[k], h2Tb[k],
            lg[k], gsm[k], em[k], top8[k], gt1[k], gts[k])
        S.dma(QS, SGL[0][0:nt, :], sgf_scr[tok0:tok0 + nt, :], w=[SGL[1]])
        S.dma(QS, MGL[0][0:nt, :], mg_scr[tok0:tok0 + nt, :], w=[MGL[1]])
        S.dma(QS, XL[0][0:nt, :], xsrc, w=[XL[1]])
        for half in range(2):
            p, Bp = ps()
            for c in range(4):
                S.op(PE, lambda e, c=c, p=p, half=half: e.matmul(out=p[0:nt, :], lhsT=OFT[0][:, c, 0:nt],
                                                                 rhs=wfo[:, c, half * 512:(half + 1) * 512],
                                                                 start=(c == 0), stop=(c == 3)),
                     r=[OFT[1], B_wfo], w=[Bp])
            S.op(DVE, lambda e, p=p, half=half: e.tensor_tensor(out=M1[0][0:nt, half * 512:(half + 1) * 512],
                                                                in0=p[0:nt, :],
                                                                in1=SGL[0][0:nt, half * 512:(half + 1) * 512],
                                                                op=ALU.mult), r=[Bp, SGL[1]], w=[M1[1]])
        S.op(DVE, lambda e: e.tensor_tensor(out=MR[0][0:nt, :], in0=M1[0][0:nt, :], in1=MGL[0][0:nt, :], op=ALU.add),
             r=[M1[1], MGL[1]], w=[MR[1]])
        p, Bp = ps()
        pv = bfv(p)
        for c in range(8):
            S.op(PE, lambda e, c=c: e.transpose(out=pv[:, c * 128:c * 128 + nt], in_=MR[0][0:nt, c * 128:(c + 1) * 128],
                                                identity=id_b[0:nt, 0:nt]), r=[MR[1], B_idb], w=[Bp])
        S.op(ACT, lambda e: e.activation(out=MT_[0][:, :, 0:nt],
                                         in_=pv.rearrange("p (c t) -> p c t", c=8)[:, :, 0:nt], func=AF.Copy),
             r=[Bp], w=[MT_[1]])
        for half in range(2):
            p, Bp = ps()
            for c in range(8):
                S.op(PE, lambda e, c=c, p=p, half=half: e.matmul(out=p[0:nt, :], lhsT=MT_[0][:, c, 0:nt],
                                                                 rhs=wo[:, c, half * 512:(half + 1) * 512],
                                                                 start=(c == 0), stop=(c == 7)),
                     r=[MT_[1], B_wo], w=[Bp])
            S.op(DVE, lambda e, p=p, half=half: e.tensor_tensor(out=X1[0][0:nt, half * 512:(half + 1) * 512],
                                                                in0=p[0:nt, :],
                                                                in1=XL[0][0:nt, half * 512:(half + 1) * 512],
                                                                op=ALU.add), r=[Bp, XL[1]], w=[X1[1]])
        S.dma(QS, x1_scr[tok0:tok0 + nt, :], X1[0][0:nt, :], r=[X1[1]])
        rmsnorm_stats(X1[0][0:nt, :], X1[1], nt, SS2, RS2, D)
        S.op(DVE, lambda e: e.scalar_tensor_tensor(out=H2[0][0:nt, :], in0=X1[0][0:nt, :], scalar=RS2[0][0:nt, :],
                                                   in1=gffn_bc[0:nt, :], op0=ALU.mult, op1=ALU.mult),
             r=[X1[1], RS2[1], B_gffn], w=[H2[1]])
        for half in range(2):
            p, Bp = ps()
            for c in range(4):
                cc = half * 4 + c
                S.op(PE, lambda e, c=c, cc=cc, p=p: e.transpose(out=p[:, c * 128:c * 128 + nt],
                                                                in_=H2[0][0:nt, cc * 128:(cc + 1) * 128],
                                                                identity=id_f[0:nt, 0:nt]), r=[H2[1], B_idf], w=[Bp])
            S.op(ACT, lambda e, p=p, half=half: e.activation(
                out=H32[0][:, half * 4:(half + 1) * 4, 0:nt],
                in_=p[:, :].rearrange("p (c t) -> p c t", c=4)[:, :, 0:nt], func=AF.Copy), r=[Bp], w=[H32[1]])
        S.op(POOL, lambda e: e.tensor_copy(out=H2B[0][:, :, 0:nt], in_=H32[0][:, :, 0:nt]), r=[H32[1]], w=[H2B[1]])
        S.dma(QS, h2T_scr[:, :, tok0:tok0 + nt], H2B[0][:, :, 0:nt], r=[H2B[1]])
        p, Bp = ps()
        for c in range(8):
            S.op(PE, lambda e, c=c: e.matmul(out=p[0:nt, 0:36], lhsT=H32[0][:, c, 0:nt], rhs=wr[:, c, :],
                                             start=(c == 0), stop=(c == 7)), r=[H32[1], B_wr], w=[Bp])
        S.op(DVE, lambda e: e.tensor_tensor(out=LG[0][0:nt, :], in0=p[0:nt, 0:36], in1=rb_bc[0:nt, :], op=ALU.add),
             r=[Bp, B_rb], w=[LG[1]])
        g = GS[0]
        S.op(DVE, lambda e: e.tensor_reduce(out=g[0:nt, 0:1], in_=LG[0][0:nt, 0:4], axis=AX.X, op=ALU.max),
             r=[LG[1]], w=[GS[1]])
        S.op(DVE, lambda e: e.tensor_scalar(out=g[0:nt, 1:2], in0=g[0:nt, 0:1], scalar1=-1.0, scalar2=None, op0=ALU.mult),
             r=[GS[1]], w=[GS[1]])
        S.op(DVE, lambda e: e.tensor_scalar(out=g[0:nt, 8:12], in0=LG[0][0:nt, 0:4], scalar1=g[0:nt, 0:1], scalar2=None,
                                            op0=ALU.is_equal), r=[LG[1], GS[1]], w=[GS[1]])
        S.op(DVE, lambda e: e.tensor_scalar(out=g[0:nt, 12:16], in0=g[0:nt, 8:12], scalar1=-1.0, scalar2=1e30,
                                            op0=ALU.add, op1=ALU.mult), r=[GS[1]], w=[GS[1]])
        S.op(ACT, lambda e: e.activation(out=junk[0:nt, 0:4], in_=LG[0][0:nt, 0:4], func=AF.Exp, bias=g[0:nt, 1:2],
                                         accum_out=g[0:nt, 2:3]), r=[LG[1], GS[1]], w=[GS[1]])
        S.op(DVE, lambda e: e.reciprocal(out=g[0:nt, 3:4], in_=g[0:nt, 2:3]), r=[GS[1]], w=[GS[1]])
        S.op(DVE, lambda e: e.tensor_tensor(out=EM[0][0:nt, :].rearrange("p (g k) -> p g k", g=4),
                                            in0=LG[0][0:nt, 4:36].rearrange("p (g k) -> p g k", g=4),
                                            in1=g[0:nt, 12:16].unsqueeze(2).to_broadcast([nt, 4, 8]), op=ALU.add),
             r=[LG[1], GS[1]], w=[EM[1]])
        S.op(DVE, lambda e: e.max(out=T8[0][0:nt, :], in_=EM[0][0:nt, :]), r=[EM[1]], w=[T8[1]])
        S.op(DVE, lambda e: e.tensor_tensor(out=g[0:nt, 4:5], in0=T8[0][0:nt, 1:2], in1=T8[0][0:nt, 0:1], op=ALU.subtract),
             r=[T8[1], GS[1]], w=[GS[1]])
        S.op(ACT, lambda e: e.activation(out=g[0:nt, 5:6], in_=g[0:nt, 4:5], func=AF.Exp), r=[GS[1]], w=[GS[1]])
        S.op(DVE, lambda e: e.tensor_scalar(out=g[0:nt, 5:6], in0=g[0:nt, 5:6], scalar1=1.0, scalar2=None, op0=ALU.add),
             r=[GS[1]], w=[GS[1]])
        S.op(DVE, lambda e: e.reciprocal(out=g[0:nt, 5:6], in_=g[0:nt, 5:6]), r=[GS[1]], w=[GS[1]])
        S.op(DVE, lambda e: e.tensor_tensor(out=g[0:nt, 5:6], in0=g[0:nt, 5:6], in1=g[0:nt, 3:4], op=ALU.mult),
             r=[GS[1]], w=[GS[1]])
        S.op(DVE, lambda e: e.tensor_tensor(out=g[0:nt, 6:7], in0=g[0:nt, 3:4], in1=g[0:nt, 5:6], op=ALU.subtract),
             r=[GS[1]], w=[GS[1]])
        S.op(DVE, lambda e: e.tensor_scalar(out=G1[0][0:nt, :], in0=EM[0][0:nt, :], scalar1=T8[0][0:nt, 0:1],
                                            scalar2=g[0:nt, 5:6], op0=ALU.is_equal, op1=ALU.mult),
             r=[EM[1], T8[1], GS[1]], w=[G1[1]])
        S.op(DVE, lambda e: e.tensor_scalar(out=GT[0][0:nt, :], in0=EM[0][0:nt, :], scalar1=T8[0][0:nt, 1:2],
                                            scalar2=g[0:nt, 6:7], op0=ALU.is_equal, op1=ALU.mult),
             r=[EM[1], T8[1], GS[1]], w=[GT[1]])
        S.op(DVE, lambda e: e.tensor_tensor(out=GT[0][0:nt, :], in0=GT[0][0:nt, :], in1=G1[0][0:nt, :], op=ALU.add),
             r=[GT[1], G1[1]], w=[GT[1]])
        S.dma(QS, gat_scr[tok0:tok0 + nt, :], GT[0][0:nt, :], r=[GT[1]])

    po_banks = [(psb[6], psB[6]), (psb[7], psB[7])]

    def attention_group(gi, tiles, tok0):
        nts = [16 if j == 0 else 128 for j in tiles]
        nq = sum(nts)
        j0 = tiles[0]
        jlast = tiles[-1]
        ntq = len(tiles)
        S.dma(QS, KT[:, :, tok0:tok0 + nq], kT_scr[:, :, tok0:tok0 + nq], w=[B_KT])
        for qi, j in enumerate(tiles):
            t0 = tok0 + sum(nts[:qi])
            S.dma(QS, V1a[0:nts[qi], j, :], v1_scr[t0:t0 + nts[qi], :], w=[B_V1])
            S.dma(QS, call[0:nts[qi], j, :], c_scr[t0:t0 + nts[qi], :], w=[B_call])
        S.dma(QS, QTg[:, :, 0:nq], qT_scr[:, :, tok0:tok0 + nq], w=[B_QTg])
        nj = jlast + 1
        if j0 == 0:
            S.op(DVE, lambda e: e.tensor_scalar(out=bias_all[:, 0:1, :], in0=call[:, 0:1, :], scalar1=-1.0,
                                                scalar2=None, op0=ALU.mult), r=[B_call], w=[B_bias])
        else:
            S.op(DVE, lambda e: e.tensor_tensor(out=bias_all[:, 0:nj, :],
                                                in0=cref[:, gi:gi + 1, :].to_broadcast([128, nj, 8]),
                                                in1=call[:, 0:nj, :], op=ALU.subtract),
                 r=[B_cref, B_call], w=[B_bias])
        for h in range(8):
            r0, c = (h % 2) * 64, h // 2
            po, Bpo = po_banks[h % 2]
            po3 = po[:, 0:260].rearrange("p (q d) -> p q d", q=4)
            S.op(PE, lambda e, po=po: e.matmul(out=po[:, 0:260], lhsT=zer_b[0:1, 0:128], rhs=zer_b[0:1, 0:260],
                                               start=True, stop=True), r=[B_zer], w=[Bpo])
            for j in range(nj):
                nk = 16 if j == 0 else 128
                kt0 = 0 if j == 0 else 16 + (j - 1) * 128
                m = j - j0
                col0 = max(m, 0) * 128 if j0 > 0 else 0
                ncol = nq - col0
                p, Bp = ps()
                S.op(PE, lambda e, p=p, nk=nk, kt0=kt0, col0=col0, ncol=ncol: e.matmul(
                    out=p[0:nk, 0:ncol], lhsT=KT[r0:r0 + 64, c, kt0:kt0 + nk], rhs=QTg[r0:r0 + 64, c, col0:col0 + ncol],
                    start=True, stop=True), r=[B_KT, B_QTg], w=[Bp])
                P_ = PT[(h * 64 + j) % 3]
                S.op(ACT, lambda e, p=p, P_=P_, nk=nk, ncol=ncol, j=j: e.activation(
                    out=P_[0][0:nk, 0:ncol], in_=p[0:nk, 0:ncol], func=AF.Exp, scale=0.125,
                    bias=bias_all[0:nk, j, h:h + 1]), r=[Bp, B_bias], w=[P_[1]])
                if m >= 0:
                    nd = nts[m]
                    S.op(POOL, lambda e, P_=P_, nk=nk, nd=nd: e.tensor_tensor(
                        out=P_[0][0:nk, 0:nd], in0=P_[0][0:nk, 0:nd], in1=tri_b[0:nk, 0:nd], op=ALU.mult),
                        r=[P_[1], B_trib], w=[P_[1]])
                for qt in range(max(m, 0), ntq):
                    qc0 = sum(nts[:qt]) - col0
                    nqt = nts[qt]
                    S.op(PE, lambda e, P_=P_, nk=nk, qc0=qc0, nqt=nqt, qt=qt, j=j: e.matmul(
                        out=po3[0:nqt, qt, :], lhsT=P_[0][0:nk, qc0:qc0 + nqt], rhs=V1a[0:nk, j, h * 65:(h + 1) * 65],
                        start=False, stop=True, skip_group_check=True), r=[P_[1], B_V1], w=[Bpo])
            RD = rden[h % 2]
            nqt = nts[0]
            S.op(DVE, lambda e, po3=po3, RD=RD: e.reciprocal(out=RD[0][0:nqt, 0:ntq], in_=po3[0:nqt, 0:ntq, 64]),
                 r=[Bpo], w=[RD[1]])
            S.op(DVE, lambda e, po3=po3, RD=RD, h=h: e.tensor_tensor(
                out=ofn[0:nqt, 0:ntq, h * 64:(h + 1) * 64], in0=po3[0:nqt, 0:ntq, 0:64],
                in1=RD[0][0:nqt, 0:ntq].unsqueeze(2).to_broadcast([nqt, ntq, 64]), op=ALU.mult),
                r=[Bpo, RD[1]], w=[B_ofn])
        for qi, j in enumerate(tiles):
            nt = nts[qi]
            t0 = tok0 + sum(nts[:qi])
            OF = ofT[qi % 2]
            p, Bp = ps()
            pv = bfv(p)
            for c in range(4):
                S.op(PE, lambda e, c=c, qi=qi: e.transpose(out=pv[:, c * 128:c * 128 + nt],
                                                           in_=ofn[0:nt, qi, c * 128:(c + 1) * 128],
                                                           identity=id_b[0:nt, 0:nt]), r=[B_ofn, B_idb], w=[Bp])
            S.op(ACT, lambda e, OF=OF: e.activation(out=OF[0][:, :, 0:nt],
                                                    in_=pv[:, 0:512].rearrange("p (c t) -> p c t", c=4)[:, :, 0:nt],
                                                    func=AF.Copy), r=[Bp], w=[OF[1]])
            xsrc = meta if j == 0 else xp[(j - 1) * 128:j * 128, :]
            merge_tile(OF, nt, t0, xsrc)

    attention_group(0, [0], 0)
    for g in range(ngroups):
        attention_group(g, [1 + 4 * g + i for i in range(4)], 16 + 512 * g)

    S.barrier()
    if STOP == "1b":
        return nc
    A.cur = attn_mark
    NPGp = NPG
    idxc = T2([128, 1], I32, "idxc")
    lfpg, B_lfpg = T([128, 1024], F32, "lfpg")
    lfT, B_lfT = T([128, 8, NPGp], F32, "lfT")
    pre, B_pre = T([128, 8, NPGp], F32, "pre")
    tot_s, B_tot = T([128, 8, NPGp], F32, "tots")
    wexp, B_wexp = T([128, 8, NPGp], F32, "wexp")
    QTs, B_QTs = T([128, 4, 8], BF16, "QTs")
    KTs, B_KTs = T([128, 4, 8], BF16, "KTs")
    Qbd, B_Qbd = T([128, 4, 16], BF16, "Qbd")
    Vs, B_Vs = T([8, 520], BF16, "Vs")
    cs_s, B_css = T([8, 8], F32, "css")
    wnew, B_wnew = T([8, 8], F32, "wnew")
    kpg = T2([128, 512], F32, "kpg", 3)
    vpg = T2([128, 512], F32, "vpg", 3)
    kTp = T2([128, 4, 128], BF16, "kTp")
    vbp = T2([128, 512], BF16, "vbp")
    Pf = T2([128, 64], F32, "Pf")
    Pw = T2([128, 64], BF16, "Pw")
    Ofn_s, B_Ofn = T([64, 512], BF16, "Ofns")
    rd_s, B_rds = T([64, 1], F32, "rds")
    ofTs, B_ofTs = T([128, 4, 8], BF16, "ofTs")
    Of_b, B_Of = psb[6], psB[6]
    dn_b, B_dn = psb[7], psB[7]

    def sample_seq(sj):
        tok0 = LTOK + sj * 8
        S.dma(QS, QTs[:, :, :], qT_scr[:, :, tok0:tok0 + 8], w=[B_QTs])
        S.dma(QS, KTs[:, :, :], kT_scr[:, :, tok0:tok0 + 8], w=[B_KTs])
        S.dma(QS, Vs[:, :], v1_scr[tok0:tok0 + 8, :], w=[B_Vs])
        S.dma(QS, cs_s[:, :], c_scr[tok0:tok0 + 8, :], w=[B_css])
        IX = idxc[sj % 2]
        S.dma(QS, IX[0][0:NPG, :], ptcol[sj * NPG:(sj + 1) * NPG, :], w=[IX[1]])
        S.op(POOL, lambda e: e.memset(Qbd[:, :, :], 0.0), w=[B_Qbd])
        S.op(POOL, lambda e: e.tensor_copy(out=Qbd[0:64, :, 0:8], in_=QTs[0:64, :, :]), r=[B_QTs], w=[B_Qbd])
        S.op(POOL, lambda e: e.tensor_copy(out=Qbd[64:128, :, 8:16], in_=QTs[64:128, :, :]), r=[B_QTs], w=[B_Qbd])
        S.op(ACT, lambda e: e.activation(out=wnew[:, :], in_=cs_s[:, :], func=AF.Exp, scale=-1.0), r=[B_css], w=[B_wnew])
        S.idma(lfpg[0:NPG, :], clf, IX[0][0:NPG, 0:1], r=[IX[1]], w=[B_lfpg])
        lf3 = lfpg[:, :].rearrange("p (s h) -> p s h", h=8)
        for half in range(2):
            p, Bp = ps()
            for hh in range(4):
                h = half * 4 + hh
                S.op(PE, lambda e, h=h, hh=hh, p=p: e.transpose(out=p[:, hh * NPG:(hh + 1) * NPG], in_=lf3[0:NPG, :, h],
                                                                identity=id_f[0:NPG, 0:NPG]), r=[B_lfpg, B_idf], w=[Bp])
            S.op(ACT, lambda e, p=p, half=half: e.activation(
                out=lfT[:, half * 4:(half + 1) * 4, :], in_=p[:, 0:4 * NPG].rearrange("p (h g) -> p h g", h=4),
                func=AF.Copy), r=[Bp], w=[B_lfT])
        lfTf = lfT[:, :, :].rearrange("p h g -> p (h g)")
        nn = 8 * NPG
        pw_, Bpw = [], []
        pt_, Bpt = [], []
        for c0 in range(0, nn, 512):
            c1 = min(nn, c0 + 512)
            p, Bp = ps()
            S.op(PE, lambda e, p=p, c0=c0, c1=c1: e.matmul(out=p[:, 0:c1 - c0], lhsT=triR_f[:, :], rhs=lfTf[:, c0:c1],
                                                           start=True, stop=True), r=[B_triR, B_lfT], w=[Bp])
            p2, Bp2 = ps()
            S.op(PE, lambda e, p2=p2, c0=c0, c1=c1: e.matmul(out=p2[:, 0:c1 - c0], lhsT=ones_f[:, :], rhs=lfTf[:, c0:c1],
                                                             start=True, stop=True), r=[B_ones, B_lfT], w=[Bp2])
            totf = tot_s[:, :, :].rearrange("p h g -> p (h g)")
            S.op(DVE, lambda e, p2=p2, c0=c0, c1=c1, totf=totf: e.tensor_copy(out=totf[:, c0:c1], in_=p2[:, 0:c1 - c0]),
                 r=[Bp2], w=[B_tot])
            pw_.append((p, Bp, c0, c1))
        for h in range(8):
            S.op(DVE, lambda e, h=h: e.tensor_tensor_scan(out=pre[:, h, :], data0=ones_f[:, 0:NPG], data1=tot_s[:, h, :],
                                                          initial=0.0, op0=ALU.mult, op1=ALU.add),
                 r=[B_tot, B_ones], w=[B_pre])
        S.op(DVE, lambda e: e.tensor_tensor(out=tot_s[:, :, :], in0=pre[:, :, :],
                                            in1=pre[:, :, NPG - 1:NPG].to_broadcast([128, 8, NPG]), op=ALU.subtract),
             r=[B_pre], w=[B_tot])
        wexf = wexp[:, :, :].rearrange("p h g -> p (h g)")
        pref = tot_s[:, :, :].rearrange("p h g -> p (h g)")
        for (p, Bp, c0, c1) in pw_:
            S.op(DVE, lambda e, p=p, c0=c0, c1=c1: e.tensor_tensor(out=wexf[:, c0:c1], in0=p[:, 0:c1 - c0],
                                                                   in1=pref[:, c0:c1], op=ALU.subtract),
                 r=[Bp, B_tot], w=[B_wexp])
        S.op(ACT, lambda e: e.activation(out=wexf[:, :], in_=wexf[:, :], func=AF.Exp), r=[B_wexp], w=[B_wexp])
        S.op(PE, lambda e: e.matmul(out=Of_b[0:64, :], lhsT=zer_b[0:1, 0:64], rhs=zer_b[0:1, 0:512],
                                    start=True, stop=True), r=[B_zer], w=[B_Of])
        S.op(PE, lambda e: e.matmul(out=dn_b[0:64, 0:1], lhsT=zer_b[0:1, 0:64], rhs=zer_b[0:1, 0:1],
                                    start=True, stop=True), r=[B_zer], w=[B_dn])
        for pg in range(NPG):
            KP, VP, KTP, VB, PF, PW_ = kpg[pg % 3], vpg[pg % 3], kTp[pg % 2], vbp[pg % 2], Pf[pg % 2], Pw[pg % 2]
            col = sj * NPG + pg
            S.idma(KP[0][:, :], ck_rows, idx_all[:, col:col + 1], r=[B_idx], w=[KP[1]])
            S.idma(VP[0][:, :], cv_rows, idx_all[:, col:col + 1], r=[B_idx], w=[VP[1]])
            p, Bp = ps()
            for c in range(4):
                S.op(PE, lambda e, c=c, p=p: e.transpose(out=p[:, c * 128:(c + 1) * 128], in_=KP[0][:, c * 128:(c + 1) * 128],
                                                         identity=id_f[:, :]), r=[KP[1], B_idf], w=[Bp])
            S.op(ACT, lambda e, p=p: e.activation(out=KTP[0][:, :, :], in_=p[:, :].rearrange("p (c t) -> p c t", c=4),
                                                  func=AF.Copy), r=[Bp], w=[KTP[1]])
            S.op(DVE, lambda e: e.tensor_copy(out=VB[0][:, :], in_=VP[0][:, :]), r=[VP[1]], w=[VB[1]])
            p, Bp = ps()
            for c in range(4):
                S.op(PE, lambda e, c=c, p=p: e.matmul(out=p[:, c * 16:(c + 1) * 16], lhsT=KTP[0][:, c, :], rhs=Qbd[:, c, :],
                                                      start=True, stop=True), r=[KTP[1], B_Qbd], w=[Bp])
            S.op(ACT, lambda e, p=p: e.activation(out=PF[0][:, :], in_=p[:, 0:64], func=AF.Exp, scale=0.125),
                 r=[Bp], w=[PF[1]])
            S.op(DVE, lambda e, pg=pg: e.tensor_tensor(out=PW_[0][:, :].rearrange("p (h q) -> p h q", h=8),
                                                       in0=PF[0][:, :].rearrange("p (h q) -> p h q", h=8),
                                                       in1=wexp[:, :, pg:pg + 1].to_broadcast([128, 8, 8]), op=ALU.mult),
                 r=[PF[1], B_wexp], w=[PW_[1]])
            S.op(PE, lambda e: e.matmul(out=Of_b[0:64, :], lhsT=PW_[0][:, :], rhs=VB[0][:, :], start=False, stop=True,
                                        skip_group_check=True), r=[PW_[1], VB[1]], w=[B_Of])
            S.op(PE, lambda e: e.matmul(out=dn_b[0:64, 0:1], lhsT=PW_[0][:, :], rhs=ones_b[:, 0:1], start=False, stop=True,
                                        skip_group_check=True), r=[PW_[1], B_onesb], w=[B_dn])
        PF, PW_ = Pf[0], Pw[0]
        p, Bp = ps()
        for c in range(4):
            S.op(PE, lambda e, c=c, p=p: e.matmul(out=p[0:8, c * 16:(c + 1) * 16], lhsT=KTs[:, c, :], rhs=Qbd[:, c, :],
                                                  start=True, stop=True), r=[B_KTs, B_Qbd], w=[Bp])
        S.op(ACT, lambda e: e.activation(out=PF[0][0:8, :], in_=p[0:8, 0:64], func=AF.Exp, scale=0.125), r=[Bp], w=[PF[1]])
        S.op(DVE, lambda e: e.tensor_tensor(out=PF[0][0:8, :].rearrange("p (h q) -> p h q", h=8),
                                            in0=PF[0][0:8, :].rearrange("p (h q) -> p h q", h=8),
                                            in1=wnew[:, :].unsqueeze(2).to_broadcast([8, 8, 8]), op=ALU.mult),
             r=[PF[1], B_wnew], w=[PF[1]])
        S.op(DVE, lambda e: e.tensor_tensor(out=PW_[0][0:8, :].rearrange("p (h q) -> p h q", h=8),
                                            in0=PF[0][0:8, :].rearrange("p (h q) -> p h q", h=8),
                                            in1=tri_f[0:8, 0:8].unsqueeze(1).to_broadcast([8, 8, 8]), op=ALU.mult),
             r=[PF[1], B_tri], w=[PW_[1]])
        Vs3 = Vs[:, :].rearrange("p (h d) -> p h d", h=8)
        for h in range(8):
            S.op(PE, lambda e, h=h: e.matmul(out=Of_b[0:64, h * 64:(h + 1) * 64], lhsT=PW_[0][0:8, :], rhs=Vs3[:, h, 0:64],
                                             start=False, stop=True, skip_group_check=True), r=[PW_[1], B_Vs], w=[B_Of])
        S.op(PE, lambda e: e.matmul(out=dn_b[0:64, 0:1], lhsT=PW_[0][0:8, :], rhs=ones_b[0:8, 0:1], start=False, stop=True,
                                    skip_group_check=True), r=[PW_[1], B_onesb], w=[B_dn])
        S.op(DVE, lambda e: e.reciprocal(out=rd_s[:, :], in_=dn_b[0:64, 0:1]), r=[B_dn], w=[B_rds])
        S.op(DVE, lambda e: e.tensor_scalar(out=Ofn_s[:, :], in0=Of_b[0:64, :], scalar1=rd_s[:, 0:1], scalar2=None,
                                            op0=ALU.mult), r=[B_Of, B_rds], w=[B_Ofn])
        p, Bp = ps()
        for h in range(8):
            r0, c = (h % 2) * 64, h // 2
            S.op(PE, lambda e, h=h, r0=r0, c=c, p=p: e.matmul(out=p[r0:r0 + 64, c * 8:(c + 1) * 8],
                                                              lhsT=Ofn_s[0:64, h * 64:(h + 1) * 64],
                                                              rhs=id_b[0:64, h * 8:(h + 1) * 8], start=True, stop=True),
                 r=[B_Ofn, B_idb], w=[Bp])
        S.op(ACT, lambda e: e.activation(out=ofTs[:, :, :], in_=p[:, 0:32].rearrange("p (c t) -> p c t", c=4),
                                         func=AF.Copy), r=[Bp], w=[B_ofTs])
        merge_tile((ofTs, B_ofTs), 8, tok0, xs[sj * 8:(sj + 1) * 8, :])

    for sj in range(NSS):
        sample_seq(sj)

    S.barrier()
    if STOP == "1s":
        return nc
    A.cur = SB_BASE
    rot["n"] = 8
    gfin_bc, B_gfin = T([128, D], F32, "gfin")
    S.dma(QS, gfin_bc[:, :], g_fin.partition_broadcast(128), w=[B_gfin])
    eps2, B_eps2 = T([128, 1], F32, "eps2")
    S.op(DVE, lambda e: e.memset(eps2[:, :], EPS), w=[B_eps2])
    junk2 = A.t([128, 1024], BF16, "junk2")
    ntile2 = (TOK + 127) // 128
    nsb = max(1, ntile2 // 8)
    sb_tiles = [list(range(i * 8, (i + 1) * 8 if i < nsb - 1 else ntile2)) for i in range(nsb)]
    maxt = max(len(x) for x in sb_tiles)
    yacc, B_y = T([128, maxt, D], F32, "yacc")
    gat, B_gat = T([128, maxt, 32], F32, "gat")
    h2s, B_h2s = T([128, 8, maxt * 128], BF16, "h2s")
    NWB = 3
    wgs = T2([128, 8, 256], BF16, "wgs", NWB)
    wus = T2([128, 8, 256], BF16, "wus", NWB)
    wds = T2([128, 2, D], BF16, "wds", NWB)
    ytmp = T2([128, D], F32, "ytmp")

    def load_expert(gidx):
        ex_ = gidx % NE
        k_ = gidx % NWB
        S.dma(QP, wgs[k_][0][:, :, :], w_eg[ex_].rearrange("(c p) f -> p c f", p=128), w=[wgs[k_][1]])
        S.dma(QP, wus[k_][0][:, :, :], w_eu[ex_].rearrange("(c p) f -> p c f", p=128), w=[wus[k_][1]])
        S.dma(QP, wds[k_][0][:, :, :], w_ed[ex_].rearrange("(c p) f -> p c f", p=128), w=[wds[k_][1]])

    n_exp_total = NE * len(sb_tiles)
    load_expert(0)
    if n_exp_total > 1:
        load_expert(1)
    eg = T2([128, 2, 512], F32, "eg")
    sgs = T2([128, 2, 512], F32, "sgs")
    heT = T2([128, 2, 512], BF16, "heT")
    yo = T2([128, D], F32, "yo")
    ssf_all, B_ssf = T([128, 16], F32, "ssfall")
    rsf_all, B_rsf = T([128, 16], F32, "rsfall")
    S.op(DVE, lambda e: e.memset(ssf_all[:, :], 1.0), w=[B_ssf])
    B_ytile = [Buf(f"y{i}") for i in range(maxt)]
    ecount = 0
    for tl in sb_tiles:
        t0 = tl[0] * 128
        t1 = min(TOK, (tl[-1] + 1) * 128)
        ntk = t1 - t0
        for li, ti2 in enumerate(tl):
            a0 = ti2 * 128
            n = min(128, TOK - a0)
            S.dma(QS, yacc[0:n, li, :], x1_scr[a0:a0 + n, :], w=[B_ytile[li]])
            S.dma(QS, gat[0:n, li, :], gat_scr[a0:a0 + n, :], w=[B_gat])
        S.dma(QS, h2s[:, :, 0:ntk], h2T_scr[:, :, t0:t1], w=[B_h2s])
        for ex in range(NE):
            k = ecount % NWB
            if ecount + 2 < n_exp_total:
                load_expert(ecount + 2)
            ecount += 1
            WG, WU, WD = wgs[k], wus[k], wds[k]
            for n0 in range(0, ntk, 512):
                nn_ = min(512, ntk - n0)
                kk = (n0 // 512) % 2
                EG, SGS, HE = eg[kk], sgs[kk], heT[kk]
                banks = [ps() for _ in range(4)]
                for c in range(8):
                    for bi, (wsb, fc) in enumerate(((WG, 0), (WG, 1), (WU, 0), (WU, 1))):
                        p, Bp = banks[bi]
                        S.op(PE, lambda e, p=p, wsb=wsb, fc=fc, c=c: e.matmul(
                            out=p[:, 0:nn_], lhsT=wsb[0][:, c, fc * 128:(fc + 1) * 128], rhs=h2s[:, c, n0:n0 + nn_],
                            start=(c == 0), stop=(c == 7)), r=[wsb[1], B_h2s], w=[Bp])
                for fc in range(2):
                    p, Bp = banks[fc]
                    S.op(ACT, lambda e, p=p, fc=fc: e.activation(out=SGS[0][:, fc, 0:nn_], in_=p[:, 0:nn_], func=AF.Silu),
                         r=[Bp], w=[SGS[1]])
                for fc in range(2):
                    p, Bp = banks[2 + fc]
                    S.op(DVE, lambda e, p=p, fc=fc: e.tensor_tensor(out=HE[0][:, fc, 0:nn_], in0=p[:, 0:nn_],
                                                                    in1=SGS[0][:, fc, 0:nn_], op=ALU.mult),
                         r=[Bp, SGS[1]], w=[HE[1]])
                for q0 in range(0, nn_, 128):
                    nq_ = min(128, nn_ - q0)
                    li = (n0 + q0) // 128
                    for half in range(2):
                        p, Bp = ps()
                        for fc in range(2):
                            S.op(PE, lambda e, p=p, fc=fc, half=half: e.matmul(
                                out=p[0:nq_, :], lhsT=HE[0][:, fc, q0:q0 + nq_], rhs=WD[0][:, fc, half * 512:(half + 1) * 512],
                                start=(fc == 0), stop=(fc == 1)), r=[HE[1], WD[1]], w=[Bp])
                        if (q0 // 128) % 2 == 0:
                            S.op(DVE, lambda e, p=p, half=half, li=li: e.scalar_tensor_tensor(
                                out=yacc[0:nq_, li, half * 512:(half + 1) * 512], in0=p[0:nq_, :],
                                scalar=gat[0:nq_, li, ex:ex + 1], in1=yacc[0:nq_, li, half * 512:(half + 1) * 512],
                                op0=ALU.mult, op1=ALU.add), r=[Bp, B_gat, B_ytile[li]], w=[B_ytile[li]])
                        else:
                            YT = ytmp[half]
                            S.op(ACT, lambda e, p=p, YT=YT, li=li: e.activation(
                                out=YT[0][0:nq_, 0:512], in_=p[0:nq_, :], func=AF.Identity,
                                scale=gat[0:nq_, li, ex:ex + 1]), r=[Bp, B_gat], w=[YT[1]])
                            S.op(POOL, lambda e, YT=YT, half=half, li=li: e.tensor_tensor(
                                out=yacc[0:nq_, li, half * 512:(half + 1) * 512], in0=YT[0][0:nq_, 0:512],
                                in1=yacc[0:nq_, li, half * 512:(half + 1) * 512], op=ALU.add),
                                r=[YT[1], B_ytile[li]], w=[B_ytile[li]])
        infos = []
        for li, ti2 in enumerate(tl):
            a0 = ti2 * 128
            infos.append((li, a0, min(128, TOK - a0)))
        for li, a0, n in infos:
            S.op(ACT, lambda e, li=li, n=n: e.activation(out=junk2[0:n, :], in_=yacc[0:n, li, :], func=AF.Square,
                                                         accum_out=ssf_all[0:n, li:li + 1]), r=[B_ytile[li]], w=[B_ssf])
        S.op(ACT, lambda e: e.activation(out=rsf_all[:, 0:len(tl)], in_=ssf_all[:, 0:len(tl)], func=AF.Ln, scale=1.0 / D,
                                         bias=eps2[:, :]), r=[B_ssf, B_eps2], w=[B_rsf])
        S.op(ACT, lambda e: e.activation(out=rsf_all[:, 0:len(tl)], in_=rsf_all[:, 0:len(tl)], func=AF.Exp, scale=-0.5),
             r=[B_rsf], w=[B_rsf])
        for li, a0, n in infos:
            YO = yo[li % 2]
            S.op(DVE, lambda e, li=li, n=n, YO=YO: e.scalar_tensor_tensor(
                out=YO[0][0:n, :], in0=yacc[0:n, li, :], scalar=rsf_all[0:n, li:li + 1], in1=gfin_bc[0:n, :],
                op0=ALU.mult, op1=ALU.mult), r=[B_ytile[li], B_rsf, B_gfin], w=[YO[1]])
            lo, hi = max(a0, 16), min(a0 + n, LTOK)
            if hi > lo:
                S.dma(QS, y_p[lo - 16:hi - 16, :], YO[0][lo - a0:hi - a0, :], r=[YO[1]])
            lo, hi = max(a0, LTOK), min(a0 + n, TOK)
            if hi > lo:
                S.dma(QS, y_s[lo - LTOK:hi - LTOK, :], YO[0][lo - a0:hi - a0, :], r=[YO[1]])
    S.barrier()
    return nc


_CACHE = {}


def kernel(x_prompt, x_sample, cache_k, cache_v, cache_log_f, state_gla, page_table,
           meta_tokens, norm_mix_g, w_in, fox_f_bias, gla_w_a2, gla_b_a, gla_norm_g,
           w_fox_out, w_gla_out, w_o, norm_ffn_g, w_group_router, b_group_router,
           w_expert_router, b_expert_router, w_expert_gate, w_expert_up, w_expert_down,
           norm_final_g):
    f = lambda a: np.ascontiguousarray(np.asarray(a), dtype=np.float32)
    x_prompt, x_sample = f(x_prompt), f(x_sample)
    B, SEQ, _ = x_prompt.shape
    DB, DS, _ = x_sample.shape
    NC = 8
    NPT = SEQ // 128
    NSS = DB // NC
    NPG = page_table.shape[1]
    NPHYS = cache_k.shape[1]
    LTOK = 16 + SEQ
    key = (NPT, NSS, NPG, NPHYS)
    if key not in _CACHE:
        _CACHE[key] = build(*key)
    nc = _CACHE[key]
    ck = f(cache_k)[0].reshape(NPHYS, 128, 512)
    cv = f(cache_v)[0].reshape(NPHYS, 128, 512)
    clf = f(cache_log_f)[0].reshape(NPHYS, 1024)
    pt = np.ascontiguousarray(np.asarray(page_table), dtype=np.int32)
    shared = dict(
        ck=ck, cv=cv, clf=clf, meta=f(meta_tokens), g_mix=f(norm_mix_g)[0], w_in=f(w_in)[0],
        f_bias=f(fox_f_bias)[0], w_a2=f(gla_w_a2)[0], b_a=f(gla_b_a)[0], g_gla=f(gla_norm_g)[0],
        w_fo=f(w_fox_out)[0], w_go=f(w_gla_out)[0], w_o=f(w_o)[0], g_ffn=f(norm_ffn_g)[0],
        w_gr=f(w_group_router)[0], b_gr=f(b_group_router)[0], w_er=f(w_expert_router)[0],
        b_er=f(b_expert_router)[0], w_eg=f(w_expert_gate)[0], w_eu=f(w_expert_up)[0],
        w_ed=f(w_expert_down)[0], g_fin=f(norm_final_g))
    in_maps = []
    for c in range(NC):
        m = dict(shared)
        m["xp"] = x_prompt[c]
        m["xs"] = x_sample[c * NSS:(c + 1) * NSS].reshape(NSS * DS, D)
        m["sgl"] = f(state_gla)[0, c * NSS:(c + 1) * NSS]
        ptc = pt[c * NSS:(c + 1) * NSS].reshape(1, NSS * NPG)
        m["ptab"] = ptc
        m["ptcol"] = np.ascontiguousarray(ptc.reshape(NSS * NPG, 1))
        in_maps.append(m)
    res = run_bass_kernel_spmd(nc, in_maps, core_ids=list(range(NC))).results
    cat = lambda k: np.stack([np.asarray(r[k]) for r in res])
    y_prompt = cat("y_p").reshape(B, SEQ, D)
    y_sample = cat("y_s").reshape(DB, DS, D)
    nk_p = cat("nk_p").reshape(1, B, LTOK, 8, 64)
    nv_p = cat("nv_p").reshape(1, B, LTOK, 8, 64)
    nlf_p = cat("nlf_p").reshape(1, B, LTOK, 8)
    ngl_p = cat("ngl_p").reshape(1, B, 4, 64, 128)
    nk_s = cat("nk_s").reshape(1, DB, DS, 8, 64)
    nv_s = cat("nv_s").reshape(1, DB, DS, 8, 64)
    nlf_s = cat("nlf_s").reshape(1, DB, DS, 8)
    ngl_s = cat("ngl_s").reshape(1, DB, 4, 64, 128)
    return (y_prompt.astype(np.float32), y_sample.astype(np.float32), nk_p, nv_p, nlf_p, ngl_p,
            nk_s, nv_s, nlf_s, ngl_s)
```
